# Optimizing a Trainium2 kernel written in Bass

```python
import jax, jax.numpy as jnp
from jax import lax
import numpy as np


D_MODEL = 4096
BATCH = 2
SEQ = 8192
DEPTH = 1

CHUNK = 64
N_META = 16
LEAD_PAD = CHUNK - N_META
EPS = 1e-6

D_MIX = D_MODEL
GLA_WIDTH = D_MIX // 2
GLA_HEADS = 8
GLA_DV = GLA_WIDTH // GLA_HEADS
GLA_DK = GLA_DV // 2
GLA_KEY_WIDTH = GLA_HEADS * GLA_DK
GLA_GATE_RANK = 16
GLA_TAU = 16.0
CONV_WIDTH = D_MIX - GLA_WIDTH
CONV_KERNEL = 31

Q_END = GLA_KEY_WIDTH
K_END = Q_END + GLA_KEY_WIDTH
V_END = K_END + GLA_WIDTH
R_END = V_END + GLA_WIDTH
GD_END = R_END + GLA_GATE_RANK
CA_END = GD_END + CONV_WIDTH
IN_COLS = CA_END + CONV_WIDTH
IN_SPLITS = (Q_END, K_END, V_END, R_END, GD_END, CA_END)

PEER_HEADS = 8
PEER_NKEYS = 128
PEER_NEXPERTS = PEER_NKEYS * PEER_NKEYS
PEER_DKEY = 256
PEER_HALF = PEER_DKEY // 2
PEER_TOPK = 16
PEER_TOKEN_BLOCK = 64

kernel_name = 'hymba_gla_conformer_peer_block'


def rms_norm(x, g):
    xf = x.astype(jnp.float32)
    y = xf * lax.rsqrt(jnp.mean(xf * xf, axis=-1, keepdims=True) + EPS)
    return (y * g.astype(jnp.float32)).astype(x.dtype)


def layer_norm(x, g, b):
    xf = x.astype(jnp.float32)
    mu = jnp.mean(xf, axis=-1, keepdims=True)
    xc = xf - mu
    y = xc * lax.rsqrt(jnp.mean(xc * xc, axis=-1, keepdims=True) + EPS)
    return (y * g.astype(jnp.float32) + b.astype(jnp.float32)).astype(x.dtype)


def gla_chunk_causal(q, k, v, log_a):
    B, L, H, DK = q.shape
    DV = v.shape[-1]
    pad = ((0, 0), (LEAD_PAD, 0), (0, 0), (0, 0))
    q, k, v, log_a = [jnp.pad(t, pad) for t in (q, k, v, log_a)]
    NC = (L + LEAD_PAD) // CHUNK

    def to_chunks(t):
        return t.reshape(B, NC, CHUNK, H, t.shape[-1]).transpose(1, 0, 3, 2, 4)

    qc, kc, vc, ac = [to_chunks(t) for t in (q, k, v, log_a)]
    bcum = jnp.cumsum(ac, axis=3)
    total = bcum[:, :, :, -1:, :]
    k_dec = kc * jnp.exp(total - bcum)
    chunk_decay = jnp.exp(total[:, :, :, 0, :])

    def step(S, xs):
        q_t, k_t, v_t, d_t = xs
        S = d_t[..., None] * S + jnp.einsum('bhck,bhcv->bhkv', k_t, v_t)
        return S, jnp.einsum('bhck,bhkv->bhcv', q_t, S)

    S0 = jnp.zeros((B, H, DK, DV), jnp.float32)
    _, o = lax.scan(step, S0, (qc, k_dec, vc, chunk_decay))
    o = o.transpose(1, 0, 3, 2, 4).reshape(B, NC * CHUNK, H, DV)
    return o[:, LEAD_PAD:]


def token_mix(xn, w_in, w_gate_up, b_gate, gla_norm_g, w_dw, b_dw, conv_ln_g, conv_ln_b, w_out):
    B, L, _ = xn.shape
    p = xn @ w_in
    q, k, v, r, gd, ca, cb = jnp.split(p, IN_SPLITS, axis=-1)

    f32 = jnp.float32
    qh = q.astype(f32).reshape(B, L, GLA_HEADS, GLA_DK) * (GLA_DK ** -0.5)
    kh = k.astype(f32).reshape(B, L, GLA_HEADS, GLA_DK)
    vh = v.astype(f32).reshape(B, L, GLA_HEADS, GLA_DV)
    z = (gd @ w_gate_up + b_gate).astype(f32)
    log_a = (jax.nn.log_sigmoid(z) / GLA_TAU).reshape(B, L, GLA_HEADS, GLA_DK)
    o = gla_chunk_causal(qh, kh, vh, log_a).astype(xn.dtype)
    o = rms_norm(o, gla_norm_g.reshape(GLA_HEADS, GLA_DV)).reshape(B, L, GLA_WIDTH)
    gla_out = o * jax.nn.silu(r)

    u = ca * jax.nn.sigmoid(cb)
    y = lax.conv_general_dilated(
        u, w_dw[:, None, :], window_strides=(1,), padding=[(CONV_KERNEL - 1, 0)],
        dimension_numbers=('NWC', 'WIO', 'NWC'), feature_group_count=CONV_WIDTH) + b_dw
    conv_out = jax.nn.silu(layer_norm(y, conv_ln_g, conv_ln_b))

    return jnp.concatenate([gla_out, conv_out], axis=-1) @ w_out


def peer(xn, w_q, keys1, keys2, u_tab, v_tab):
    B, L, D = xn.shape
    q = (xn @ w_q).reshape(B, L, PEER_HEADS, 2, PEER_HALF)
    s1 = jnp.einsum('blhd,hnd->blhn', q[..., 0, :], keys1).astype(jnp.float32)
    s2 = jnp.einsum('blhd,hnd->blhn', q[..., 1, :], keys2).astype(jnp.float32)
    v1, i1 = lax.top_k(s1, PEER_TOPK)
    v2, i2 = lax.top_k(s2, PEER_TOPK)
    n_cand = PEER_TOPK * PEER_TOPK
    cand = (v1[..., :, None] + v2[..., None, :]).reshape(B, L, PEER_HEADS, n_cand)
    cand_idx = (i1[..., :, None] * PEER_NKEYS + i2[..., None, :]).reshape(B, L, PEER_HEADS, n_cand)
    top_s, pos = lax.top_k(cand, PEER_TOPK)
    idx = jnp.take_along_axis(cand_idx, pos, axis=-1)
    g = jax.nn.softmax(top_s, axis=-1).astype(xn.dtype)

    T = B * L
    HK = PEER_HEADS * PEER_TOPK
    padn = (-T) % PEER_TOKEN_BLOCK
    xt = jnp.pad(xn.reshape(T, D), ((0, padn), (0, 0)))
    it = jnp.pad(idx.reshape(T, HK), ((0, padn), (0, 0)))
    gt = jnp.pad(g.reshape(T, HK), ((0, padn), (0, 0)))
    nb = (T + padn) // PEER_TOKEN_BLOCK

    def block(args):
        xb, ib, gb = args
        a = jnp.einsum('td,ted->te', xb, u_tab[ib])
        hgate = jax.nn.gelu(a, approximate=False) * gb
        return jnp.einsum('te,ted->td', hgate, v_tab[ib])

    out = lax.map(block, (xt.reshape(nb, PEER_TOKEN_BLOCK, D),
                          it.reshape(nb, PEER_TOKEN_BLOCK, HK),
                          gt.reshape(nb, PEER_TOKEN_BLOCK, HK)))
    return out.reshape(-1, D)[:T].reshape(B, L, D)


def setup_inputs(seed: int = 0) -> dict:
    key = jax.random.key(seed)
    ks = jax.random.split(key, 20)
    f32 = jnp.float32
    n = lambda k, s, sc: jax.random.normal(k, s, f32) * sc
    return {
        'x': n(ks[0], (BATCH, SEQ, D_MODEL), 1.0),
        'meta_tokens': n(ks[1], (N_META, D_MODEL), 1.0),
        'norm1_g': 1.0 + n(ks[2], (DEPTH, D_MODEL), 0.02),
        'w_in': n(ks[3], (DEPTH, D_MODEL, IN_COLS), D_MODEL ** -0.5),
        'w_gate_up': n(ks[4], (DEPTH, GLA_GATE_RANK, GLA_KEY_WIDTH), GLA_GATE_RANK ** -0.5),
        'b_gate': n(ks[5], (DEPTH, GLA_KEY_WIDTH), 0.1),
        'gla_norm_g': 1.0 + n(ks[6], (DEPTH, GLA_WIDTH), 0.02),
        'w_dw': n(ks[7], (DEPTH, CONV_KERNEL, CONV_WIDTH), CONV_KERNEL ** -0.5),
        'b_dw': n(ks[8], (DEPTH, CONV_WIDTH), 0.02),
        'conv_ln_g': 1.0 + n(ks[9], (DEPTH, CONV_WIDTH), 0.02),
        'conv_ln_b': n(ks[10], (DEPTH, CONV_WIDTH), 0.02),
        'w_out': n(ks[11], (DEPTH, D_MIX, D_MODEL), D_MIX ** -0.5),
        'norm2_g': 1.0 + n(ks[12], (DEPTH, D_MODEL), 0.02),
        'peer_wq': n(ks[13], (DEPTH, D_MODEL, PEER_HEADS * PEER_DKEY), D_MODEL ** -0.5),
        'peer_keys1': n(ks[14], (DEPTH, PEER_HEADS, PEER_NKEYS, PEER_HALF), PEER_HALF ** -0.5),
        'peer_keys2': n(ks[15], (DEPTH, PEER_HEADS, PEER_NKEYS, PEER_HALF), PEER_HALF ** -0.5),
        'peer_u': n(ks[16], (DEPTH, PEER_NEXPERTS, D_MODEL), D_MODEL ** -0.5),
        'peer_v': n(ks[17], (DEPTH, PEER_NEXPERTS, D_MODEL), PEER_HEADS ** -0.5),
        'final_norm_g': 1.0 + n(ks[18], (D_MODEL,), 0.02),
    }


def reference(x, meta_tokens, norm1_g, w_in, w_gate_up, b_gate, gla_norm_g, w_dw, b_dw,
              conv_ln_g, conv_ln_b, w_out, norm2_g, peer_wq, peer_keys1, peer_keys2,
              peer_u, peer_v, final_norm_g):
    B = x.shape[0]
    meta = jnp.broadcast_to(meta_tokens[None].astype(x.dtype), (B, N_META, D_MODEL))
    h = jnp.concatenate([meta, x], axis=1)
    for l in range(DEPTH):
        h = h + token_mix(rms_norm(h, norm1_g[l]), w_in[l], w_gate_up[l], b_gate[l],
                          gla_norm_g[l], w_dw[l], b_dw[l], conv_ln_g[l], conv_ln_b[l], w_out[l])
        h = h + peer(rms_norm(h, norm2_g[l]), peer_wq[l], peer_keys1[l], peer_keys2[l],
                     peer_u[l], peer_v[l])
    return rms_norm(h[:, N_META:], final_norm_g)
```

```python
import contextlib
import numpy as np
import concourse.bass as bass
import concourse.mybir as mybir
from concourse.bass_utils import run_bass_kernel_spmd

F32 = mybir.dt.float32
BF16 = mybir.dt.bfloat16
AF = mybir.ActivationFunctionType
OP = mybir.AluOpType

D = 4096
KC = 32
TT = 256
EPS = 1e-6
N_MAIN_TILES = 8
N_PRE_TILES = 26
NCOLS = 32 * 3 + 16 * 4 + 16 * 31


class Sched:
    ENGS = ("pe", "act", "dve", "pool", "sp")

    def __init__(self, nc, same_engine_sync=True):
        self.nc = nc
        self.ops = []
        self.state = {}
        self.same_engine_sync = same_engine_sync
        self.n_dma_sems = {"pool": 28, "sp": 24, "act": 2}
        self.unbarriered_dma = []
        self.nosync = ("pe",)
        self.persist = set()

    def _entries(self, key):
        base, idx = key if isinstance(key, tuple) else (key, None)
        st = self.state.setdefault(base, {"*": [None, []]})
        if idx is None:
            return list(st.values())
        if idx not in st:
            st[idx] = [st["*"][0], list(st["*"][1])]
        return [st[idx]]

    def _add(self, eng, fn, r, w, dma):
        i = len(self.ops)
        deps = set()
        for key in r:
            for ent in self._entries(key):
                if ent[0] is not None:
                    deps.add(ent[0])
        for key in w:
            for ent in self._entries(key):
                if ent[0] is not None:
                    deps.add(ent[0])
                deps.update(ent[1])
        for key in r:
            for ent in self._entries(key):
                if not dma:
                    ent[1][:] = [j for j in ent[1] if self.ops[j]["dma"] or self.ops[j]["eng"] != eng]
                ent[1].append(i)
        for key in w:
            for ent in self._entries(key):
                ent[0] = i
                ent[1] = []
        deps.discard(i)
        pers = dma and any((k[0] if isinstance(k, tuple) else k) in self.persist for k in w)
        self.ops.append(dict(eng=eng, fn=fn, deps=deps, dma=dma, needs_inc=False, pers=pers))
        if dma and not pers:
            self.unbarriered_dma.append(i)
        return i

    def op(self, eng, fn, r=(), w=(), big=False):
        i = self._add(eng, fn, tuple(r), tuple(w), False)
        self.ops[i]["big"] = big
        return i

    def dma(self, eng, fn, r=(), w=()):
        return self._add(eng, fn, tuple(r), tuple(w), True)

    def barrier(self):
        last = {}
        for i, o in enumerate(self.ops):
            if o.get("barrier") or o["dma"]:
                continue
            last[o["eng"]] = i
        deps = set(last.values()) | set(self.unbarriered_dma)
        self.unbarriered_dma = []
        self.state = {k: v for k, v in self.state.items() if k in self.persist}
        self.ops.append(dict(eng=None, fn=None, deps=deps, dma=False, needs_inc=False, barrier=True))

    def finish(self):
        self.barrier()
        self.ops.append(dict(eng="sp", fn=None, deps=set(), dma=False, needs_inc=False))

    def plan(self):
        ops = self.ops
        pending = {e: set() for e in self.ENGS}
        for o in ops:
            if o.get("barrier"):
                for e in self.ENGS:
                    pending[e] |= o["deps"]
            else:
                e = o["eng"]
                if pending[e]:
                    o["deps"] = set(o["deps"]) | pending[e]
                    pending[e] = set()
        real = [(i, o) for i, o in enumerate(ops) if not o.get("barrier")]
        dma_ctr = {e: 0 for e in self.ENGS}
        dma_prev = {}
        for i, o in real:
            if o["dma"]:
                e = o["eng"]
                if o.get("pers"):
                    e = "cv"
                    slot = dma_ctr.get("cv", 0)
                    dma_ctr["cv"] = slot + 1
                else:
                    slot = dma_ctr[e] % self.n_dma_sems[e]
                    dma_ctr[e] += 1
                o["dslot"] = (e, slot)
                prev = dma_prev.get((e, slot))
                o["dprev"] = prev
                o["dtarget"] = (ops[prev]["dtarget"] + 16) if prev is not None else 16
                dma_prev[(e, slot)] = i

        def skip_same(p, e):
            return p["eng"] == e and (e in self.nosync or (e == "dve" and p.get("big")) or not self.same_engine_sync)

        for i, o in real:
            for d in o["deps"]:
                p = ops[d]
                if p["dma"] or skip_same(p, o["eng"]):
                    continue
                p["needs_inc"] = True
        cnt = {e: 0 for e in self.ENGS}
        for i, o in real:
            if (not o["dma"]) and o["needs_inc"]:
                cnt[o["eng"]] += 1
                o["count"] = cnt[o["eng"]]
        seen = {e: {f: 0 for f in self.ENGS} for e in self.ENGS}
        seen_dma = {e: {} for e in self.ENGS}
        for i, o in real:
            e = o["eng"]
            best = {}
            cands = []
            if o["dma"] and o["dprev"] is not None:
                cands.append(o["dprev"])
            cands.extend(o["deps"])
            for d in cands:
                p = ops[d]
                if p["dma"]:
                    key = p["dslot"]
                    if seen_dma[e].get(key, 0) < p["dtarget"]:
                        seen_dma[e][key] = p["dtarget"]
                        best[("dma", key)] = max(best.get(("dma", key), 0), p["dtarget"])
                else:
                    if skip_same(p, e):
                        continue
                    if seen[e][p["eng"]] < p["count"]:
                        seen[e][p["eng"]] = p["count"]
                        best[("eng", p["eng"])] = max(best.get(("eng", p["eng"]), 0), p["count"])
            o["waits"] = [(k[0], k[1], v) for k, v in best.items()]
        self.cnt = cnt
        return real

    def run(self):
        nc = self.nc
        real = self.plan()
        with contextlib.ExitStack() as es:
            esem = {e: es.enter_context(nc.semaphore("s_" + e)) for e in self.ENGS}
            dsem = {}
            for key in sorted(set(o["dslot"] for i, o in real if o["dma"])):
                dsem[key] = es.enter_context(nc.semaphore("d_%s_%d" % key))
            block = es.enter_context(nc.Block())
            per_eng = {e: [] for e in self.ENGS}
            for i, o in real:
                per_eng[o["eng"]].append(o)

            def emit_engine(ename, eng):
                for o in per_eng[ename]:
                    for kind, key, val in o["waits"]:
                        eng.wait_ge(dsem[key] if kind == "dma" else esem[key], val)
                    if o["fn"] is None:
                        continue
                    ins = o["fn"](eng)
                    if o["dma"]:
                        ins.then_inc(dsem[o["dslot"]], 16)
                    elif o["needs_inc"]:
                        ins.then_inc(esem[ename], 1)

            @block.tensor
            def _(eng):
                emit_engine("pe", eng)

            @block.scalar
            def _(eng):
                emit_engine("act", eng)

            @block.vector
            def _(eng):
                emit_engine("dve", eng)

            @block.gpsimd
            def _(eng):
                emit_engine("pool", eng)

            @block.sync
            def _(eng):
                emit_engine("sp", eng)


def build_nc(n_main=N_MAIN_TILES, n_pre=N_PRE_TILES):
    nc = bass.Bass("TRN2", target_bir_lowering=False)
    S = Sched(nc)

    def din(name, shape):
        return nc.dram_tensor(name, shape, F32, kind="ExternalInput").ap()

    xm = din("xm", [n_main * TT, D])
    xp = din("xp", [n_pre * TT, D])
    win = din("win", [80, 128, D])
    wgd = din("wgd", [128, KC * 16])
    wout = din("wout", [32, 128, D])
    wqd = din("wq", [16, 128, D])
    utd = din("ut", [128, 128, D])
    vrd = din("vr", [128, 128, D])
    keyd = din("keyst", [128, 16 * 128])
    wgad = din("wga", [17, 1024])
    colsd = din("cols", [128, NCOLS])
    constd = din("consts", [128, 258])
    y = nc.dram_tensor("y", [n_main * TT, D], F32, kind="ExternalOutput").ap()
    winb = nc.dram_tensor("winb", [80, 128, D], BF16).ap()
    woutb = nc.dram_tensor("woutb", [32, 128, D], BF16).ap()
    wqb = nc.dram_tensor("wqb", [16, 128, D], BF16).ap()
    utb = nc.dram_tensor("utb", [128, 128, D], BF16).ap()
    vrb = nc.dram_tensor("vrb", [128, 128, D], BF16).ap()
    S.persist.update(["winb", "woutb", "wqb", "utb", "vrb"])

    off = [16512]

    def sb(name, shape, dt, at=None):
        nb = int(np.prod(shape[1:])) * (4 if dt == F32 else 2)
        nb = (nb + 63) // 64 * 64
        if at is None:
            t = nc.alloc_sbuf_tensor_at(name, shape, dt, offset=off[0])
            off[0] += nb
        else:
            t = nc.alloc_sbuf_tensor_at(name, shape, dt, offset=at[0])
            at[0] += nb
        return t

    identF = sb("identF", [128, 128], F32)
    triN = sb("triN", [128, 128], F32)
    chunkN = sb("chunkN", [128, 2], F32)
    identB = sb("identB", [128, 128], BF16)
    onesB = sb("onesB", [128, 128], BF16)
    cols = sb("cols", [128, NCOLS], F32)
    keysT = sb("keysT", [128, 16, 128], BF16)
    wgdT = sb("wgdT", [128, KC, 16], BF16)
    wgA = sb("wgA", [32, 1024], F32)
    gdA = sb("gdA", [32, TT], F32)
    Sst = sb("Sst", [128, 8, 256], F32)
    Sbf = sb("Sbf", [128, 8, 256], BF16)
    halo = sb("halo", [128, 16, 30], F32)
    xT = sb("xT", [128, KC, TT], F32)
    xnT = sb("xnT", [128, KC, TT], BF16)
    wt = [sb("wt%d" % i, [128, KC, 128], BF16) for i in range(3)]
    xin = sb("xin", [128, D], F32)
    rstd = sb("rstd", [128, TT], F32)
    sqb = [sb("sqb%d" % i, [128, 2 * TT], BF16) for i in range(2)]
    region = off[0]
    a1 = [region]
    mixT = sb("mixT", [128, KC, TT], BF16, a1)
    nsp = sb("nsp", [128, 2, 1024], F32, a1)
    erevT = sb("erevT", [128, 8, TT], F32, a1)
    dec = sb("dec", [128, 32], F32, a1)
    qT = sb("qT", [128, 8, TT], BF16, a1)
    kdT = [sb("kdT%d" % i, [128, TT], BF16, a1) for i in range(2)]
    kd = sb("kd", [128, 2, 8, 128], BF16, a1)
    vtok = sb("vtok", [128, 2, 2048], BF16, a1)
    srT = sb("srT", [128, 16, TT], BF16, a1)
    uT = sb("uT", [128, 16, TT + 30], F32, a1)
    sig = [sb("sig%d" % i, [128, TT], F32, a1) for i in range(2)]
    tf = [sb("tf%d" % i, [128, TT], F32, a1) for i in range(3)]
    end1 = a1[0]
    a2 = [region]
    pqT = sb("pqT", [128, 16, TT], BF16, a2)
    sall = sb("sall", [128, 2, 16, 128], F32, a2)
    E2 = sb("E2", [128, 2, 8, 128], F32, a2)
    TH = sb("TH", [128, 2, 8, 128], F32, a2)
    E1 = sb("E1", [128, 2, 8, 128], F32, a2)
    vt = [sb("vt%d" % i, [128, 8, 512], BF16, a2) for i in range(2)]
    HT = [sb("HT%d" % i, [128, 8, TT], BF16, a2) for i in range(2)]
    gl = [sb("gl%d" % i, [128, TT], F32, a2) for i in range(2)]
    Gb = [sb("Gb%d" % i, [128, 128], BF16, a2) for i in range(4)]
    at = [sb("at%d" % i, [128, TT], F32, a2) for i in range(4)]
    gt = [sb("gt%d" % i, [128, 128], F32, a2) for i in range(2)]
    ga = [sb("ga%d" % i, [128, 128], F32, a2) for i in range(2)]
    v1 = sb("v1", [128, 16], F32, a2)
    v2 = sb("v2", [128, 16], F32, a2)
    c24 = sb("c24", [128, 24], F32, a2)
    cand = sb("cand", [128, 256], F32, a2)
    wk = sb("wk", [128, 256], F32, a2)
    sm = sb("sm", [128, 16], F32, a2)
    d16 = sb("d16", [128, 16], F32, a2)
    end2 = a2[0]
    assert max(end1, end2) <= 229344, (end1, end2)
    yT = xin

    P = [nc.alloc_psum_tensor("P%d" % i, [128, 512], F32) for i in range(3)]
    P3 = nc.alloc_psum_tensor("P3", [128, 1024], BF16)
    P += [None] + [nc.alloc_psum_tensor("P%d" % i, [128, 512], F32) for i in range(4, 8)]

    g1c = cols[:, 0:32]
    g2c = cols[:, 32:64]
    gfc = cols[:, 64:96]
    ggc = cols[:, 96:112]
    lgc = cols[:, 112:128]
    lbc = cols[:, 128:144]
    bdc = cols[:, 144:160]
    wdwc = cols[:, 160:160 + 496]

    def MM(out, lhsT, rhs, start, stop, r, w):
        S.op("pe", lambda e: e.matmul(out, lhsT=lhsT, rhs=rhs, start=start, stop=stop), r=r, w=w)

    def TR(out, in_, ident, r, w):
        S.op("pe", lambda e: e.transpose(out=out, in_=in_, identity=ident), r=r, w=w)

    def ACT(out, in_, func, r, w, **kw):
        S.op("act", lambda e: e.activation(out=out, in_=in_, func=func, **kw), r=r, w=w)

    def isbig(ap):
        return int(np.prod(ap.shape[1:])) >= 128

    def TS(out, in0, s1, s2, op0, op1, r, w, eng="dve"):
        if s2 is None:
            S.op(eng, lambda e: e.tensor_scalar(out=out, in0=in0, scalar1=s1, scalar2=None, op0=op0), r=r, w=w, big=isbig(out))
        else:
            S.op(eng, lambda e: e.tensor_scalar(out=out, in0=in0, scalar1=s1, scalar2=s2, op0=op0, op1=op1), r=r, w=w, big=isbig(out))

    def TTo(out, in0, in1, op, r, w, eng="dve"):
        S.op(eng, lambda e: e.tensor_tensor(out=out, in0=in0, in1=in1, op=op), r=r, w=w, big=isbig(out))

    def STT(out, in0, scalar, in1, op0, op1, r, w):
        S.op("dve", lambda e: e.scalar_tensor_tensor(out=out, in0=in0, scalar=scalar, in1=in1, op0=op0, op1=op1), r=r, w=w,
             big=isbig(out))

    def CP(eng, out, in_, r, w):
        if eng == "act":
            S.op("act", lambda e: e.activation(out=out, in_=in_, func=AF.Copy), r=r, w=w)
        else:
            S.op(eng, lambda e: e.tensor_copy(out=out, in_=in_), r=r, w=w)

    def DMA(eng, out, in_, r, w):
        S.dma(eng, lambda e: e.dma_start(out=out, in_=in_), r=r, w=w)

    cp_rr = [0]

    def cp_eng():
        cp_rr[0] += 1
        return "act" if cp_rr[0] % 2 else "dve"

    wctr = [0]

    class WStream:
        def __init__(self, srcs, depth=2):
            self.srcs = srcs
            self.issued = 0
            self.base = wctr[0]
            self.depth = depth
            wctr[0] += len(srcs)

        def get(self, i):
            while self.issued < min(len(self.srcs), i + 1 + self.depth):
                b = (self.base + self.issued) % 3
                src, skey = self.srcs[self.issued]
                DMA("sp", wt[b][:].rearrange("p k c -> p (k c)"), src, r=[skey], w=[("wt", b)])
                self.issued += 1
            return (self.base + i) % 3

    pj = [0]

    def proj(b, rhsT, rkey, ncols=128):
        bank = pj[0] % 2
        pj[0] += 1
        for kc in range(KC):
            MM(P[bank][0:ncols, 0:TT], wt[b][:, kc, 0:ncols], rhsT[:, kc, :], kc == 0, kc == KC - 1,
               r=[("wt", b), rkey], w=["P%d" % bank])
        return P[bank], "P%d" % bank

    def fm_rstd(dim, nblk, src_of, src_key_of, pbank=4):
        for kc in range(nblk):
            sq = sqb[kc % 2]
            ACT(sq[:, 0:TT], src_of(kc), AF.Square, r=[src_key_of(kc)], w=[("sqb", kc % 2)])
            MM(P[pbank][:, 0:TT], onesB[:], sq[:, 0:TT], kc == 0, kc == nblk - 1,
               r=[("sqb", kc % 2), "onesB"], w=["P%d" % pbank])
        TS(rstd[:], P[pbank][:, 0:TT], 1.0 / dim, EPS, OP.mult, OP.add, r=["P%d" % pbank], w=["rstd"])
        ACT(rstd[:], rstd[:], AF.Sqrt, r=["rstd"], w=["rstd"])
        S.op("dve", lambda e: e.reciprocal(out=rstd[:], in_=rstd[:]), r=["rstd"], w=["rstd"])

    DMA("sp", identF[:], constd[:, 0:128], r=[], w=["identF"])
    DMA("sp", triN[:], constd[:, 128:256], r=[], w=["triN"])
    DMA("sp", chunkN[:], constd[:, 256:258], r=[], w=["chunkN"])
    DMA("sp", cols[:], colsd, r=[], w=["cols"])
    DMA("sp", wgA[0:17, :], wgad, r=[], w=["wgA"])
    DMA("pool", keysT[:].rearrange("p (a b) n -> p a (b n)", a=2), keyd.rearrange("p (a b) -> p a b", a=2), r=[], w=["keysT"])
    DMA("pool", wgdT[:].rearrange("p k c -> p (k c)"), wgd, r=[], w=["wgdT"])
    CP("act", identB[:], identF[:], r=["identF"], w=["identB"])
    S.op("dve", lambda e: e.memset(onesB[:], 1.0), w=["onesB"])
    S.op("dve", lambda e: e.memset(gdA[:], 1.0), w=["gdA"])
    S.op("dve", lambda e: e.memset(Sst[:].rearrange("p h v -> p (h v)"), 0.0), w=["Sst"])
    S.op("dve", lambda e: e.memset(halo[:].rearrange("p b j -> p (b j)"), 0.0), w=["halo"])

    def convert(src3, dst3, nblk, key):
        for c0 in range(0, nblk, 16):
            n = min(16, nblk - c0)
            DMA("pool", dst3[c0:c0 + n].rearrange("b p (a e) -> (b p) a e", a=2),
                src3[c0:c0 + n].rearrange("b p (a e) -> (b p) a e", a=2), r=[], w=[(key, c0 // 16)])

    convert(win, winb, 80, "winb")
    convert(wout, woutb, 32, "woutb")
    convert(wqd, wqb, 16, "wqb")
    convert(utd, utb, 128, "utb")
    convert(vrd, vrb, 128, "vrb")

    def load_norm(src, gcol):
        for g in range(2):
            DMA("sp", xin[:], src[g * 128:(g + 1) * 128, :], r=[], w=["xin"])
            for kq in range(8):
                for i in range(4):
                    kc = kq * 4 + i
                    TR(P[2][:, i * 128:(i + 1) * 128], xin[:, kc * 128:(kc + 1) * 128], identF[:],
                       r=["xin", "identF"], w=["P2"])
                CP(cp_eng(), xT[:, kq * 4:(kq + 1) * 4, g * 128:(g + 1) * 128],
                   P[2][:, 0:512].rearrange("p (a t) -> p a t", a=4), r=["P2"], w=[("xT", kq)])
        norm_xT(gcol)

    def norm_xT(gcol):
        fm_rstd(D, KC, lambda kc: xT[:, kc, :], lambda kc: ("xT", kc // 4))
        for kc in range(KC):
            STT(xnT[:, kc, :], xT[:, kc, :], gcol[:, kc:kc + 1], rstd[:], OP.mult, OP.mult,
                r=[("xT", kc // 4), "cols", "rstd"], w=[("xnT", kc)])

    def gla_gate():
        for kc in range(KC):
            MM(P[0][0:16, 0:TT], wgdT[:, kc, :], xnT[:, kc, :], kc == 0, kc == KC - 1,
               r=["wgdT", ("xnT", kc)], w=["P0"])
        CP("act", gdA[0:16, :], P[0][0:16, 0:TT], r=["P0"], w=["gdA"])
        for g in range(2):
            for hf in range(2):
                MM(P[4 + hf][:, 0:512], gdA[0:17, g * 128:(g + 1) * 128], wgA[0:17, hf * 512:(hf + 1) * 512],
                   True, True, r=["gdA", "wgA"], w=["P%d" % (4 + hf)])
                ACT(nsp[:, g, hf * 512:(hf + 1) * 512], P[4 + hf][:, 0:512], AF.Exp, r=["P%d" % (4 + hf)],
                    w=[("nsp", g)], scale=-1.0)
            TS(nsp[:, g, :], nsp[:, g, :], 1.0, None, OP.add, None, r=[("nsp", g)], w=[("nsp", g)])
            ACT(nsp[:, g, :], nsp[:, g, :], AF.Ln, r=[("nsp", g)], w=[("nsp", g)])
        for h in range(8):
            pb = 4 + (h % 2)
            for g in range(2):
                MM(P[pb][:, g * 128:(g + 1) * 128], nsp[:, g, h * 128:(h + 1) * 128], triN[:], True, True,
                   r=[("nsp", g), "triN"], w=["P%d" % pb])
            ACT(erevT[:, h, :], P[pb][:, 0:TT], AF.Exp, r=["P%d" % pb], w=[("erevT", h)])
            for g in range(2):
                MM(P[6][:, h * 4 + g * 2:h * 4 + g * 2 + 2], nsp[:, g, h * 128:(h + 1) * 128], chunkN[:], True, True,
                   r=[("nsp", g), "chunkN"], w=["P6"])
        ACT(dec[:], P[6][:, 0:32], AF.Exp, r=["P6"], w=["dec"])

    def token_mix(src, mode, ws):
        main = mode == "main"
        load_norm(src, g1c)
        wi = [0]

        def nextw():
            b = ws.get(wi[0])
            wi[0] += 1
            return b

        yv = yT[:].rearrange("p (b t) -> p b t", b=16)

        def conv_taps(blk):
            TS(yv[:, blk, :], uT[:, blk, 0:TT], wdwc[:, blk * 31:blk * 31 + 1], bdc[:, blk:blk + 1], OP.mult, OP.add,
               r=[("uT", blk), "uTh", "cols"], w=[("yT", blk)])
            for j in range(1, 31):
                STT(yv[:, blk, :], uT[:, blk, j:j + TT], wdwc[:, blk * 31 + j:blk * 31 + j + 1], yv[:, blk, :], OP.mult, OP.add,
                    r=[("uT", blk), "uTh", "cols", ("yT", blk)], w=[("yT", blk)])

        if mode != "pre":
            if main:
                CP("act", uT[:, :, 0:30], halo[:], r=["halo"], w=["uTh"])
            for blk in range(16):
                b = nextw()
                pcb, pkey = proj(b, xnT, "xnT")
                ACT(sig[blk % 2][:], pcb[:, 0:TT], AF.Sigmoid, r=[pkey], w=[("sig", blk % 2)])
                b = nextw()
                pca, pkey = proj(b, xnT, "xnT")
                TTo(uT[:, blk, 30:30 + TT], pca[:, 0:TT], sig[blk % 2][:], OP.mult, r=[pkey, ("sig", blk % 2)], w=[("uT", blk)])
            if not main:
                CP("act", halo[:], uT[:, :, TT:TT + 30], r=["uT"], w=["halo"])
        gla_gate()
        for h in range(8):
            b = nextw()
            pk, pkey = proj(b, xnT, "xnT")
            kt = kdT[h % 2]
            TTo(kt[:], pk[:, 0:TT], erevT[:, h, :], OP.mult, r=[pkey, ("erevT", h)], w=[("kdT", h % 2)])
            for g in range(2):
                TR(P3[:, g * 128:(g + 1) * 128], kt[:, g * 128:(g + 1) * 128], identB[:],
                   r=[("kdT", h % 2), "identB"], w=["P3a"])
            CP(cp_eng(), kd[:, :, h, :], P3[:, 0:256].rearrange("p (g n) -> p g n", g=2), r=["P3a"], w=[("kd", h)])
            for a in range(2):
                b = nextw()
                pv, pkey = proj(b, xnT, "xnT")
                vtmp = sqb[a]
                CP("act", vtmp[:, 0:TT], pv[:, 0:TT], r=[pkey], w=[("sqb", a)])
                for g in range(2):
                    TR(P3[:, 256 + g * 128:256 + (g + 1) * 128], vtmp[:, g * 128:(g + 1) * 128], identB[:],
                       r=[("sqb", a), "identB"], w=["P3b"])
                CP("act", vtok[:, :, (2 * h + a) * 128:(2 * h + a + 1) * 128],
                   P3[:, 256:512].rearrange("p (g n) -> p g n", g=2), r=["P3b"], w=[("vtok", h)])
            if main:
                b = nextw()
                pq_, pkey = proj(b, xnT, "xnT")
                ACT(qT[:, h, :], pq_[:, 0:TT], AF.Copy, r=[pkey], w=[("qT", h)], scale=128.0 ** -0.5)
                for a in range(2):
                    b = nextw()
                    pr, pkey = proj(b, xnT, "xnT")
                    ACT(srT[:, 2 * h + a, :], pr[:, 0:TT], AF.Silu, r=[pkey], w=[("srT", 2 * h + a)])
            po = P[6 + (h % 2)]
            pokey = "P%d" % (6 + (h % 2))
            for c in range(4):
                g, hf = c // 2, c % 2
                pkv = P[4 + (c % 2)]
                kvkey = "P%d" % (4 + (c % 2))
                MM(pkv[:, 0:256], kd[hf * 64:(hf + 1) * 64, g, h, :], vtok[hf * 64:(hf + 1) * 64, g, h * 256:(h + 1) * 256],
                   True, True, r=[("kd", h), ("vtok", h)], w=[kvkey])
                STT(Sst[:, h, :], Sst[:, h, :], dec[:, h * 4 + c:h * 4 + c + 1], pkv[:, 0:256], OP.mult, OP.add,
                    r=[("Sst", h), "dec", kvkey], w=[("Sst", h)])
                if main:
                    CP("act", Sbf[:, h, :], Sst[:, h, :], r=[("Sst", h)], w=[("Sbf", h)])
                    for a in range(2):
                        MM(po[:, a * 256 + c * 64:a * 256 + (c + 1) * 64], Sbf[:, h, a * 128:(a + 1) * 128],
                           qT[:, h, c * 64:(c + 1) * 64], True, True, r=[("Sbf", h), ("qT", h)], w=[pokey])
            if main:
                ACT(sqb[0][:], po[:, 0:512], AF.Square, r=[pokey], w=[("sqb", 0)])
                MM(P[4][:, 0:TT], onesB[:], sqb[0][:, 0:TT], True, False, r=[("sqb", 0), "onesB"], w=["P4"])
                MM(P[4][:, 0:TT], onesB[:], sqb[0][:, TT:2 * TT], False, True, r=[("sqb", 0), "onesB"], w=["P4"])
                TS(tf[0][:], P[4][:, 0:TT], 1.0 / 256, EPS, OP.mult, OP.add, r=["P4"], w=[("tf", 0)])
                ACT(tf[0][:], tf[0][:], AF.Sqrt, r=[("tf", 0)], w=[("tf", 0)])
                S.op("dve", lambda e: e.reciprocal(out=tf[0][:], in_=tf[0][:]), r=[("tf", 0)], w=[("tf", 0)])
                for a in range(2):
                    blk = 2 * h + a
                    TTo(tf[1 + a][:], po[:, a * 256:(a + 1) * 256], tf[0][:], OP.mult, r=[pokey, ("tf", 0)], w=[("tf", 1 + a)])
                    STT(mixT[:, blk, :], tf[1 + a][:], ggc[:, blk:blk + 1], srT[:, blk, :], OP.mult, OP.mult,
                        r=[("tf", 1 + a), "cols", ("srT", blk)], w=[("mixT", blk)])
                conv_taps(2 * h)
                conv_taps(2 * h + 1)
        if not main:
            return
        CP("act", halo[:], uT[:, :, TT:TT + 30], r=["uT"], w=["halo"])
        for blk in range(16):
            ACT(sqb[0][:, 0:TT], yv[:, blk, :], AF.Copy, r=[("yT", blk)], w=[("sqb", 0)])
            MM(P[4][:, 0:TT], onesB[:], sqb[0][:, 0:TT], blk == 0, blk == 15, r=[("sqb", 0), "onesB"], w=["P4"])
            ACT(sqb[1][:, 0:TT], yv[:, blk, :], AF.Square, r=[("yT", blk)], w=[("sqb", 1)])
            MM(P[5][:, 0:TT], onesB[:], sqb[1][:, 0:TT], blk == 0, blk == 15, r=[("sqb", 1), "onesB"], w=["P5"])
        TS(tf[0][:], P[4][:, 0:TT], 1.0 / 2048, None, OP.mult, None, r=["P4"], w=[("tf", 0)])
        TTo(tf[1][:], tf[0][:], tf[0][:], OP.mult, r=[("tf", 0)], w=[("tf", 1)])
        STT(tf[1][:], P[5][:, 0:TT], 1.0 / 2048, tf[1][:], OP.mult, OP.subtract, r=["P5", ("tf", 1)], w=[("tf", 1)])
        TS(tf[1][:], tf[1][:], EPS, None, OP.add, None, r=[("tf", 1)], w=[("tf", 1)])
        ACT(tf[1][:], tf[1][:], AF.Sqrt, r=[("tf", 1)], w=[("tf", 1)])
        S.op("dve", lambda e: e.reciprocal(out=tf[1][:], in_=tf[1][:]), r=[("tf", 1)], w=[("tf", 1)])
        for blk in range(16):
            TTo(yv[:, blk, :], yv[:, blk, :], tf[0][:], OP.subtract, r=[("yT", blk), ("tf", 0)], w=[("yT", blk)])
            TTo(yv[:, blk, :], yv[:, blk, :], tf[1][:], OP.mult, r=[("yT", blk), ("tf", 1)], w=[("yT", blk)])
            TS(yv[:, blk, :], yv[:, blk, :], lgc[:, blk:blk + 1], lbc[:, blk:blk + 1], OP.mult, OP.add,
               r=[("yT", blk), "cols"], w=[("yT", blk)])
            ACT(mixT[:, 16 + blk, :], yv[:, blk, :], AF.Silu, r=[("yT", blk)], w=[("mixT", 16 + blk)])

    def out_proj(ws):
        for ob in range(32):
            b = ws.get(ob)
            po_, pkey = proj(b, mixT, "mixT")
            TTo(xT[:, ob, :], po_[:, 0:TT], xT[:, ob, :], OP.add, r=[pkey, ("xT", ob // 4)], w=[("xT", ob // 4)])

    def peer(wsq, wsu, vsrc):
        norm_xT(g2c)
        for blk in range(16):
            b = wsq.get(blk)
            pp, pkey = proj(b, xnT, "xnT")
            ACT(pqT[:, blk, :], pp[:, 0:TT], AF.Copy, r=[pkey], w=[("pqT", blk)])
        for g in range(2):
            for quad in range(4):
                for i in range(4):
                    blk = quad * 4 + i
                    MM(P[2][:, i * 128:(i + 1) * 128], pqT[:, blk, g * 128:(g + 1) * 128], keysT[:, blk, :], True, True,
                       r=[("pqT", blk), "keysT"], w=["P2"])
                CP(cp_eng(), sall[:, g, quad * 4:(quad + 1) * 4, :], P[2][:, 0:512].rearrange("p (a n) -> p a n", a=4),
                   r=["P2"], w=[("sall", g)])
        for g in range(2):
            for h in range(8):
                s1 = sall[:, g, 2 * h, :]
                s2 = sall[:, g, 2 * h + 1, :]
                for (s_, v_, vk) in ((s1, v1, "v1"), (s2, v2, "v2")):
                    S.op("dve", lambda e, s_=s_, v_=v_: e.max(out=v_[:, 0:8], in_=s_), r=[("sall", g)], w=[vk])
                    S.op("dve", lambda e, s_=s_, v_=v_: e.match_replace(out=wk[:, 0:128], in_to_replace=v_[:, 0:8], in_values=s_,
                                                                        imm_value=-1e30), r=[("sall", g), vk], w=["wk"])
                    S.op("dve", lambda e, v_=v_: e.max(out=v_[:, 8:16], in_=wk[:, 0:128]), r=["wk"], w=[vk])
                TTo(cand[:].rearrange("p (i j) -> p i j", i=16), v1[:].unsqueeze(2).to_broadcast([128, 16, 16]),
                    v2[:].unsqueeze(1).to_broadcast([128, 16, 16]), OP.add, r=["v1", "v2"], w=["cand"])
                S.op("dve", lambda e: e.max(out=c24[:, 0:8], in_=cand[:]), r=["cand"], w=["c24"])
                S.op("dve", lambda e: e.match_replace(out=wk[:], in_to_replace=c24[:, 0:8], in_values=cand[:], imm_value=-1e30),
                     r=["cand", "c24"], w=["wk"])
                S.op("dve", lambda e: e.max(out=c24[:, 8:16], in_=wk[:]), r=["wk"], w=["c24"])
                S.op("dve", lambda e: e.match_replace(out=cand[:], in_to_replace=c24[:, 8:16], in_values=wk[:], imm_value=-1e30),
                     r=["wk", "c24"], w=["cand"])
                S.op("dve", lambda e: e.max(out=c24[:, 16:24], in_=cand[:]), r=["cand"], w=["c24"])
                TS(sm[:, 0:1], c24[:, 15:16], c24[:, 16:17], 0.5, OP.add, OP.mult, r=["c24"], w=["sm"])
                TS(d16[:], c24[:, 0:16], c24[:, 0:1], None, OP.subtract, None, r=["c24"], w=["d16"])
                ACT(d16[:], d16[:], AF.Exp, r=["d16"], w=["d16", "smz"], accum_out=sm[:, 1:2])
                S.op("dve", lambda e: e.reciprocal(out=sm[:, 2:3], in_=sm[:, 1:2]), r=["smz", "sm"], w=["sm"])
                TS(gt[0][:], s1, v1[:, 0:1], None, OP.subtract, None, r=[("sall", g), "v1"], w=[("gt", 0)])
                ACT(gt[0][:], gt[0][:], AF.Exp, r=[("gt", 0)], w=[("gt", 0)])
                STT(gt[0][:], s1, v1[:, 15:16], gt[0][:], OP.is_ge, OP.mult, r=[("sall", g), "v1", ("gt", 0)], w=[("gt", 0)])
                TS(E1[:, g, h, :], gt[0][:], sm[:, 2:3], None, OP.mult, None, r=[("gt", 0), "sm"], w=[("E1", g)])
                TS(gt[1][:], s2, v2[:, 0:1], None, OP.subtract, None, r=[("sall", g), "v2"], w=[("gt", 1)])
                ACT(gt[1][:], gt[1][:], AF.Exp, r=[("gt", 1)], w=[("gt", 1)])
                STT(E2[:, g, h, :], s2, v2[:, 15:16], gt[1][:], OP.is_ge, OP.mult, r=[("sall", g), "v2", ("gt", 1)], w=[("E2", g)])
                TS(TH[:, g, h, :], s1, -1.0, sm[:, 0:1], OP.mult, OP.add, r=[("sall", g), "sm"], w=[("TH", g)])
        vctr = [0]
        actr = [0]

        def stage_A(n1):
            b = wsu.get(n1)
            pa, pakey = proj(b, xnT, "xnT")
            ACT(gl[n1 % 2][:], pa[:, 0:TT], AF.Gelu, r=[pakey], w=[("gl", n1 % 2)])

        def stage_G(n1):
            for g in range(2):
                gbuf = Gb[(n1 % 2) * 2 + g]
                gkey = ("Gb", (n1 % 2) * 2 + g)
                for h in range(8):
                    STT(gt[g][:], sall[:, g, 2 * h + 1, :], TH[:, g, h, n1:n1 + 1], E2[:, g, h, :], OP.is_ge, OP.mult,
                        r=[("sall", g), ("TH", g), ("E2", g)], w=[("gt", g)])
                    if h == 0:
                        TS(ga[g][:], gt[g][:], E1[:, g, h, n1:n1 + 1], None, OP.mult, None, r=[("gt", g), ("E1", g)], w=[("ga", g)])
                    elif h < 7:
                        STT(ga[g][:], gt[g][:], E1[:, g, h, n1:n1 + 1], ga[g][:], OP.mult, OP.add,
                            r=[("gt", g), ("E1", g), ("ga", g)], w=[("ga", g)])
                    else:
                        STT(gbuf[:], gt[g][:], E1[:, g, h, n1:n1 + 1], ga[g][:], OP.mult, OP.add,
                            r=[("gt", g), ("E1", g), ("ga", g)], w=[gkey])

        def stage_T(n1):
            eg, eb = n1 // 8, n1 % 8
            hb = eg % 2
            gs = n1 % 2
            for g in range(2):
                TR(P3[:, gs * 256 + g * 128:gs * 256 + (g + 1) * 128], Gb[gs * 2 + g][:], identB[:],
                   r=[("Gb", gs * 2 + g), "identB"], w=[("P3", gs)])
            TTo(HT[hb][:, eb, :], gl[n1 % 2][:], P3[:, gs * 256:(gs + 1) * 256], OP.mult, r=[("gl", n1 % 2), ("P3", gs)], w=[("HT", hb)])
            if eb == 7:
                for ds in range(8):
                    vb = vctr[0] % 2
                    vctr[0] += 1
                    DMA("sp", vt[vb][:].rearrange("p e d -> p (e d)"), vrb[eg * 8 + ds], r=[("vrb", (eg * 8 + ds) // 16)],
                        w=[("vt", vb)])
                    for dq in range(4):
                        db = ds * 4 + dq
                        for e8 in range(8):
                            MM(P[4 + dq][:, 0:TT], vt[vb][:, e8, dq * 128:(dq + 1) * 128], HT[hb][:, e8, :], e8 == 0, e8 == 7,
                               r=[("vt", vb), ("HT", hb)], w=["P%d" % (4 + dq)])
                        k = actr[0] % 4
                        actr[0] += 1
                        CP("act", at[k][:], P[4 + dq][:, 0:TT], r=["P%d" % (4 + dq)], w=[("at", k)])
                        TTo(xT[:, db, :], at[k][:], xT[:, db, :], OP.add, r=[("at", k), ("xT", db // 4)], w=[("xT", db // 4)],
                            eng="pool")

        for n1 in range(129):
            if n1 < 128:
                stage_A(n1)
                stage_G(n1)
            if n1 >= 1:
                stage_T(n1 - 1)

    def final_out(dst):
        fm_rstd(D, KC, lambda kc: xT[:, kc, :], lambda kc: ("xT", kc // 4))
        for kc in range(KC):
            STT(xT[:, kc, :], xT[:, kc, :], gfc[:, kc:kc + 1], rstd[:], OP.mult, OP.mult,
                r=[("xT", kc // 4), "cols", "rstd"], w=[("xT", kc // 4)])
        for g in range(2):
            for kq in range(8):
                for i in range(4):
                    kc = kq * 4 + i
                    TR(P[2][:, i * 128:(i + 1) * 128], xT[:, kc, g * 128:(g + 1) * 128], identF[:],
                       r=[("xT", kq), "identF"], w=["P2"])
                CP(cp_eng(), xin[:, kq * 512:(kq + 1) * 512], P[2][:, 0:512], r=["P2"], w=["xin"])
            DMA("sp", dst[g * 128:(g + 1) * 128, :], xin[:], r=["xin"], w=["y"])

    def win_srcs(mode):
        idx = []
        if mode != "pre":
            idx += [48 + i for i in range(32)]
        for h in range(8):
            idx += [h * 6 + i for i in (range(6) if mode == "main" else range(3))]
        return [(winb[i], ("winb", i // 16)) for i in idx]

    for t in range(n_pre):
        mode = "pre_last" if t == n_pre - 1 else "pre"
        token_mix(xp[t * TT:(t + 1) * TT, :], mode, WStream(win_srcs(mode)))
        S.barrier()
    for t in range(n_main):
        token_mix(xm[t * TT:(t + 1) * TT, :], "main", WStream(win_srcs("main")))
        out_proj(WStream([(woutb[i], ("woutb", i // 16)) for i in range(32)]))
        S.barrier()
        peer(WStream([(wqb[i], ("wqb", i // 16)) for i in range(16)]),
             WStream([(utb[i], ("utb", i // 16)) for i in range(128)]), vrd)
        final_out(y[t * TT:(t + 1) * TT, :])
        S.barrier()
    S.finish()
    S.run()
    return nc


def _blk(wm):
    k, n = wm.shape
    return np.ascontiguousarray(wm.reshape(k // 128, 128, n // 128, 128).transpose(2, 1, 0, 3)).reshape(n // 128, 128, k)


def _consts():
    c = np.zeros((128, 258), np.float32)
    c[:, 0:128] = np.eye(128, dtype=np.float32)
    t = np.arange(128)
    c[:, 128:256] = np.where((t[:, None] > t[None, :]) & ((t[:, None] // 64) == (t[None, :] // 64)), -1.0 / 16.0, 0.0)
    c[:, 256] = np.where(t < 64, -1.0 / 16.0, 0.0)
    c[:, 257] = np.where(t >= 64, -1.0 / 16.0, 0.0)
    return c


def prep_weights(w_in, w_gate_up, b_gate, gla_norm_g, w_dw, b_dw, conv_ln_g, conv_ln_b, w_out, norm1_g, norm2_g,
                 peer_wq, peer_keys1, peer_keys2, peer_u, peer_v, final_norm_g):
    f = lambda a: np.asarray(a, dtype=np.float32)
    wi = f(w_in)[0]
    q, k, v, r = wi[:, 0:1024], wi[:, 1024:2048], wi[:, 2048:4096], wi[:, 4096:6144]
    gd, ca, cb = wi[:, 6144:6160], wi[:, 6160:8208], wi[:, 8208:10256]
    qb, kb, vb, rb, cab, cbb = _blk(q), _blk(k), _blk(v), _blk(r), _blk(ca), _blk(cb)
    blocks = []
    for h in range(8):
        blocks += [kb[h], vb[2 * h], vb[2 * h + 1], qb[h], rb[2 * h], rb[2 * h + 1]]
    for b in range(16):
        blocks += [cbb[b], cab[b]]
    win = np.stack(blocks)
    wgd = np.ascontiguousarray(gd.reshape(32, 128, 16).transpose(1, 0, 2)).reshape(128, 512)
    wout = _blk(f(w_out)[0])
    wq = _blk(f(peer_wq)[0])
    ut = np.ascontiguousarray(f(peer_u)[0].reshape(128, 128, 32, 128).transpose(0, 3, 2, 1)).reshape(128, 128, 4096)
    vr = np.ascontiguousarray(f(peer_v)[0].reshape(16, 8, 128, 8, 512).transpose(0, 3, 2, 1, 4)).reshape(128, 128, 4096)
    k1, k2 = f(peer_keys1)[0], f(peer_keys2)[0]
    keyst = np.zeros((128, 16, 128), np.float32)
    for h in range(8):
        keyst[:, 2 * h, :] = k1[h].T
        keyst[:, 2 * h + 1, :] = k2[h].T
    keyst = keyst.reshape(128, 2048)
    wga = np.concatenate([f(w_gate_up)[0], f(b_gate)[0][None, :]], axis=0)
    colv = lambda a, n: np.ascontiguousarray(f(a).reshape(n, 128).T)
    cols = np.concatenate([
        colv(norm1_g[0], 32), colv(norm2_g[0], 32), colv(final_norm_g, 32),
        colv(gla_norm_g[0], 16), colv(conv_ln_g[0], 16), colv(conv_ln_b[0], 16), colv(b_dw[0], 16),
        np.ascontiguousarray(f(w_dw)[0].reshape(31, 16, 128).transpose(2, 1, 0)).reshape(128, 496),
    ], axis=1)
    assert cols.shape == (128, NCOLS)
    return dict(win=win, wgd=wgd, wout=wout, wq=wq, ut=ut, vr=vr, keyst=keyst, wga=wga,
                cols=np.ascontiguousarray(cols), consts=_consts())


def make_core_inputs(xb, meta, start, n_main, n_pre):
    xm = np.ascontiguousarray(xb[start:start + n_main * TT])
    xp = np.zeros((n_pre * TT, D), np.float32)
    pre = np.concatenate([meta, xb[:start]], axis=0)
    assert pre.shape[0] <= n_pre * TT
    xp[n_pre * TT - pre.shape[0]:] = pre
    return xm, xp


_NC_CACHE = {}


def kernel(x, meta_tokens, norm1_g, w_in, w_gate_up, b_gate, gla_norm_g, w_dw, b_dw, conv_ln_g, conv_ln_b,
           w_out, norm2_g, peer_wq, peer_keys1, peer_keys2, peer_u, peer_v, final_norm_g):
    x = np.asarray(x, dtype=np.float32)
    meta = np.asarray(meta_tokens, dtype=np.float32)
    wts = prep_weights(w_in, w_gate_up, b_gate, gla_norm_g, w_dw, b_dw, conv_ln_g, conv_ln_b, w_out, norm1_g,
                       norm2_g, peer_wq, peer_keys1, peer_keys2, peer_u, peer_v, final_norm_g)
    B, L, _ = x.shape
    per = N_MAIN_TILES * TT
    in_maps = []
    for c in range(8):
        b, j = c // 4, c % 4
        xm, xp = make_core_inputs(x[b], meta, j * per, N_MAIN_TILES, N_PRE_TILES)
        m = dict(wts)
        m["xm"] = xm
        m["xp"] = xp
        in_maps.append(m)
    if "nc" not in _NC_CACHE:
        _NC_CACHE["nc"] = build_nc()
    res = run_bass_kernel_spmd(_NC_CACHE["nc"], in_maps, core_ids=list(range(8)))
    out = np.empty((B, L, D), np.float32)
    for c in range(8):
        b, j = c // 4, c % 4
        out[b, j * per:(j + 1) * per] = res.results[c]["y"]
    return out
```

```python
import contextlib
import numpy as np
import concourse.bass as bass
import concourse.mybir as mybir
from concourse.bass_utils import run_bass_kernel_spmd

F32 = mybir.dt.float32
BF16 = mybir.dt.bfloat16
AF = mybir.ActivationFunctionType
OP = mybir.AluOpType

D = 4096
KC = 32
TT = 256
EPS = 1e-6
N_MAIN_TILES = 8
N_PRE_TILES = 26
NCOLS = 32 * 3 + 16 * 4 + 16 * 31


class Sched:
    ENGS = ("pe", "act", "dve", "pool", "sp")

    def __init__(self, nc, same_engine_sync=True):
        self.nc = nc
        self.ops = []
        self.state = {}
        self.same_engine_sync = same_engine_sync
        self.n_dma_sems = {"pool": 28, "sp": 24, "act": 2}
        self.unbarriered_dma = []
        self.nosync = ("pe",)
        self.persist = set()
        self.excl = set("P%d" % i for i in range(8))

    def _entries(self, key):
        base, idx = key if isinstance(key, tuple) else (key, None)
        st = self.state.setdefault(base, {"*": [None, []]})
        if idx is None:
            return list(st.values())
        if idx not in st:
            st[idx] = [st["*"][0], list(st["*"][1])]
        return [st[idx]]

    def _add(self, eng, fn, r, w, dma):
        xr = tuple(k for k in r if (k[0] if isinstance(k, tuple) else k) in self.excl)
        if xr:
            r = tuple(k for k in r if k not in xr)
            w = tuple(w) + xr
        i = len(self.ops)
        deps = set()
        for key in r:
            for ent in self._entries(key):
                if ent[0] is not None:
                    deps.add(ent[0])
        for key in w:
            for ent in self._entries(key):
                if ent[0] is not None:
                    deps.add(ent[0])
                deps.update(ent[1])
        for key in r:
            for ent in self._entries(key):
                if not dma:
                    ent[1][:] = [j for j in ent[1] if self.ops[j]["dma"] or self.ops[j]["eng"] != eng]
                ent[1].append(i)
        for key in w:
            for ent in self._entries(key):
                ent[0] = i
                ent[1] = []
        deps.discard(i)
        pers = dma and any((k[0] if isinstance(k, tuple) else k) in self.persist for k in w)
        self.ops.append(dict(eng=eng, fn=fn, deps=deps, dma=dma, needs_inc=False, pers=pers))
        if dma and not pers:
            self.unbarriered_dma.append(i)
        return i

    def op(self, eng, fn, r=(), w=(), big=False):
        i = self._add(eng, fn, tuple(r), tuple(w), False)
        self.ops[i]["big"] = big
        return i

    def dma(self, eng, fn, r=(), w=()):
        return self._add(eng, fn, tuple(r), tuple(w), True)

    def barrier(self):
        last = {}
        for i, o in enumerate(self.ops):
            if o.get("barrier") or o["dma"]:
                continue
            last[o["eng"]] = i
        deps = set(last.values()) | set(self.unbarriered_dma)
        self.unbarriered_dma = []
        self.state = {k: v for k, v in self.state.items() if k in self.persist}
        self.ops.append(dict(eng=None, fn=None, deps=deps, dma=False, needs_inc=False, barrier=True))

    def finish(self):
        self.barrier()
        self.ops.append(dict(eng="sp", fn=None, deps=set(), dma=False, needs_inc=False))

    def plan(self):
        ops = self.ops
        pending = {e: set() for e in self.ENGS}
        for o in ops:
            if o.get("barrier"):
                for e in self.ENGS:
                    pending[e] |= o["deps"]
            else:
                e = o["eng"]
                if pending[e]:
                    o["deps"] = set(o["deps"]) | pending[e]
                    pending[e] = set()
        real = [(i, o) for i, o in enumerate(ops) if not o.get("barrier")]
        dma_ctr = {e: 0 for e in self.ENGS}
        dma_prev = {}
        for i, o in real:
            if o["dma"]:
                e = o["eng"]
                if o.get("pers"):
                    e = "cv"
                    slot = dma_ctr.get("cv", 0)
                    dma_ctr["cv"] = slot + 1
                else:
                    slot = dma_ctr[e] % self.n_dma_sems[e]
                    dma_ctr[e] += 1
                o["dslot"] = (e, slot)
                prev = dma_prev.get((e, slot))
                o["dprev"] = prev
                o["dtarget"] = (ops[prev]["dtarget"] + 16) if prev is not None else 16
                dma_prev[(e, slot)] = i

        def skip_same(p, e):
            return p["eng"] == e and (e in self.nosync or (e == "dve" and p.get("big")) or not self.same_engine_sync)

        for i, o in real:
            for d in o["deps"]:
                p = ops[d]
                if p["dma"] or skip_same(p, o["eng"]):
                    continue
                p["needs_inc"] = True
        cnt = {e: 0 for e in self.ENGS}
        for i, o in real:
            if (not o["dma"]) and o["needs_inc"]:
                cnt[o["eng"]] += 1
                o["count"] = cnt[o["eng"]]
        seen = {e: {f: 0 for f in self.ENGS} for e in self.ENGS}
        seen_dma = {e: {} for e in self.ENGS}
        for i, o in real:
            e = o["eng"]
            best = {}
            cands = []
            if o["dma"] and o["dprev"] is not None:
                cands.append(o["dprev"])
            cands.extend(o["deps"])
            for d in cands:
                p = ops[d]
                if p["dma"]:
                    key = p["dslot"]
                    if seen_dma[e].get(key, 0) < p["dtarget"]:
                        seen_dma[e][key] = p["dtarget"]
                        best[("dma", key)] = max(best.get(("dma", key), 0), p["dtarget"])
                else:
                    if skip_same(p, e):
                        continue
                    if seen[e][p["eng"]] < p["count"]:
                        seen[e][p["eng"]] = p["count"]
                        best[("eng", p["eng"])] = max(best.get(("eng", p["eng"]), 0), p["count"])
            o["waits"] = [(k[0], k[1], v) for k, v in best.items()]
        self.cnt = cnt
        return real

    def run(self):
        nc = self.nc
        real = self.plan()
        with contextlib.ExitStack() as es:
            esem = {e: es.enter_context(nc.semaphore("s_" + e)) for e in self.ENGS}
            dsem = {}
            for key in sorted(set(o["dslot"] for i, o in real if o["dma"])):
                dsem[key] = es.enter_context(nc.semaphore("d_%s_%d" % key))
            block = es.enter_context(nc.Block())
            per_eng = {e: [] for e in self.ENGS}
            for i, o in real:
                per_eng[o["eng"]].append(o)

            def emit_engine(ename, eng):
                for o in per_eng[ename]:
                    for kind, key, val in o["waits"]:
                        eng.wait_ge(dsem[key] if kind == "dma" else esem[key], val)
                    if o["fn"] is None:
                        continue
                    ins = o["fn"](eng)
                    if o["dma"]:
                        ins.then_inc(dsem[o["dslot"]], 16)
                    elif o["needs_inc"]:
                        ins.then_inc(esem[ename], 1)

            @block.tensor
            def _(eng):
                emit_engine("pe", eng)

            @block.scalar
            def _(eng):
                emit_engine("act", eng)

            @block.vector
            def _(eng):
                emit_engine("dve", eng)

            @block.gpsimd
            def _(eng):
                emit_engine("pool", eng)

            @block.sync
            def _(eng):
                emit_engine("sp", eng)


def build_nc(n_main=N_MAIN_TILES, n_pre=N_PRE_TILES):
    nc = bass.Bass("TRN2", target_bir_lowering=False)
    S = Sched(nc)

    def din(name, shape):
        return nc.dram_tensor(name, shape, F32, kind="ExternalInput").ap()

    xm = din("xm", [n_main * TT, D])
    xp = din("xp", [n_pre * TT, D])
    win = din("win", [80, 128, D])
    wgd = din("wgd", [128, KC * 16])
    wout = din("wout", [32, 128, D])
    wqd = din("wq", [16, 128, D])
    utd = din("ut", [128, 128, D])
    vrd = din("vr", [128, 128, D])
    keyd = din("keyst", [128, 16 * 128])
    wgad = din("wga", [17, 1024])
    colsd = din("cols", [128, NCOLS])
    constd = din("consts", [128, 258])
    y = nc.dram_tensor("y", [n_main * TT, D], F32, kind="ExternalOutput").ap()
    winb = nc.dram_tensor("winb", [80, 128, D], BF16).ap()
    woutb = nc.dram_tensor("woutb", [32, 128, D], BF16).ap()
    wqb = nc.dram_tensor("wqb", [16, 128, D], BF16).ap()
    utb = nc.dram_tensor("utb", [128, 128, D], BF16).ap()
    vrb = nc.dram_tensor("vrb", [128, 128, D], BF16).ap()
    S.persist.update(["winb", "woutb", "wqb", "utb", "vrb"])

    off = [16512]

    def sb(name, shape, dt, at=None):
        nb = int(np.prod(shape[1:])) * (4 if dt == F32 else 2)
        nb = (nb + 63) // 64 * 64
        if at is None:
            t = nc.alloc_sbuf_tensor_at(name, shape, dt, offset=off[0])
            off[0] += nb
        else:
            t = nc.alloc_sbuf_tensor_at(name, shape, dt, offset=at[0])
            at[0] += nb
        return t

    identF = sb("identF", [128, 128], F32)
    triN = sb("triN", [128, 128], F32)
    chunkN = sb("chunkN", [128, 2], F32)
    identB = sb("identB", [128, 128], BF16)
    onesB = sb("onesB", [128, 128], BF16)
    cols = sb("cols", [128, NCOLS], F32)
    keysT = sb("keysT", [128, 16, 128], BF16)
    wgdT = sb("wgdT", [128, KC, 16], BF16)
    wgA = sb("wgA", [32, 1024], F32)
    gdA = sb("gdA", [32, TT], F32)
    Sst = sb("Sst", [128, 8, 256], F32)
    Sbf = sb("Sbf", [128, 8, 256], BF16)
    halo = sb("halo", [128, 16, 30], F32)
    xT = sb("xT", [128, KC, TT], F32)
    xnT = sb("xnT", [128, KC, TT], BF16)
    wt = [sb("wt%d" % i, [128, KC, 128], BF16) for i in range(3)]
    xin = sb("xin", [128, D], F32)
    rstd = sb("rstd", [128, TT], F32)
    sqb = [sb("sqb%d" % i, [128, 2 * TT], BF16) for i in range(2)]
    region = off[0]
    a1 = [region]
    mixT = sb("mixT", [128, KC, TT], BF16, a1)
    nsp = sb("nsp", [128, 2, 1024], F32, a1)
    erevT = sb("erevT", [128, 8, TT], F32, a1)
    dec = sb("dec", [128, 32], F32, a1)
    qT = sb("qT", [128, 8, TT], BF16, a1)
    kdT = [sb("kdT%d" % i, [128, TT], BF16, a1) for i in range(2)]
    kd = sb("kd", [128, 2, 8, 128], BF16, a1)
    vtok = sb("vtok", [128, 2, 2048], BF16, a1)
    srT = sb("srT", [128, 16, TT], BF16, a1)
    uT = sb("uT", [128, 16, TT + 30], F32, a1)
    sig = [sb("sig%d" % i, [128, TT], F32, a1) for i in range(2)]
    tf = [sb("tf%d" % i, [128, TT], F32, a1) for i in range(3)]
    end1 = a1[0]
    a2 = [region]
    pqT = sb("pqT", [128, 16, TT], BF16, a2)
    sall = sb("sall", [128, 2, 16, 128], F32, a2)
    E2 = sb("E2", [128, 2, 8, 128], F32, a2)
    TH = sb("TH", [128, 2, 8, 128], F32, a2)
    E1 = sb("E1", [128, 2, 8, 128], F32, a2)
    vt = [sb("vt%d" % i, [128, 8, 512], BF16, a2) for i in range(2)]
    HT = [sb("HT%d" % i, [128, 8, TT], BF16, a2) for i in range(2)]
    gl = [sb("gl%d" % i, [128, TT], F32, a2) for i in range(2)]
    Wb = [sb("Wb%d" % i, [128, 128], BF16, a2) for i in range(16)]
    gtr = [sb("gtr%d" % i, [128, 128], F32, a2) for i in range(4)]
    gt = [sb("gt%d" % i, [128, 128], F32, a2) for i in range(2)]
    ga = [sb("ga%d" % i, [128, 128], F32, a2) for i in range(2)]
    v1s = [sb("v1_%d" % i, [128, 16], F32, a2) for i in range(2)]
    v2s = [sb("v2_%d" % i, [128, 16], F32, a2) for i in range(2)]
    c24s = [sb("c24_%d" % i, [128, 24], F32, a2) for i in range(2)]
    cands = [sb("cand%d" % i, [128, 256], F32, a2) for i in range(2)]
    wks = [sb("wk%d" % i, [128, 256], F32, a2) for i in range(2)]
    sms = [sb("sm%d" % i, [128, 16], F32, a2) for i in range(2)]
    d16s = [sb("d16_%d" % i, [128, 16], F32, a2) for i in range(2)]

    end2 = a2[0]
    assert max(end1, end2) <= 229344, (end1, end2)
    yT = xin

    P = [nc.alloc_psum_tensor("P%d" % i, [128, 512], F32) for i in range(3)]
    P3 = nc.alloc_psum_tensor("P3", [128, 1024], BF16)
    P += [None] + [nc.alloc_psum_tensor("P%d" % i, [128, 512], F32) for i in range(4, 8)]
    P2b = P[2][:].bitcast(BF16)
    TSLOT = [(P3, "P3"), (P2b, "P2")]
    GSLOT = [(P3[:].bitcast(F32), "P3"), (P[2], "P2")]

    g1c = cols[:, 0:32]
    g2c = cols[:, 32:64]
    gfc = cols[:, 64:96]
    ggc = cols[:, 96:112]
    lgc = cols[:, 112:128]
    lbc = cols[:, 128:144]
    bdc = cols[:, 144:160]
    wdwc = cols[:, 160:160 + 496]

    def MM(out, lhsT, rhs, start, stop, r, w):
        S.op("pe", lambda e: e.matmul(out, lhsT=lhsT, rhs=rhs, start=start, stop=stop), r=r, w=w)

    def TR(out, in_, ident, r, w):
        S.op("pe", lambda e: e.transpose(out=out, in_=in_, identity=ident), r=r, w=w)

    def ACT(out, in_, func, r, w, **kw):
        S.op("act", lambda e: e.activation(out=out, in_=in_, func=func, **kw), r=r, w=w)

    def isbig(ap):
        return int(np.prod(ap.shape[1:])) >= 128

    def TS(out, in0, s1, s2, op0, op1, r, w, eng="dve"):
        if s2 is None:
            S.op(eng, lambda e: e.tensor_scalar(out=out, in0=in0, scalar1=s1, scalar2=None, op0=op0), r=r, w=w, big=isbig(out))
        else:
            S.op(eng, lambda e: e.tensor_scalar(out=out, in0=in0, scalar1=s1, scalar2=s2, op0=op0, op1=op1), r=r, w=w, big=isbig(out))

    def TTo(out, in0, in1, op, r, w, eng="dve"):
        S.op(eng, lambda e: e.tensor_tensor(out=out, in0=in0, in1=in1, op=op), r=r, w=w, big=isbig(out))

    def STT(out, in0, scalar, in1, op0, op1, r, w):
        S.op("dve", lambda e: e.scalar_tensor_tensor(out=out, in0=in0, scalar=scalar, in1=in1, op0=op0, op1=op1), r=r, w=w,
             big=isbig(out))

    def CP(eng, out, in_, r, w):
        if eng == "act":
            S.op("act", lambda e: e.activation(out=out, in_=in_, func=AF.Copy), r=r, w=w)
        else:
            S.op(eng, lambda e: e.tensor_copy(out=out, in_=in_), r=r, w=w)

    def DMA(eng, out, in_, r, w):
        S.dma(eng, lambda e: e.dma_start(out=out, in_=in_), r=r, w=w)

    cp_rr = [0]

    def cp_eng():
        cp_rr[0] += 1
        return "act" if cp_rr[0] % 2 else "dve"

    wctr = [0]

    class WStream:
        def __init__(self, srcs, depth=2):
            self.srcs = srcs
            self.issued = 0
            self.base = wctr[0]
            self.depth = depth
            wctr[0] += len(srcs)

        def get(self, i):
            while self.issued < min(len(self.srcs), i + 1 + self.depth):
                b = (self.base + self.issued) % 3
                src, skey = self.srcs[self.issued]
                DMA("sp", wt[b][:].rearrange("p k c -> p (k c)"), src, r=[skey], w=[("wt", b)])
                self.issued += 1
            return (self.base + i) % 3

    pj = [0]

    def proj(b, rhsT, rkey, ncols=128):
        bank = pj[0] % 2
        pj[0] += 1
        for kc in range(KC):
            MM(P[bank][0:ncols, 0:TT], wt[b][:, kc, 0:ncols], rhsT[:, kc, :], kc == 0, kc == KC - 1,
               r=[("wt", b), rkey], w=["P%d" % bank])
        return P[bank], "P%d" % bank

    def fm_rstd(dim, nblk, src_of, src_key_of, pbank=4):
        for kc in range(nblk):
            sq = sqb[kc % 2]
            ACT(sq[:, 0:TT], src_of(kc), AF.Square, r=[src_key_of(kc)], w=[("sqb", kc % 2)])
            MM(P[pbank][:, 0:TT], onesB[:], sq[:, 0:TT], kc == 0, kc == nblk - 1,
               r=[("sqb", kc % 2), "onesB"], w=["P%d" % pbank])
        TS(rstd[:], P[pbank][:, 0:TT], 1.0 / dim, EPS, OP.mult, OP.add, r=["P%d" % pbank], w=["rstd"])
        ACT(rstd[:], rstd[:], AF.Sqrt, r=["rstd"], w=["rstd"])
        S.op("dve", lambda e: e.reciprocal(out=rstd[:], in_=rstd[:]), r=["rstd"], w=["rstd"])

    DMA("sp", identF[:], constd[:, 0:128], r=[], w=["identF"])
    DMA("sp", triN[:], constd[:, 128:256], r=[], w=["triN"])
    DMA("sp", chunkN[:], constd[:, 256:258], r=[], w=["chunkN"])
    DMA("sp", cols[:], colsd, r=[], w=["cols"])
    DMA("sp", wgA[0:17, :], wgad, r=[], w=["wgA"])
    DMA("pool", keysT[:].rearrange("p (a b) n -> p a (b n)", a=2), keyd.rearrange("p (a b) -> p a b", a=2), r=[], w=["keysT"])
    DMA("pool", wgdT[:].rearrange("p k c -> p (k c)"), wgd, r=[], w=["wgdT"])
    CP("act", identB[:], identF[:], r=["identF"], w=["identB"])
    S.op("dve", lambda e: e.memset(onesB[:], 1.0), w=["onesB"])
    S.op("dve", lambda e: e.memset(gdA[:], 1.0), w=["gdA"])
    S.op("dve", lambda e: e.memset(Sst[:].rearrange("p h v -> p (h v)"), 0.0), w=["Sst"])
    S.op("dve", lambda e: e.memset(halo[:].rearrange("p b j -> p (b j)"), 0.0), w=["halo"])

    def convert(src3, dst3, nblk, key):
        for c0 in range(0, nblk, 16):
            n = min(16, nblk - c0)
            DMA("pool", dst3[c0:c0 + n].rearrange("b p (a e) -> (b p) a e", a=2),
                src3[c0:c0 + n].rearrange("b p (a e) -> (b p) a e", a=2), r=[], w=[(key, c0 // 16)])

    convert(win, winb, 80, "winb")
    convert(wout, woutb, 32, "woutb")
    convert(wqd, wqb, 16, "wqb")
    convert(utd, utb, 128, "utb")
    convert(vrd, vrb, 128, "vrb")

    def load_norm(src, gcol):
        for g in range(2):
            DMA("sp", xin[:], src[g * 128:(g + 1) * 128, :], r=[], w=["xin"])
            for kq in range(8):
                for i in range(4):
                    kc = kq * 4 + i
                    TR(P[2][:, i * 128:(i + 1) * 128], xin[:, kc * 128:(kc + 1) * 128], identF[:],
                       r=["xin", "identF"], w=["P2"])
                CP(cp_eng(), xT[:, kq * 4:(kq + 1) * 4, g * 128:(g + 1) * 128],
                   P[2][:, 0:512].rearrange("p (a t) -> p a t", a=4), r=["P2"], w=[("xT", kq)])
        norm_xT(gcol)

    def norm_xT(gcol):
        fm_rstd(D, KC, lambda kc: xT[:, kc, :], lambda kc: ("xT", kc // 4))
        for kc in range(KC):
            STT(xnT[:, kc, :], xT[:, kc, :], gcol[:, kc:kc + 1], rstd[:], OP.mult, OP.mult,
                r=[("xT", kc // 4), "cols", "rstd"], w=[("xnT", kc)])

    def gla_gate():
        for kc in range(KC):
            MM(P[0][0:16, 0:TT], wgdT[:, kc, :], xnT[:, kc, :], kc == 0, kc == KC - 1,
               r=["wgdT", ("xnT", kc)], w=["P0"])
        CP("act", gdA[0:16, :], P[0][0:16, 0:TT], r=["P0"], w=["gdA"])
        for g in range(2):
            for hf in range(2):
                MM(P[4 + hf][:, 0:512], gdA[0:17, g * 128:(g + 1) * 128], wgA[0:17, hf * 512:(hf + 1) * 512],
                   True, True, r=["gdA", "wgA"], w=["P%d" % (4 + hf)])
                ACT(nsp[:, g, hf * 512:(hf + 1) * 512], P[4 + hf][:, 0:512], AF.Exp, r=["P%d" % (4 + hf)],
                    w=[("nsp", g)], scale=-1.0)
            TS(nsp[:, g, :], nsp[:, g, :], 1.0, None, OP.add, None, r=[("nsp", g)], w=[("nsp", g)])
            ACT(nsp[:, g, :], nsp[:, g, :], AF.Ln, r=[("nsp", g)], w=[("nsp", g)])
        for h in range(8):
            pb = 4 + (h % 2)
            for g in range(2):
                MM(P[pb][:, g * 128:(g + 1) * 128], nsp[:, g, h * 128:(h + 1) * 128], triN[:], True, True,
                   r=[("nsp", g), "triN"], w=["P%d" % pb])
            ACT(erevT[:, h, :], P[pb][:, 0:TT], AF.Exp, r=["P%d" % pb], w=[("erevT", h)])
            for g in range(2):
                MM(P[6][:, h * 4 + g * 2:h * 4 + g * 2 + 2], nsp[:, g, h * 128:(h + 1) * 128], chunkN[:], True, True,
                   r=[("nsp", g), "chunkN"], w=["P6"])
        ACT(dec[:], P[6][:, 0:32], AF.Exp, r=["P6"], w=["dec"])

    def token_mix(src, mode, ws):
        main = mode == "main"
        load_norm(src, g1c)
        wi = [0]

        def nextw():
            b = ws.get(wi[0])
            wi[0] += 1
            return b

        yv = yT[:].rearrange("p (b t) -> p b t", b=16)

        def conv_taps(blk):
            TS(yv[:, blk, :], uT[:, blk, 0:TT], wdwc[:, blk * 31:blk * 31 + 1], bdc[:, blk:blk + 1], OP.mult, OP.add,
               r=[("uT", blk), "uTh", "cols"], w=[("yT", blk)])
            for j in range(1, 31):
                STT(yv[:, blk, :], uT[:, blk, j:j + TT], wdwc[:, blk * 31 + j:blk * 31 + j + 1], yv[:, blk, :], OP.mult, OP.add,
                    r=[("uT", blk), "uTh", "cols", ("yT", blk)], w=[("yT", blk)])

        if mode != "pre":
            if main:
                CP("act", uT[:, :, 0:30], halo[:], r=["halo"], w=["uTh"])
            for blk in range(16):
                b = nextw()
                pcb, pkey = proj(b, xnT, "xnT")
                ACT(sig[blk % 2][:], pcb[:, 0:TT], AF.Sigmoid, r=[pkey], w=[("sig", blk % 2)])
                b = nextw()
                pca, pkey = proj(b, xnT, "xnT")
                TTo(uT[:, blk, 30:30 + TT], pca[:, 0:TT], sig[blk % 2][:], OP.mult, r=[pkey, ("sig", blk % 2)], w=[("uT", blk)])
            if not main:
                CP("act", halo[:], uT[:, :, TT:TT + 30], r=["uT"], w=["halo"])
        gla_gate()
        for h in range(8):
            b = nextw()
            pk, pkey = proj(b, xnT, "xnT")
            kt = kdT[h % 2]
            TTo(kt[:], pk[:, 0:TT], erevT[:, h, :], OP.mult, r=[pkey, ("erevT", h)], w=[("kdT", h % 2)])
            for g in range(2):
                TR(P3[:, g * 128:(g + 1) * 128], kt[:, g * 128:(g + 1) * 128], identB[:],
                   r=[("kdT", h % 2), "identB"], w=["P3"])
            CP("act", kd[:, :, h, :], P3[:, 0:256].rearrange("p (g n) -> p g n", g=2), r=["P3"], w=[("kd", h)])
            if main:
                conv_taps(2 * h)
            for a in range(2):
                b = nextw()
                pv, pkey = proj(b, xnT, "xnT")
                vtmp = sqb[a]
                CP("act", vtmp[:, 0:TT], pv[:, 0:TT], r=[pkey], w=[("sqb", a)])
                for g in range(2):
                    TR(P2b[:, g * 128:(g + 1) * 128], vtmp[:, g * 128:(g + 1) * 128], identB[:],
                       r=[("sqb", a), "identB"], w=["P2"])
                CP("act", vtok[:, :, (2 * h + a) * 128:(2 * h + a + 1) * 128],
                   P2b[:, 0:256].rearrange("p (g n) -> p g n", g=2), r=["P2"], w=[("vtok", h)])
            if main:
                conv_taps(2 * h + 1)
                b = nextw()
                pq_, pkey = proj(b, xnT, "xnT")
                ACT(qT[:, h, :], pq_[:, 0:TT], AF.Copy, r=[pkey], w=[("qT", h)], scale=128.0 ** -0.5)
                for a in range(2):
                    b = nextw()
                    pr, pkey = proj(b, xnT, "xnT")
                    ACT(srT[:, 2 * h + a, :], pr[:, 0:TT], AF.Silu, r=[pkey], w=[("srT", 2 * h + a)])
            po = P[6 + (h % 2)]
            pokey = "P%d" % (6 + (h % 2))
            for c in range(4):
                g, hf = c // 2, c % 2
                pkv = P[4 + (c % 2)]
                kvkey = "P%d" % (4 + (c % 2))
                MM(pkv[:, 0:256], kd[hf * 64:(hf + 1) * 64, g, h, :], vtok[hf * 64:(hf + 1) * 64, g, h * 256:(h + 1) * 256],
                   True, True, r=[("kd", h), ("vtok", h)], w=[kvkey])
                STT(Sst[:, h, :], Sst[:, h, :], dec[:, h * 4 + c:h * 4 + c + 1], pkv[:, 0:256], OP.mult, OP.add,
                    r=[("Sst", h), "dec", kvkey], w=[("Sst", h)])
                if main:
                    CP("act", Sbf[:, h, :], Sst[:, h, :], r=[("Sst", h)], w=[("Sbf", h)])
                    for a in range(2):
                        MM(po[:, a * 256 + c * 64:a * 256 + (c + 1) * 64], Sbf[:, h, a * 128:(a + 1) * 128],
                           qT[:, h, c * 64:(c + 1) * 64], True, True, r=[("Sbf", h), ("qT", h)], w=[pokey])
            if main:
                ACT(sqb[0][:], po[:, 0:512], AF.Square, r=[pokey], w=[("sqb", 0)])
                MM(P[4][:, 0:TT], onesB[:], sqb[0][:, 0:TT], True, False, r=[("sqb", 0), "onesB"], w=["P4"])
                MM(P[4][:, 0:TT], onesB[:], sqb[0][:, TT:2 * TT], False, True, r=[("sqb", 0), "onesB"], w=["P4"])
                TS(tf[0][:], P[4][:, 0:TT], 1.0 / 256, EPS, OP.mult, OP.add, r=["P4"], w=[("tf", 0)])
                ACT(tf[0][:], tf[0][:], AF.Sqrt, r=[("tf", 0)], w=[("tf", 0)])
                S.op("dve", lambda e: e.reciprocal(out=tf[0][:], in_=tf[0][:]), r=[("tf", 0)], w=[("tf", 0)])
                for a in range(2):
                    blk = 2 * h + a
                    TTo(tf[1 + a][:], po[:, a * 256:(a + 1) * 256], tf[0][:], OP.mult, r=[pokey, ("tf", 0)], w=[("tf", 1 + a)])
                    STT(mixT[:, blk, :], tf[1 + a][:], ggc[:, blk:blk + 1], srT[:, blk, :], OP.mult, OP.mult,
                        r=[("tf", 1 + a), "cols", ("srT", blk)], w=[("mixT", blk)])
        if not main:
            return
        CP("act", halo[:], uT[:, :, TT:TT + 30], r=["uT"], w=["halo"])
        for blk in range(16):
            ACT(sqb[0][:, 0:TT], yv[:, blk, :], AF.Copy, r=[("yT", blk)], w=[("sqb", 0)])
            MM(P[4][:, 0:TT], onesB[:], sqb[0][:, 0:TT], blk == 0, blk == 15, r=[("sqb", 0), "onesB"], w=["P4"])
            ACT(sqb[1][:, 0:TT], yv[:, blk, :], AF.Square, r=[("yT", blk)], w=[("sqb", 1)])
            MM(P[5][:, 0:TT], onesB[:], sqb[1][:, 0:TT], blk == 0, blk == 15, r=[("sqb", 1), "onesB"], w=["P5"])
        TS(tf[0][:], P[4][:, 0:TT], 1.0 / 2048, None, OP.mult, None, r=["P4"], w=[("tf", 0)])
        TTo(tf[1][:], tf[0][:], tf[0][:], OP.mult, r=[("tf", 0)], w=[("tf", 1)])
        STT(tf[1][:], P[5][:, 0:TT], 1.0 / 2048, tf[1][:], OP.mult, OP.subtract, r=["P5", ("tf", 1)], w=[("tf", 1)])
        TS(tf[1][:], tf[1][:], EPS, None, OP.add, None, r=[("tf", 1)], w=[("tf", 1)])
        ACT(tf[1][:], tf[1][:], AF.Sqrt, r=[("tf", 1)], w=[("tf", 1)])
        S.op("dve", lambda e: e.reciprocal(out=tf[1][:], in_=tf[1][:]), r=[("tf", 1)], w=[("tf", 1)])
        for blk in range(16):
            TTo(yv[:, blk, :], yv[:, blk, :], tf[0][:], OP.subtract, r=[("yT", blk), ("tf", 0)], w=[("yT", blk)])
            TTo(yv[:, blk, :], yv[:, blk, :], tf[1][:], OP.mult, r=[("yT", blk), ("tf", 1)], w=[("yT", blk)])
            TS(yv[:, blk, :], yv[:, blk, :], lgc[:, blk:blk + 1], lbc[:, blk:blk + 1], OP.mult, OP.add,
               r=[("yT", blk), "cols"], w=[("yT", blk)])
            ACT(mixT[:, 16 + blk, :], yv[:, blk, :], AF.Silu, r=[("yT", blk)], w=[("mixT", 16 + blk)])

    def out_proj(ws):
        for ob in range(32):
            b = ws.get(ob)
            po_, pkey = proj(b, mixT, "mixT")
            TTo(xT[:, ob, :], po_[:, 0:TT], xT[:, ob, :], OP.add, r=[pkey, ("xT", ob // 4)], w=[("xT", ob // 4)])

    def peer(wsq, wsu, vsrc):
        norm_xT(g2c)
        for blk in range(16):
            b = wsq.get(blk)
            pp, pkey = proj(b, xnT, "xnT")
            ACT(pqT[:, blk, :], pp[:, 0:TT], AF.Copy, r=[pkey], w=[("pqT", blk)])
        for g in range(2):
            for quad in range(4):
                for i in range(4):
                    blk = quad * 4 + i
                    MM(P[2][:, i * 128:(i + 1) * 128], pqT[:, blk, g * 128:(g + 1) * 128], keysT[:, blk, :], True, True,
                       r=[("pqT", blk), "keysT"], w=["P2"])
                CP(cp_eng(), sall[:, g, quad * 4:(quad + 1) * 4, :], P[2][:, 0:512].rearrange("p (a n) -> p a n", a=4),
                   r=["P2"], w=[("sall", g)])
        def topk_chain(g, h):
            v1, v2, c24, cand, wk, sm, d16 = v1s[g], v2s[g], c24s[g], cands[g], wks[g], sms[g], d16s[g]
            ta, tb = gt[g], ga[g]
            K = lambda n: (n, g)
            s1 = sall[:, g, 2 * h, :]
            s2 = sall[:, g, 2 * h + 1, :]
            for (s_, v_, vk) in ((s1, v1, K("v1")), (s2, v2, K("v2"))):
                S.op("dve", lambda e, s_=s_, v_=v_: e.max(out=v_[:, 0:8], in_=s_), r=[("sall", g)], w=[vk])
                yield
                S.op("dve", lambda e, s_=s_, v_=v_: e.match_replace(out=wk[:, 0:128], in_to_replace=v_[:, 0:8], in_values=s_,
                                                                    imm_value=-1e30), r=[("sall", g), vk], w=[K("wk")])
                yield
                S.op("dve", lambda e, v_=v_: e.max(out=v_[:, 8:16], in_=wk[:, 0:128]), r=[K("wk")], w=[vk])
                yield
            TTo(cand[:].rearrange("p (i j) -> p i j", i=16), v1[:].unsqueeze(2).to_broadcast([128, 16, 16]),
                v2[:].unsqueeze(1).to_broadcast([128, 16, 16]), OP.add, r=[K("v1"), K("v2")], w=[K("cand")])
            yield
            S.op("dve", lambda e: e.max(out=c24[:, 0:8], in_=cand[:]), r=[K("cand")], w=[K("c24")])
            yield
            S.op("dve", lambda e: e.match_replace(out=wk[:], in_to_replace=c24[:, 0:8], in_values=cand[:], imm_value=-1e30),
                 r=[K("cand"), K("c24")], w=[K("wk")])
            yield
            S.op("dve", lambda e: e.max(out=c24[:, 8:16], in_=wk[:]), r=[K("wk")], w=[K("c24")])
            yield
            S.op("dve", lambda e: e.match_replace(out=cand[:], in_to_replace=c24[:, 8:16], in_values=wk[:], imm_value=-1e30),
                 r=[K("wk"), K("c24")], w=[K("cand")])
            yield
            S.op("dve", lambda e: e.max(out=c24[:, 16:24], in_=cand[:]), r=[K("cand")], w=[K("c24")])
            yield
            TS(sm[:, 0:1], c24[:, 15:16], c24[:, 16:17], 0.5, OP.add, OP.mult, r=[K("c24")], w=[K("sm")])
            yield
            TS(d16[:], c24[:, 0:16], c24[:, 0:1], None, OP.subtract, None, r=[K("c24")], w=[K("d16")])
            yield
            ACT(d16[:], d16[:], AF.Exp, r=[K("d16")], w=[K("d16"), K("smz")], accum_out=sm[:, 1:2])
            yield
            S.op("dve", lambda e: e.reciprocal(out=sm[:, 2:3], in_=sm[:, 1:2]), r=[K("smz"), K("sm")], w=[K("sm")])
            yield
            TS(ta[:], s1, v1[:, 0:1], None, OP.subtract, None, r=[("sall", g), K("v1")], w=[("gt", g)])
            yield
            ACT(ta[:], ta[:], AF.Exp, r=[("gt", g)], w=[("gt", g)])
            yield
            TS(tb[:], s2, v2[:, 0:1], None, OP.subtract, None, r=[("sall", g), K("v2")], w=[("ga", g)])
            yield
            ACT(tb[:], tb[:], AF.Exp, r=[("ga", g)], w=[("ga", g)])
            yield
            STT(ta[:], s1, v1[:, 15:16], ta[:], OP.is_ge, OP.mult, r=[("sall", g), K("v1"), ("gt", g)], w=[("gt", g)])
            yield
            TS(E1[:, g, h, :], ta[:], sm[:, 2:3], None, OP.mult, None, r=[("gt", g), K("sm")], w=[("E1", g)])
            yield
            STT(E2[:, g, h, :], s2, v2[:, 15:16], tb[:], OP.is_ge, OP.mult, r=[("sall", g), K("v2"), ("ga", g)], w=[("E2", g)])
            yield
            TS(TH[:, g, h, :], s1, -1.0, sm[:, 0:1], OP.mult, OP.add, r=[("sall", g), K("sm")], w=[("TH", g)])
            yield

        for h in range(8):
            chains = [topk_chain(0, h), topk_chain(1, h)]
            while chains:
                for c in list(chains):
                    try:
                        next(c)
                    except StopIteration:
                        chains.remove(c)

        vctr = [0]
        wring = [0]

        def a_mms(n1):
            b = wsu.get(n1)
            bank = n1 % 2
            return [lambda kc=kc, b=b, bank=bank: MM(P[bank][:, 0:TT], wt[b][:, kc, :], xnT[:, kc, :], kc == 0, kc == KC - 1,
                                                     r=[("wt", b), "xnT"], w=["P%d" % bank]) for kc in range(KC)]

        def stage_G(n1):
            pe_ops = []
            sbuf_, skey = GSLOT[n1 % 2]
            for g in range(2):
                for h in range(8):
                    i = wring[0]
                    wring[0] += 1
                    gb, wb = gtr[i % 4], Wb[i % 16]
                    STT(gb[:], sall[:, g, 2 * h + 1, :], TH[:, g, h, n1:n1 + 1], E2[:, g, h, :], OP.is_ge, OP.mult,
                        r=[("sall", g), ("TH", g), ("E2", g)], w=[("gtr", i % 4)])
                    ACT(wb[:], gb[:], AF.Copy, r=[("gtr", i % 4), ("E1", g)], w=[("Wb", i % 16)], scale=E1[:, g, h, n1:n1 + 1])
                    pe_ops.append(lambda g=g, h=h, wb=wb, i=i, sbuf_=sbuf_, skey=skey: MM(
                        sbuf_[:, g * 128:(g + 1) * 128], wb[:], identB[:], h == 0, h == 7,
                        r=[("Wb", i % 16), "identB"], w=[skey]))
            return pe_ops

        def stage_T(n1):
            eg, eb = n1 // 8, n1 % 8
            hb = eg % 2
            sbuf_, skey = GSLOT[n1 % 2]
            TTo(HT[hb][:, eb, :], gl[n1 % 2][:], sbuf_[:, 0:256], OP.mult, r=[("gl", n1 % 2), skey], w=[("HT", hb)])

        def v_chunk(eg, ds):
            hb = eg % 2
            vb = vctr[0] % 2
            vctr[0] += 1
            DMA("sp", vt[vb][:].rearrange("p e d -> p (e d)"), vrb[eg * 8 + ds], r=[("vrb", (eg * 8 + ds) // 16)],
                w=[("vt", vb)])
            for dq in range(4):
                db = ds * 4 + dq
                for e8 in range(8):
                    MM(P[4 + dq][:, 0:TT], vt[vb][:, e8, dq * 128:(dq + 1) * 128], HT[hb][:, e8, :], e8 == 0, e8 == 7,
                       r=[("vt", vb), ("HT", hb)], w=["P%d" % (4 + dq)])
                TTo(xT[:, db, :], P[4 + dq][:, 0:TT], xT[:, db, :], OP.add, r=["P%d" % (4 + dq), ("xT", db // 4)], w=[("xT", db // 4)])

        pend = []
        for n1 in range(129):
            if n1 < 128:
                k = 0
                for j, f in enumerate(a_mms(n1)):
                    f()
                    if j % 2 == 1 and k < len(pend):
                        pend[k]()
                        k += 1
                while k < len(pend):
                    pend[k]()
                    k += 1
                ACT(gl[n1 % 2][:], P[n1 % 2][:, 0:TT], AF.Gelu, r=["P%d" % (n1 % 2)], w=[("gl", n1 % 2)])
                new_pend = stage_G(n1)
            else:
                for f in pend:
                    f()
                new_pend = []
            if n1 >= 1:
                m = n1 - 1
                stage_T(m)
                if m >= 8:
                    v_chunk(m // 8 - 1, m % 8)
            pend = new_pend
        for ds in range(8):
            v_chunk(15, ds)

    def final_out(dst):
        fm_rstd(D, KC, lambda kc: xT[:, kc, :], lambda kc: ("xT", kc // 4))
        for kc in range(KC):
            STT(xT[:, kc, :], xT[:, kc, :], gfc[:, kc:kc + 1], rstd[:], OP.mult, OP.mult,
                r=[("xT", kc // 4), "cols", "rstd"], w=[("xT", kc // 4)])
        for g in range(2):
            for kq in range(8):
                for i in range(4):
                    kc = kq * 4 + i
                    TR(P[2][:, i * 128:(i + 1) * 128], xT[:, kc, g * 128:(g + 1) * 128], identF[:],
                       r=[("xT", kq), "identF"], w=["P2"])
                CP(cp_eng(), xin[:, kq * 512:(kq + 1) * 512], P[2][:, 0:512], r=["P2"], w=["xin"])
            DMA("sp", dst[g * 128:(g + 1) * 128, :], xin[:], r=["xin"], w=["y"])

    def win_srcs(mode):
        idx = []
        if mode != "pre":
            idx += [48 + i for i in range(32)]
        for h in range(8):
            idx += [h * 6 + i for i in (range(6) if mode == "main" else range(3))]
        return [(winb[i], ("winb", i // 16)) for i in idx]

    for t in range(n_pre):
        mode = "pre_last" if t == n_pre - 1 else "pre"
        token_mix(xp[t * TT:(t + 1) * TT, :], mode, WStream(win_srcs(mode)))
        if t == n_pre - 1:
            S.barrier()
    for t in range(n_main):
        token_mix(xm[t * TT:(t + 1) * TT, :], "main", WStream(win_srcs("main")))
        out_proj(WStream([(woutb[i], ("woutb", i // 16)) for i in range(32)]))
        S.barrier()
        peer(WStream([(wqb[i], ("wqb", i // 16)) for i in range(16)]),
             WStream([(utb[i], ("utb", i // 16)) for i in range(128)]), vrd)
        final_out(y[t * TT:(t + 1) * TT, :])
        S.barrier()
    S.finish()
    S.run()
    return nc


def _blk(wm):
    k, n = wm.shape
    return np.ascontiguousarray(wm.reshape(k // 128, 128, n // 128, 128).transpose(2, 1, 0, 3)).reshape(n // 128, 128, k)


def _consts():
    c = np.zeros((128, 258), np.float32)
    c[:, 0:128] = np.eye(128, dtype=np.float32)
    t = np.arange(128)
    c[:, 128:256] = np.where((t[:, None] > t[None, :]) & ((t[:, None] // 64) == (t[None, :] // 64)), -1.0 / 16.0, 0.0)
    c[:, 256] = np.where(t < 64, -1.0 / 16.0, 0.0)
    c[:, 257] = np.where(t >= 64, -1.0 / 16.0, 0.0)
    return c


def prep_weights(w_in, w_gate_up, b_gate, gla_norm_g, w_dw, b_dw, conv_ln_g, conv_ln_b, w_out, norm1_g, norm2_g,
                 peer_wq, peer_keys1, peer_keys2, peer_u, peer_v, final_norm_g):
    f = lambda a: np.asarray(a, dtype=np.float32)
    wi = f(w_in)[0]
    q, k, v, r = wi[:, 0:1024], wi[:, 1024:2048], wi[:, 2048:4096], wi[:, 4096:6144]
    gd, ca, cb = wi[:, 6144:6160], wi[:, 6160:8208], wi[:, 8208:10256]
    qb, kb, vb, rb, cab, cbb = _blk(q), _blk(k), _blk(v), _blk(r), _blk(ca), _blk(cb)
    blocks = []
    for h in range(8):
        blocks += [kb[h], vb[2 * h], vb[2 * h + 1], qb[h], rb[2 * h], rb[2 * h + 1]]
    for b in range(16):
        blocks += [cbb[b], cab[b]]
    win = np.stack(blocks)
    wgd = np.ascontiguousarray(gd.reshape(32, 128, 16).transpose(1, 0, 2)).reshape(128, 512)
    wout = _blk(f(w_out)[0])
    wq = _blk(f(peer_wq)[0])
    ut = np.ascontiguousarray(f(peer_u)[0].reshape(128, 128, 32, 128).transpose(0, 3, 2, 1)).reshape(128, 128, 4096)
    vr = np.ascontiguousarray(f(peer_v)[0].reshape(16, 8, 128, 8, 512).transpose(0, 3, 2, 1, 4)).reshape(128, 128, 4096)
    k1, k2 = f(peer_keys1)[0], f(peer_keys2)[0]
    keyst = np.zeros((128, 16, 128), np.float32)
    for h in range(8):
        keyst[:, 2 * h, :] = k1[h].T
        keyst[:, 2 * h + 1, :] = k2[h].T
    keyst = keyst.reshape(128, 2048)
    wga = np.concatenate([f(w_gate_up)[0], f(b_gate)[0][None, :]], axis=0)
    colv = lambda a, n: np.ascontiguousarray(f(a).reshape(n, 128).T)
    cols = np.concatenate([
        colv(norm1_g[0], 32), colv(norm2_g[0], 32), colv(final_norm_g, 32),
        colv(gla_norm_g[0], 16), colv(conv_ln_g[0], 16), colv(conv_ln_b[0], 16), colv(b_dw[0], 16),
        np.ascontiguousarray(f(w_dw)[0].reshape(31, 16, 128).transpose(2, 1, 0)).reshape(128, 496),
    ], axis=1)
    assert cols.shape == (128, NCOLS)
    return dict(win=win, wgd=wgd, wout=wout, wq=wq, ut=ut, vr=vr, keyst=keyst, wga=wga,
                cols=np.ascontiguousarray(cols), consts=_consts())


def make_core_inputs(xb, meta, start, n_main, n_pre):
    xm = np.ascontiguousarray(xb[start:start + n_main * TT])
    xp = np.zeros((n_pre * TT, D), np.float32)
    pre = np.concatenate([meta, xb[:start]], axis=0)
    assert pre.shape[0] <= n_pre * TT
    xp[n_pre * TT - pre.shape[0]:] = pre
    return xm, xp


_NC_CACHE = {}


def kernel(x, meta_tokens, norm1_g, w_in, w_gate_up, b_gate, gla_norm_g, w_dw, b_dw, conv_ln_g, conv_ln_b,
           w_out, norm2_g, peer_wq, peer_keys1, peer_keys2, peer_u, peer_v, final_norm_g):
    x = np.asarray(x, dtype=np.float32)
    meta = np.asarray(meta_tokens, dtype=np.float32)
    wts = prep_weights(w_in, w_gate_up, b_gate, gla_norm_g, w_dw, b_dw, conv_ln_g, conv_ln_b, w_out, norm1_g,
                       norm2_g, peer_wq, peer_keys1, peer_keys2, peer_u, peer_v, final_norm_g)
    B, L, _ = x.shape
    per = N_MAIN_TILES * TT
    in_maps = []
    for c in range(8):
        b, j = c // 4, c % 4
        xm, xp = make_core_inputs(x[b], meta, j * per, N_MAIN_TILES, N_PRE_TILES)
        m = dict(wts)
        m["xm"] = xm
        m["xp"] = xp
        in_maps.append(m)
    if "nc" not in _NC_CACHE:
        _NC_CACHE["nc"] = build_nc()
    res = run_bass_kernel_spmd(_NC_CACHE["nc"], in_maps, core_ids=list(range(8)))
    out = np.empty((B, L, D), np.float32)
    for c in range(8):
        b, j = c // 4, c % 4
        out[b, j * per:(j + 1) * per] = res.results[c]["y"]
    return out
```

```python
import contextlib
import numpy as np
import concourse.bass as bass
import concourse.mybir as mybir
from concourse.bass_utils import run_bass_kernel_spmd

F32 = mybir.dt.float32
BF16 = mybir.dt.bfloat16
AF = mybir.ActivationFunctionType
OP = mybir.AluOpType

D = 4096
KC = 32
TT = 256
EPS = 1e-6
N_MAIN_TILES = 8
N_PRE_TILES = 26
NCOLS = 32 * 3 + 16 * 4 + 16 * 31


class Sched:
    ENGS = ("pe", "act", "dve", "pool", "sp")

    def __init__(self, nc, same_engine_sync=True):
        self.nc = nc
        self.ops = []
        self.state = {}
        self.same_engine_sync = same_engine_sync
        self.n_dma_sems = {"pool": 28, "sp": 24, "act": 2}
        self.unbarriered_dma = []
        self.nosync = ("pe",)
        self.persist = set()
        self.excl = set("P%d" % i for i in range(8))

    def _entries(self, key):
        base, idx = key if isinstance(key, tuple) else (key, None)
        st = self.state.setdefault(base, {"*": [None, []]})
        if idx is None:
            return list(st.values())
        if idx not in st:
            st[idx] = [st["*"][0], list(st["*"][1])]
        return [st[idx]]

    def _add(self, eng, fn, r, w, dma):
        xr = tuple(k for k in r if (k[0] if isinstance(k, tuple) else k) in self.excl)
        if xr:
            r = tuple(k for k in r if k not in xr)
            w = tuple(w) + xr
        i = len(self.ops)
        deps = set()
        for key in r:
            for ent in self._entries(key):
                if ent[0] is not None:
                    deps.add(ent[0])
        for key in w:
            for ent in self._entries(key):
                if ent[0] is not None:
                    deps.add(ent[0])
                deps.update(ent[1])
        for key in r:
            for ent in self._entries(key):
                if not dma:
                    ent[1][:] = [j for j in ent[1] if self.ops[j]["dma"] or self.ops[j]["eng"] != eng]
                ent[1].append(i)
        for key in w:
            for ent in self._entries(key):
                ent[0] = i
                ent[1] = []
        deps.discard(i)
        pers = dma and any((k[0] if isinstance(k, tuple) else k) in self.persist for k in w)
        self.ops.append(dict(eng=eng, fn=fn, deps=deps, dma=dma, needs_inc=False, pers=pers))
        if dma and not pers:
            self.unbarriered_dma.append(i)
        return i

    def op(self, eng, fn, r=(), w=(), big=False):
        i = self._add(eng, fn, tuple(r), tuple(w), False)
        self.ops[i]["big"] = big
        return i

    def dma(self, eng, fn, r=(), w=()):
        return self._add(eng, fn, tuple(r), tuple(w), True)

    def barrier(self):
        last = {}
        for i, o in enumerate(self.ops):
            if o.get("barrier") or o["dma"]:
                continue
            last[o["eng"]] = i
        deps = set(last.values()) | set(self.unbarriered_dma)
        self.unbarriered_dma = []
        self.state = {k: v for k, v in self.state.items() if k in self.persist}
        self.ops.append(dict(eng=None, fn=None, deps=deps, dma=False, needs_inc=False, barrier=True))

    def finish(self):
        self.barrier()
        self.ops.append(dict(eng="sp", fn=None, deps=set(), dma=False, needs_inc=False))

    def plan(self):
        ops = self.ops
        pending = {e: set() for e in self.ENGS}
        for o in ops:
            if o.get("barrier"):
                for e in self.ENGS:
                    pending[e] |= o["deps"]
            else:
                e = o["eng"]
                if pending[e]:
                    o["deps"] = set(o["deps"]) | pending[e]
                    pending[e] = set()
        real = [(i, o) for i, o in enumerate(ops) if not o.get("barrier")]
        dma_ctr = {e: 0 for e in self.ENGS}
        dma_prev = {}
        for i, o in real:
            if o["dma"]:
                e = o["eng"]
                if o.get("pers"):
                    e = "cv"
                    slot = dma_ctr.get("cv", 0)
                    dma_ctr["cv"] = slot + 1
                else:
                    slot = dma_ctr[e] % self.n_dma_sems[e]
                    dma_ctr[e] += 1
                o["dslot"] = (e, slot)
                prev = dma_prev.get((e, slot))
                o["dprev"] = prev
                o["dtarget"] = (ops[prev]["dtarget"] + 16) if prev is not None else 16
                dma_prev[(e, slot)] = i

        def skip_same(p, e):
            return p["eng"] == e and (e in self.nosync or (e == "dve" and p.get("big")) or not self.same_engine_sync)

        for i, o in real:
            for d in o["deps"]:
                p = ops[d]
                if p["dma"] or skip_same(p, o["eng"]):
                    continue
                p["needs_inc"] = True
        cnt = {e: 0 for e in self.ENGS}
        for i, o in real:
            if (not o["dma"]) and o["needs_inc"]:
                cnt[o["eng"]] += 1
                o["count"] = cnt[o["eng"]]
        seen = {e: {f: 0 for f in self.ENGS} for e in self.ENGS}
        seen_dma = {e: {} for e in self.ENGS}
        for i, o in real:
            e = o["eng"]
            best = {}
            cands = []
            if o["dma"] and o["dprev"] is not None:
                cands.append(o["dprev"])
            cands.extend(o["deps"])
            for d in cands:
                p = ops[d]
                if p["dma"]:
                    key = p["dslot"]
                    if seen_dma[e].get(key, 0) < p["dtarget"]:
                        seen_dma[e][key] = p["dtarget"]
                        best[("dma", key)] = max(best.get(("dma", key), 0), p["dtarget"])
                else:
                    if skip_same(p, e):
                        continue
                    if seen[e][p["eng"]] < p["count"]:
                        seen[e][p["eng"]] = p["count"]
                        best[("eng", p["eng"])] = max(best.get(("eng", p["eng"]), 0), p["count"])
            o["waits"] = [(k[0], k[1], v) for k, v in best.items()]
        self.cnt = cnt
        return real

    def run(self):
        nc = self.nc
        real = self.plan()
        with contextlib.ExitStack() as es:
            esem = {e: es.enter_context(nc.semaphore("s_" + e)) for e in self.ENGS}
            dsem = {}
            for key in sorted(set(o["dslot"] for i, o in real if o["dma"])):
                dsem[key] = es.enter_context(nc.semaphore("d_%s_%d" % key))
            block = es.enter_context(nc.Block())
            per_eng = {e: [] for e in self.ENGS}
            for i, o in real:
                per_eng[o["eng"]].append(o)

            def emit_engine(ename, eng):
                for o in per_eng[ename]:
                    for kind, key, val in o["waits"]:
                        eng.wait_ge(dsem[key] if kind == "dma" else esem[key], val)
                    if o["fn"] is None:
                        continue
                    ins = o["fn"](eng)
                    if o["dma"]:
                        ins.then_inc(dsem[o["dslot"]], 16)
                    elif o["needs_inc"]:
                        ins.then_inc(esem[ename], 1)

            @block.tensor
            def _(eng):
                emit_engine("pe", eng)

            @block.scalar
            def _(eng):
                emit_engine("act", eng)

            @block.vector
            def _(eng):
                emit_engine("dve", eng)

            @block.gpsimd
            def _(eng):
                emit_engine("pool", eng)

            @block.sync
            def _(eng):
                emit_engine("sp", eng)


def build_nc(n_main=N_MAIN_TILES, n_pre=N_PRE_TILES):
    nc = bass.Bass("TRN2", target_bir_lowering=False)
    S = Sched(nc)

    def din(name, shape):
        return nc.dram_tensor(name, shape, F32, kind="ExternalInput").ap()

    xm = din("xm", [n_main * TT, D])
    xp = din("xp", [n_pre * TT, D])
    win = din("win", [80, 128, D])
    wgd = din("wgd", [128, KC * 16])
    wout = din("wout", [32, 128, D])
    wqd = din("wq", [16, 128, D])
    utd = din("ut", [128, 128, D])
    vrd = din("vr", [128, 128, D])
    keyd = din("keyst", [128, 16 * 128])
    wgad = din("wga", [17, 1024])
    colsd = din("cols", [128, NCOLS])
    constd = din("consts", [128, 258])
    y = nc.dram_tensor("y", [n_main * TT, D], F32, kind="ExternalOutput").ap()
    winb = nc.dram_tensor("winb", [80, 128, D], BF16).ap()
    woutb = nc.dram_tensor("woutb", [32, 128, D], BF16).ap()
    wqb = nc.dram_tensor("wqb", [16, 128, D], BF16).ap()
    utb = nc.dram_tensor("utb", [128, 128, D], BF16).ap()
    vrb = nc.dram_tensor("vrb", [128, 128, D], BF16).ap()
    S.persist.update(["winb", "woutb", "wqb", "utb", "vrb"])

    off = [16512]

    def sb(name, shape, dt, at=None):
        nb = int(np.prod(shape[1:])) * (4 if dt == F32 else 2)
        nb = (nb + 63) // 64 * 64
        if at is None:
            t = nc.alloc_sbuf_tensor_at(name, shape, dt, offset=off[0])
            off[0] += nb
        else:
            t = nc.alloc_sbuf_tensor_at(name, shape, dt, offset=at[0])
            at[0] += nb
        return t

    identF = sb("identF", [128, 128], F32)
    triN = sb("triN", [128, 128], F32)
    chunkN = sb("chunkN", [128, 2], F32)
    identB = sb("identB", [128, 128], BF16)
    onesB = sb("onesB", [128, 128], BF16)
    cols = sb("cols", [128, NCOLS], F32)
    keysT = sb("keysT", [128, 16, 128], BF16)
    wgdT = sb("wgdT", [128, KC, 16], BF16)
    wgA = sb("wgA", [32, 1024], F32)
    gdA = sb("gdA", [32, TT], F32)
    Sst = sb("Sst", [128, 8, 256], F32)
    Sbf = sb("Sbf", [128, 8, 256], BF16)
    halo = sb("halo", [128, 16, 30], F32)
    xT = sb("xT", [128, KC, TT], F32)
    xnT = sb("xnT", [128, KC, TT], BF16)
    wt = [sb("wt%d" % i, [128, KC, 128], BF16) for i in range(3)]
    xin = sb("xin", [128, D], F32)
    rstd = sb("rstd", [128, TT], F32)
    sqb = [sb("sqb%d" % i, [128, 2 * TT], BF16) for i in range(2)]
    region = off[0]
    a1 = [region]
    mixT = sb("mixT", [128, KC, TT], BF16, a1)
    nsp = sb("nsp", [128, 2, 1024], F32, a1)
    erevT = sb("erevT", [128, 8, TT], F32, a1)
    dec = sb("dec", [128, 32], F32, a1)
    qT = sb("qT", [128, 8, TT], BF16, a1)
    kdT = [sb("kdT%d" % i, [128, TT], BF16, a1) for i in range(2)]
    kd = sb("kd", [128, 2, 8, 128], BF16, a1)
    vtok = sb("vtok", [128, 2, 2048], BF16, a1)
    srT = sb("srT", [128, 16, TT], BF16, a1)
    uT = sb("uT", [128, 16, TT + 30], F32, a1)
    sig = [sb("sig%d" % i, [128, TT], F32, a1) for i in range(2)]
    tf = [sb("tf%d" % i, [128, TT], F32, a1) for i in range(3)]
    end1 = a1[0]
    a2 = [region]
    pqT = sb("pqT", [128, 16, TT], BF16, a2)
    sall = sb("sall", [128, 2, 16, 128], F32, a2)
    E2 = sb("E2", [128, 2, 8, 128], F32, a2)
    TH = sb("TH", [128, 2, 8, 128], F32, a2)
    E1 = sb("E1", [128, 2, 8, 128], F32, a2)
    vt = [sb("vt%d" % i, [128, 8, 512], BF16, a2) for i in range(2)]
    HT = [sb("HT%d" % i, [128, 8, TT], BF16, a2) for i in range(2)]
    gl = [sb("gl%d" % i, [128, TT], F32, a2) for i in range(2)]
    Wb = [sb("Wb%d" % i, [128, 128], BF16, a2) for i in range(16)]
    gtr = [sb("gtr%d" % i, [128, 128], F32, a2) for i in range(4)]
    gt = [sb("gt%d" % i, [128, 128], F32, a2) for i in range(2)]
    ga = [sb("ga%d" % i, [128, 128], F32, a2) for i in range(2)]
    v1s = [sb("v1_%d" % i, [128, 16], F32, a2) for i in range(2)]
    v2s = [sb("v2_%d" % i, [128, 16], F32, a2) for i in range(2)]
    c24s = [sb("c24_%d" % i, [128, 24], F32, a2) for i in range(2)]
    cands = [sb("cand%d" % i, [128, 256], F32, a2) for i in range(2)]
    wks = [sb("wk%d" % i, [128, 256], F32, a2) for i in range(2)]
    sms = [sb("sm%d" % i, [128, 16], F32, a2) for i in range(2)]
    d16s = [sb("d16_%d" % i, [128, 16], F32, a2) for i in range(2)]

    end2 = a2[0]
    assert max(end1, end2) <= 229344, (end1, end2)
    yT = xin

    P = [nc.alloc_psum_tensor("P%d" % i, [128, 512], F32) for i in range(3)]
    P3 = nc.alloc_psum_tensor("P3", [128, 1024], BF16)
    P += [None] + [nc.alloc_psum_tensor("P%d" % i, [128, 512], F32) for i in range(4, 8)]
    P2b = P[2][:].bitcast(BF16)
    TSLOT = [(P3, "P3"), (P2b, "P2")]
    GSLOT = [(P3[:].bitcast(F32), "P3"), (P[2], "P2")]

    g1c = cols[:, 0:32]
    g2c = cols[:, 32:64]
    gfc = cols[:, 64:96]
    ggc = cols[:, 96:112]
    lgc = cols[:, 112:128]
    lbc = cols[:, 128:144]
    bdc = cols[:, 144:160]
    wdwc = cols[:, 160:160 + 496]

    def MM(out, lhsT, rhs, start, stop, r, w):
        S.op("pe", lambda e: e.matmul(out, lhsT=lhsT, rhs=rhs, start=start, stop=stop), r=r, w=w)

    def TR(out, in_, ident, r, w):
        S.op("pe", lambda e: e.transpose(out=out, in_=in_, identity=ident), r=r, w=w)

    def ACT(out, in_, func, r, w, **kw):
        S.op("act", lambda e: e.activation(out=out, in_=in_, func=func, **kw), r=r, w=w)

    def isbig(ap):
        return int(np.prod(ap.shape[1:])) >= 128

    def TS(out, in0, s1, s2, op0, op1, r, w, eng="dve"):
        if s2 is None:
            S.op(eng, lambda e: e.tensor_scalar(out=out, in0=in0, scalar1=s1, scalar2=None, op0=op0), r=r, w=w, big=isbig(out))
        else:
            S.op(eng, lambda e: e.tensor_scalar(out=out, in0=in0, scalar1=s1, scalar2=s2, op0=op0, op1=op1), r=r, w=w, big=isbig(out))

    def TTo(out, in0, in1, op, r, w, eng="dve"):
        S.op(eng, lambda e: e.tensor_tensor(out=out, in0=in0, in1=in1, op=op), r=r, w=w, big=isbig(out))

    def STT(out, in0, scalar, in1, op0, op1, r, w):
        S.op("dve", lambda e: e.scalar_tensor_tensor(out=out, in0=in0, scalar=scalar, in1=in1, op0=op0, op1=op1), r=r, w=w,
             big=isbig(out))

    def CP(eng, out, in_, r, w):
        if eng == "act":
            S.op("act", lambda e: e.activation(out=out, in_=in_, func=AF.Copy), r=r, w=w)
        else:
            S.op(eng, lambda e: e.tensor_copy(out=out, in_=in_), r=r, w=w)

    def DMA(eng, out, in_, r, w):
        S.dma(eng, lambda e: e.dma_start(out=out, in_=in_), r=r, w=w)

    cp_rr = [0]

    def cp_eng():
        cp_rr[0] += 1
        return "act" if cp_rr[0] % 2 else "dve"

    wctr = [0]

    class WStream:
        def __init__(self, srcs, depth=2):
            self.srcs = srcs
            self.issued = 0
            self.base = wctr[0]
            self.depth = depth
            wctr[0] += len(srcs)

        def get(self, i):
            while self.issued < min(len(self.srcs), i + 1 + self.depth):
                b = (self.base + self.issued) % 3
                src, skey = self.srcs[self.issued]
                DMA("sp", wt[b][:].rearrange("p k c -> p (k c)"), src, r=[skey], w=[("wt", b)])
                self.issued += 1
            return (self.base + i) % 3

    pj = [0]

    def proj(b, rhsT, rkey, ncols=128):
        bank = pj[0] % 2
        pj[0] += 1
        for kc in range(KC):
            MM(P[bank][0:ncols, 0:TT], wt[b][:, kc, 0:ncols], rhsT[:, kc, :], kc == 0, kc == KC - 1,
               r=[("wt", b), rkey], w=["P%d" % bank])
        return P[bank], "P%d" % bank

    def fm_rstd(dim, nblk, src_of, src_key_of, pbank=4):
        for kc in range(nblk):
            sq = sqb[kc % 2]
            ACT(sq[:, 0:TT], src_of(kc), AF.Square, r=[src_key_of(kc)], w=[("sqb", kc % 2)])
            MM(P[pbank][:, 0:TT], onesB[:], sq[:, 0:TT], kc == 0, kc == nblk - 1,
               r=[("sqb", kc % 2), "onesB"], w=["P%d" % pbank])
        TS(rstd[:], P[pbank][:, 0:TT], 1.0 / dim, EPS, OP.mult, OP.add, r=["P%d" % pbank], w=["rstd"])
        ACT(rstd[:], rstd[:], AF.Sqrt, r=["rstd"], w=["rstd"])
        S.op("dve", lambda e: e.reciprocal(out=rstd[:], in_=rstd[:]), r=["rstd"], w=["rstd"])

    DMA("sp", identF[:], constd[:, 0:128], r=[], w=["identF"])
    DMA("sp", triN[:], constd[:, 128:256], r=[], w=["triN"])
    DMA("sp", chunkN[:], constd[:, 256:258], r=[], w=["chunkN"])
    DMA("sp", cols[:], colsd, r=[], w=["cols"])
    DMA("sp", wgA[0:17, :], wgad, r=[], w=["wgA"])
    DMA("pool", keysT[:].rearrange("p (a b) n -> p a (b n)", a=2), keyd.rearrange("p (a b) -> p a b", a=2), r=[], w=["keysT"])
    DMA("pool", wgdT[:].rearrange("p k c -> p (k c)"), wgd, r=[], w=["wgdT"])
    CP("act", identB[:], identF[:], r=["identF"], w=["identB"])
    S.op("dve", lambda e: e.memset(onesB[:], 1.0), w=["onesB"])
    S.op("dve", lambda e: e.memset(gdA[:], 1.0), w=["gdA"])
    S.op("dve", lambda e: e.memset(Sst[:].rearrange("p h v -> p (h v)"), 0.0), w=["Sst"])
    S.op("dve", lambda e: e.memset(halo[:].rearrange("p b j -> p (b j)"), 0.0), w=["halo"])

    def convert_ops(src3, dst3, nblk, key):
        ops = []
        for c0 in range(0, nblk, 16):
            n = min(16, nblk - c0)
            ops.append(lambda rr, c0=c0, n=n: DMA("pool", dst3[c0:c0 + n].rearrange("b p (a e) -> (b p) a e", a=2),
                                                  src3[c0:c0 + n].rearrange("b p (a e) -> (b p) a e", a=2), r=rr, w=[(key, c0 // 16)]))
        return ops

    for f in convert_ops(win, winb, 80, "winb"):
        f([])
    late_conv = (convert_ops(wout, woutb, 32, "woutb") + convert_ops(wqd, wqb, 16, "wqb") +
                 convert_ops(utd, utb, 128, "utb") + convert_ops(vrd, vrb, 128, "vrb"))

    def load_norm(src, gcol):
        for g in range(2):
            DMA("sp", xin[:], src[g * 128:(g + 1) * 128, :], r=[], w=["xin"])
            for kq in range(8):
                for i in range(4):
                    kc = kq * 4 + i
                    TR(P[2][:, i * 128:(i + 1) * 128], xin[:, kc * 128:(kc + 1) * 128], identF[:],
                       r=["xin", "identF"], w=["P2"])
                CP(cp_eng(), xT[:, kq * 4:(kq + 1) * 4, g * 128:(g + 1) * 128],
                   P[2][:, 0:512].rearrange("p (a t) -> p a t", a=4), r=["P2"], w=[("xT", kq)])
        norm_xT(gcol)

    def norm_xT(gcol):
        fm_rstd(D, KC, lambda kc: xT[:, kc, :], lambda kc: ("xT", kc // 4))
        for kc in range(KC):
            STT(xnT[:, kc, :], xT[:, kc, :], gcol[:, kc:kc + 1], rstd[:], OP.mult, OP.mult,
                r=[("xT", kc // 4), "cols", "rstd"], w=[("xnT", kc)])

    def gla_gate():
        for kc in range(KC):
            MM(P[0][0:16, 0:TT], wgdT[:, kc, :], xnT[:, kc, :], kc == 0, kc == KC - 1,
               r=["wgdT", ("xnT", kc)], w=["P0"])
        CP("act", gdA[0:16, :], P[0][0:16, 0:TT], r=["P0"], w=["gdA"])
        for g in range(2):
            for hf in range(2):
                MM(P[4 + hf][:, 0:512], gdA[0:17, g * 128:(g + 1) * 128], wgA[0:17, hf * 512:(hf + 1) * 512],
                   True, True, r=["gdA", "wgA"], w=["P%d" % (4 + hf)])
                ACT(nsp[:, g, hf * 512:(hf + 1) * 512], P[4 + hf][:, 0:512], AF.Exp, r=["P%d" % (4 + hf)],
                    w=[("nsp", g)], scale=-1.0)
            TS(nsp[:, g, :], nsp[:, g, :], 1.0, None, OP.add, None, r=[("nsp", g)], w=[("nsp", g)])
            ACT(nsp[:, g, :], nsp[:, g, :], AF.Ln, r=[("nsp", g)], w=[("nsp", g)])
        for h in range(8):
            pb = 4 + (h % 2)
            for g in range(2):
                MM(P[pb][:, g * 128:(g + 1) * 128], nsp[:, g, h * 128:(h + 1) * 128], triN[:], True, True,
                   r=[("nsp", g), "triN"], w=["P%d" % pb])
            ACT(erevT[:, h, :], P[pb][:, 0:TT], AF.Exp, r=["P%d" % pb], w=[("erevT", h)])
            for g in range(2):
                MM(P[6][:, h * 4 + g * 2:h * 4 + g * 2 + 2], nsp[:, g, h * 128:(h + 1) * 128], chunkN[:], True, True,
                   r=[("nsp", g), "chunkN"], w=["P6"])
        ACT(dec[:], P[6][:, 0:32], AF.Exp, r=["P6"], w=["dec"])

    def token_mix(src, mode, ws):
        main = mode == "main"
        load_norm(src, g1c)
        wi = [0]

        def nextw():
            b = ws.get(wi[0])
            wi[0] += 1
            return b

        yv = yT[:].rearrange("p (b t) -> p b t", b=16)

        def conv_taps(blk):
            TS(yv[:, blk, :], uT[:, blk, 0:TT], wdwc[:, blk * 31:blk * 31 + 1], bdc[:, blk:blk + 1], OP.mult, OP.add,
               r=[("uT", blk), "uTh", "cols"], w=[("yT", blk)])
            for j in range(1, 31):
                STT(yv[:, blk, :], uT[:, blk, j:j + TT], wdwc[:, blk * 31 + j:blk * 31 + j + 1], yv[:, blk, :], OP.mult, OP.add,
                    r=[("uT", blk), "uTh", "cols", ("yT", blk)], w=[("yT", blk)])

        if mode != "pre":
            if main:
                CP("act", uT[:, :, 0:30], halo[:], r=["halo"], w=["uTh"])
            for blk in range(16):
                b = nextw()
                pcb, pkey = proj(b, xnT, "xnT")
                ACT(sig[blk % 2][:], pcb[:, 0:TT], AF.Sigmoid, r=[pkey], w=[("sig", blk % 2)])
                b = nextw()
                pca, pkey = proj(b, xnT, "xnT")
                TTo(uT[:, blk, 30:30 + TT], pca[:, 0:TT], sig[blk % 2][:], OP.mult, r=[pkey, ("sig", blk % 2)], w=[("uT", blk)])
            if not main:
                CP("act", halo[:], uT[:, :, TT:TT + 30], r=["uT"], w=["halo"])
        gla_gate()
        for h in range(8):
            b = nextw()
            pk, pkey = proj(b, xnT, "xnT")
            kt = kdT[h % 2]
            TTo(kt[:], pk[:, 0:TT], erevT[:, h, :], OP.mult, r=[pkey, ("erevT", h)], w=[("kdT", h % 2)])
            for g in range(2):
                TR(P3[:, g * 128:(g + 1) * 128], kt[:, g * 128:(g + 1) * 128], identB[:],
                   r=[("kdT", h % 2), "identB"], w=["P3"])
            CP("act", kd[:, :, h, :], P3[:, 0:256].rearrange("p (g n) -> p g n", g=2), r=["P3"], w=[("kd", h)])
            if main:
                conv_taps(2 * h)
            for a in range(2):
                b = nextw()
                pv, pkey = proj(b, xnT, "xnT")
                vtmp = sqb[a]
                CP("act", vtmp[:, 0:TT], pv[:, 0:TT], r=[pkey], w=[("sqb", a)])
                for g in range(2):
                    TR(P2b[:, g * 128:(g + 1) * 128], vtmp[:, g * 128:(g + 1) * 128], identB[:],
                       r=[("sqb", a), "identB"], w=["P2"])
                CP("act", vtok[:, :, (2 * h + a) * 128:(2 * h + a + 1) * 128],
                   P2b[:, 0:256].rearrange("p (g n) -> p g n", g=2), r=["P2"], w=[("vtok", h)])
            if main:
                conv_taps(2 * h + 1)
                b = nextw()
                pq_, pkey = proj(b, xnT, "xnT")
                ACT(qT[:, h, :], pq_[:, 0:TT], AF.Copy, r=[pkey], w=[("qT", h)], scale=128.0 ** -0.5)
                for a in range(2):
                    b = nextw()
                    pr, pkey = proj(b, xnT, "xnT")
                    ACT(srT[:, 2 * h + a, :], pr[:, 0:TT], AF.Silu, r=[pkey], w=[("srT", 2 * h + a)])
            po = P[6 + (h % 2)]
            pokey = "P%d" % (6 + (h % 2))
            for c in range(4):
                g, hf = c // 2, c % 2
                pkv = P[4 + (c % 2)]
                kvkey = "P%d" % (4 + (c % 2))
                MM(pkv[:, 0:256], kd[hf * 64:(hf + 1) * 64, g, h, :], vtok[hf * 64:(hf + 1) * 64, g, h * 256:(h + 1) * 256],
                   True, True, r=[("kd", h), ("vtok", h)], w=[kvkey])
                STT(Sst[:, h, :], Sst[:, h, :], dec[:, h * 4 + c:h * 4 + c + 1], pkv[:, 0:256], OP.mult, OP.add,
                    r=[("Sst", h), "dec", kvkey], w=[("Sst", h)])
                if main:
                    CP("act", Sbf[:, h, :], Sst[:, h, :], r=[("Sst", h)], w=[("Sbf", h)])
                    for a in range(2):
                        MM(po[:, a * 256 + c * 64:a * 256 + (c + 1) * 64], Sbf[:, h, a * 128:(a + 1) * 128],
                           qT[:, h, c * 64:(c + 1) * 64], True, True, r=[("Sbf", h), ("qT", h)], w=[pokey])
            if main:
                ACT(sqb[0][:], po[:, 0:512], AF.Square, r=[pokey], w=[("sqb", 0)])
                MM(P[4][:, 0:TT], onesB[:], sqb[0][:, 0:TT], True, False, r=[("sqb", 0), "onesB"], w=["P4"])
                MM(P[4][:, 0:TT], onesB[:], sqb[0][:, TT:2 * TT], False, True, r=[("sqb", 0), "onesB"], w=["P4"])
                TS(tf[0][:], P[4][:, 0:TT], 1.0 / 256, EPS, OP.mult, OP.add, r=["P4"], w=[("tf", 0)])
                ACT(tf[0][:], tf[0][:], AF.Sqrt, r=[("tf", 0)], w=[("tf", 0)])
                S.op("dve", lambda e: e.reciprocal(out=tf[0][:], in_=tf[0][:]), r=[("tf", 0)], w=[("tf", 0)])
                for a in range(2):
                    blk = 2 * h + a
                    TTo(tf[1 + a][:], po[:, a * 256:(a + 1) * 256], tf[0][:], OP.mult, r=[pokey, ("tf", 0)], w=[("tf", 1 + a)])
                    STT(mixT[:, blk, :], tf[1 + a][:], ggc[:, blk:blk + 1], srT[:, blk, :], OP.mult, OP.mult,
                        r=[("tf", 1 + a), "cols", ("srT", blk)], w=[("mixT", blk)])
        if not main:
            return
        CP("act", halo[:], uT[:, :, TT:TT + 30], r=["uT"], w=["halo"])
        for blk in range(16):
            ACT(sqb[0][:, 0:TT], yv[:, blk, :], AF.Copy, r=[("yT", blk)], w=[("sqb", 0)])
            MM(P[4][:, 0:TT], onesB[:], sqb[0][:, 0:TT], blk == 0, blk == 15, r=[("sqb", 0), "onesB"], w=["P4"])
            ACT(sqb[1][:, 0:TT], yv[:, blk, :], AF.Square, r=[("yT", blk)], w=[("sqb", 1)])
            MM(P[5][:, 0:TT], onesB[:], sqb[1][:, 0:TT], blk == 0, blk == 15, r=[("sqb", 1), "onesB"], w=["P5"])
        TS(tf[0][:], P[4][:, 0:TT], 1.0 / 2048, None, OP.mult, None, r=["P4"], w=[("tf", 0)])
        TTo(tf[1][:], tf[0][:], tf[0][:], OP.mult, r=[("tf", 0)], w=[("tf", 1)])
        STT(tf[1][:], P[5][:, 0:TT], 1.0 / 2048, tf[1][:], OP.mult, OP.subtract, r=["P5", ("tf", 1)], w=[("tf", 1)])
        TS(tf[1][:], tf[1][:], EPS, None, OP.add, None, r=[("tf", 1)], w=[("tf", 1)])
        ACT(tf[1][:], tf[1][:], AF.Sqrt, r=[("tf", 1)], w=[("tf", 1)])
        S.op("dve", lambda e: e.reciprocal(out=tf[1][:], in_=tf[1][:]), r=[("tf", 1)], w=[("tf", 1)])
        for blk in range(16):
            TTo(yv[:, blk, :], yv[:, blk, :], tf[0][:], OP.subtract, r=[("yT", blk), ("tf", 0)], w=[("yT", blk)])
            TTo(yv[:, blk, :], yv[:, blk, :], tf[1][:], OP.mult, r=[("yT", blk), ("tf", 1)], w=[("yT", blk)])
            TS(yv[:, blk, :], yv[:, blk, :], lgc[:, blk:blk + 1], lbc[:, blk:blk + 1], OP.mult, OP.add,
               r=[("yT", blk), "cols"], w=[("yT", blk)])
            ACT(mixT[:, 16 + blk, :], yv[:, blk, :], AF.Silu, r=[("yT", blk)], w=[("mixT", 16 + blk)])

    def out_proj(ws):
        for ob in range(32):
            b = ws.get(ob)
            po_, pkey = proj(b, mixT, "mixT")
            TTo(xT[:, ob, :], po_[:, 0:TT], xT[:, ob, :], OP.add, r=[pkey, ("xT", ob // 4)], w=[("xT", ob // 4)])

    def peer(wsq, wsu, vsrc):
        norm_xT(g2c)
        for blk in range(16):
            b = wsq.get(blk)
            pp, pkey = proj(b, xnT, "xnT")
            ACT(pqT[:, blk, :], pp[:, 0:TT], AF.Copy, r=[pkey], w=[("pqT", blk)])
        for g in range(2):
            for quad in range(4):
                for i in range(4):
                    blk = quad * 4 + i
                    MM(P[2][:, i * 128:(i + 1) * 128], pqT[:, blk, g * 128:(g + 1) * 128], keysT[:, blk, :], True, True,
                       r=[("pqT", blk), "keysT"], w=["P2"])
                CP(cp_eng(), sall[:, g, quad * 4:(quad + 1) * 4, :], P[2][:, 0:512].rearrange("p (a n) -> p a n", a=4),
                   r=["P2"], w=[("sall", g)])
        def topk_chain(g, h):
            v1, v2, c24, cand, wk, sm, d16 = v1s[g], v2s[g], c24s[g], cands[g], wks[g], sms[g], d16s[g]
            ta, tb = gt[g], ga[g]
            K = lambda n: (n, g)
            s1 = sall[:, g, 2 * h, :]
            s2 = sall[:, g, 2 * h + 1, :]
            for (s_, v_, vk) in ((s1, v1, K("v1")), (s2, v2, K("v2"))):
                S.op("dve", lambda e, s_=s_, v_=v_: e.max(out=v_[:, 0:8], in_=s_), r=[("sall", g)], w=[vk])
                yield
                S.op("dve", lambda e, s_=s_, v_=v_: e.match_replace(out=wk[:, 0:128], in_to_replace=v_[:, 0:8], in_values=s_,
                                                                    imm_value=-1e30), r=[("sall", g), vk], w=[K("wk")])
                yield
                S.op("dve", lambda e, v_=v_: e.max(out=v_[:, 8:16], in_=wk[:, 0:128]), r=[K("wk")], w=[vk])
                yield
            TTo(cand[:].rearrange("p (i j) -> p i j", i=16), v1[:].unsqueeze(2).to_broadcast([128, 16, 16]),
                v2[:].unsqueeze(1).to_broadcast([128, 16, 16]), OP.add, r=[K("v1"), K("v2")], w=[K("cand")])
            yield
            S.op("dve", lambda e: e.max(out=c24[:, 0:8], in_=cand[:]), r=[K("cand")], w=[K("c24")])
            yield
            S.op("dve", lambda e: e.match_replace(out=wk[:], in_to_replace=c24[:, 0:8], in_values=cand[:], imm_value=-1e30),
                 r=[K("cand"), K("c24")], w=[K("wk")])
            yield
            S.op("dve", lambda e: e.max(out=c24[:, 8:16], in_=wk[:]), r=[K("wk")], w=[K("c24")])
            yield
            S.op("dve", lambda e: e.match_replace(out=cand[:], in_to_replace=c24[:, 8:16], in_values=wk[:], imm_value=-1e30),
                 r=[K("wk"), K("c24")], w=[K("cand")])
            yield
            S.op("dve", lambda e: e.max(out=c24[:, 16:24], in_=cand[:]), r=[K("cand")], w=[K("c24")])
            yield
            TS(sm[:, 0:1], c24[:, 15:16], c24[:, 16:17], 0.5, OP.add, OP.mult, r=[K("c24")], w=[K("sm")])
            yield
            TS(d16[:], c24[:, 0:16], c24[:, 0:1], None, OP.subtract, None, r=[K("c24")], w=[K("d16")])
            yield
            ACT(d16[:], d16[:], AF.Exp, r=[K("d16")], w=[K("d16"), K("smz")], accum_out=sm[:, 1:2])
            yield
            S.op("dve", lambda e: e.reciprocal(out=sm[:, 2:3], in_=sm[:, 1:2]), r=[K("smz"), K("sm")], w=[K("sm")])
            yield
            TS(ta[:], s1, v1[:, 0:1], None, OP.subtract, None, r=[("sall", g), K("v1")], w=[("gt", g)])
            yield
            ACT(ta[:], ta[:], AF.Exp, r=[("gt", g)], w=[("gt", g)])
            yield
            TS(tb[:], s2, v2[:, 0:1], None, OP.subtract, None, r=[("sall", g), K("v2")], w=[("ga", g)])
            yield
            ACT(tb[:], tb[:], AF.Exp, r=[("ga", g)], w=[("ga", g)])
            yield
            STT(ta[:], s1, v1[:, 15:16], ta[:], OP.is_ge, OP.mult, r=[("sall", g), K("v1"), ("gt", g)], w=[("gt", g)])
            yield
            TS(E1[:, g, h, :], ta[:], sm[:, 2:3], None, OP.mult, None, r=[("gt", g), K("sm")], w=[("E1", g)])
            yield
            STT(E2[:, g, h, :], s2, v2[:, 15:16], tb[:], OP.is_ge, OP.mult, r=[("sall", g), K("v2"), ("ga", g)], w=[("E2", g)])
            yield
            TS(TH[:, g, h, :], s1, -1.0, sm[:, 0:1], OP.mult, OP.add, r=[("sall", g), K("sm")], w=[("TH", g)])
            yield

        for h in range(8):
            chains = [topk_chain(0, h), topk_chain(1, h)]
            while chains:
                for c in list(chains):
                    try:
                        next(c)
                    except StopIteration:
                        chains.remove(c)

        vctr = [0]
        wring = [0]

        def a_mms(n1):
            b = wsu.get(n1)
            bank = n1 % 2
            return [lambda kc=kc, b=b, bank=bank: MM(P[bank][:, 0:TT], wt[b][:, kc, :], xnT[:, kc, :], kc == 0, kc == KC - 1,
                                                     r=[("wt", b), "xnT"], w=["P%d" % bank]) for kc in range(KC)]

        def stage_G(n1):
            pe_ops = []
            sbuf_, skey = GSLOT[n1 % 2]
            for g in range(2):
                for h in range(8):
                    i = wring[0]
                    wring[0] += 1
                    gb, wb = gtr[i % 4], Wb[i % 16]
                    STT(gb[:], sall[:, g, 2 * h + 1, :], TH[:, g, h, n1:n1 + 1], E2[:, g, h, :], OP.is_ge, OP.mult,
                        r=[("sall", g), ("TH", g), ("E2", g)], w=[("gtr", i % 4)])
                    ACT(wb[:], gb[:], AF.Copy, r=[("gtr", i % 4), ("E1", g)], w=[("Wb", i % 16)], scale=E1[:, g, h, n1:n1 + 1])
                    pe_ops.append(lambda g=g, h=h, wb=wb, i=i, sbuf_=sbuf_, skey=skey: MM(
                        sbuf_[:, g * 128:(g + 1) * 128], wb[:], identB[:], h == 0, h == 7,
                        r=[("Wb", i % 16), "identB"], w=[skey]))
            return pe_ops

        def stage_T(n1):
            eg, eb = n1 // 8, n1 % 8
            hb = eg % 2
            sbuf_, skey = GSLOT[n1 % 2]
            TTo(HT[hb][:, eb, :], gl[n1 % 2][:], sbuf_[:, 0:256], OP.mult, r=[("gl", n1 % 2), skey], w=[("HT", hb)])

        def v_chunk(eg, ds):
            hb = eg % 2
            vb = vctr[0] % 2
            vctr[0] += 1
            DMA("sp", vt[vb][:].rearrange("p e d -> p (e d)"), vrb[eg * 8 + ds], r=[("vrb", (eg * 8 + ds) // 16)],
                w=[("vt", vb)])
            for dq in range(4):
                db = ds * 4 + dq
                for e8 in range(8):
                    MM(P[4 + dq][:, 0:TT], vt[vb][:, e8, dq * 128:(dq + 1) * 128], HT[hb][:, e8, :], e8 == 0, e8 == 7,
                       r=[("vt", vb), ("HT", hb)], w=["P%d" % (4 + dq)])
                TTo(xT[:, db, :], P[4 + dq][:, 0:TT], xT[:, db, :], OP.add, r=["P%d" % (4 + dq), ("xT", db // 4)], w=[("xT", db // 4)])

        pend = []
        for n1 in range(129):
            if n1 < 128:
                k = 0
                for j, f in enumerate(a_mms(n1)):
                    f()
                    if j % 2 == 1 and k < len(pend):
                        pend[k]()
                        k += 1
                while k < len(pend):
                    pend[k]()
                    k += 1
                ACT(gl[n1 % 2][:], P[n1 % 2][:, 0:TT], AF.Gelu, r=["P%d" % (n1 % 2)], w=[("gl", n1 % 2)])
                new_pend = stage_G(n1)
            else:
                for f in pend:
                    f()
                new_pend = []
            if n1 >= 1:
                m = n1 - 1
                stage_T(m)
                if m >= 8:
                    v_chunk(m // 8 - 1, m % 8)
            pend = new_pend
        for ds in range(8):
            v_chunk(15, ds)

    def final_out(dst):
        fm_rstd(D, KC, lambda kc: xT[:, kc, :], lambda kc: ("xT", kc // 4))
        for kc in range(KC):
            STT(xT[:, kc, :], xT[:, kc, :], gfc[:, kc:kc + 1], rstd[:], OP.mult, OP.mult,
                r=[("xT", kc // 4), "cols", "rstd"], w=[("xT", kc // 4)])
        for g in range(2):
            for kq in range(8):
                for i in range(4):
                    kc = kq * 4 + i
                    TR(P[2][:, i * 128:(i + 1) * 128], xT[:, kc, g * 128:(g + 1) * 128], identF[:],
                       r=[("xT", kq), "identF"], w=["P2"])
                CP(cp_eng(), xin[:, kq * 512:(kq + 1) * 512], P[2][:, 0:512], r=["P2"], w=["xin"])
            DMA("sp", dst[g * 128:(g + 1) * 128, :], xin[:], r=["xin"], w=["y"])

    def win_srcs(mode):
        idx = []
        if mode != "pre":
            idx += [48 + i for i in range(32)]
        for h in range(8):
            idx += [h * 6 + i for i in (range(6) if mode == "main" else range(3))]
        return [(winb[i], ("winb", i // 16)) for i in idx]

    for t in range(n_pre):
        mode = "pre_last" if t == n_pre - 1 else "pre"
        token_mix(xp[t * TT:(t + 1) * TT, :], mode, WStream(win_srcs(mode)))
        if late_conv and t < n_pre - 1:
            late_conv.pop(0)(["dec"])
        if t == n_pre - 1:
            while late_conv:
                late_conv.pop(0)(["dec"])
            S.barrier()
    for t in range(n_main):
        token_mix(xm[t * TT:(t + 1) * TT, :], "main", WStream(win_srcs("main")))
        out_proj(WStream([(woutb[i], ("woutb", i // 16)) for i in range(32)]))
        S.barrier()
        peer(WStream([(wqb[i], ("wqb", i // 16)) for i in range(16)]),
             WStream([(utb[i], ("utb", i // 16)) for i in range(128)]), vrd)
        final_out(y[t * TT:(t + 1) * TT, :])
        S.barrier()
    S.finish()
    S.run()
    return nc


def _blk(wm):
    k, n = wm.shape
    return np.ascontiguousarray(wm.reshape(k // 128, 128, n // 128, 128).transpose(2, 1, 0, 3)).reshape(n // 128, 128, k)


def _consts():
    c = np.zeros((128, 258), np.float32)
    c[:, 0:128] = np.eye(128, dtype=np.float32)
    t = np.arange(128)
    c[:, 128:256] = np.where((t[:, None] > t[None, :]) & ((t[:, None] // 64) == (t[None, :] // 64)), -1.0 / 16.0, 0.0)
    c[:, 256] = np.where(t < 64, -1.0 / 16.0, 0.0)
    c[:, 257] = np.where(t >= 64, -1.0 / 16.0, 0.0)
    return c


def prep_weights(w_in, w_gate_up, b_gate, gla_norm_g, w_dw, b_dw, conv_ln_g, conv_ln_b, w_out, norm1_g, norm2_g,
                 peer_wq, peer_keys1, peer_keys2, peer_u, peer_v, final_norm_g):
    f = lambda a: np.asarray(a, dtype=np.float32)
    wi = f(w_in)[0]
    q, k, v, r = wi[:, 0:1024], wi[:, 1024:2048], wi[:, 2048:4096], wi[:, 4096:6144]
    gd, ca, cb = wi[:, 6144:6160], wi[:, 6160:8208], wi[:, 8208:10256]
    qb, kb, vb, rb, cab, cbb = _blk(q), _blk(k), _blk(v), _blk(r), _blk(ca), _blk(cb)
    blocks = []
    for h in range(8):
        blocks += [kb[h], vb[2 * h], vb[2 * h + 1], qb[h], rb[2 * h], rb[2 * h + 1]]
    for b in range(16):
        blocks += [cbb[b], cab[b]]
    win = np.stack(blocks)
    wgd = np.ascontiguousarray(gd.reshape(32, 128, 16).transpose(1, 0, 2)).reshape(128, 512)
    wout = _blk(f(w_out)[0])
    wq = _blk(f(peer_wq)[0])
    ut = np.ascontiguousarray(f(peer_u)[0].reshape(128, 128, 32, 128).transpose(0, 3, 2, 1)).reshape(128, 128, 4096)
    vr = np.ascontiguousarray(f(peer_v)[0].reshape(16, 8, 128, 8, 512).transpose(0, 3, 2, 1, 4)).reshape(128, 128, 4096)
    k1, k2 = f(peer_keys1)[0], f(peer_keys2)[0]
    keyst = np.zeros((128, 16, 128), np.float32)
    for h in range(8):
        keyst[:, 2 * h, :] = k1[h].T
        keyst[:, 2 * h + 1, :] = k2[h].T
    keyst = keyst.reshape(128, 2048)
    wga = np.concatenate([f(w_gate_up)[0], f(b_gate)[0][None, :]], axis=0)
    colv = lambda a, n: np.ascontiguousarray(f(a).reshape(n, 128).T)
    cols = np.concatenate([
        colv(norm1_g[0], 32), colv(norm2_g[0], 32), colv(final_norm_g, 32),
        colv(gla_norm_g[0], 16), colv(conv_ln_g[0], 16), colv(conv_ln_b[0], 16), colv(b_dw[0], 16),
        np.ascontiguousarray(f(w_dw)[0].reshape(31, 16, 128).transpose(2, 1, 0)).reshape(128, 496),
    ], axis=1)
    assert cols.shape == (128, NCOLS)
    return dict(win=win, wgd=wgd, wout=wout, wq=wq, ut=ut, vr=vr, keyst=keyst, wga=wga,
                cols=np.ascontiguousarray(cols), consts=_consts())


def make_core_inputs(xb, meta, start, n_main, n_pre):
    xm = np.ascontiguousarray(xb[start:start + n_main * TT])
    xp = np.zeros((n_pre * TT, D), np.float32)
    pre = np.concatenate([meta, xb[:start]], axis=0)
    assert pre.shape[0] <= n_pre * TT
    xp[n_pre * TT - pre.shape[0]:] = pre
    return xm, xp


_NC_CACHE = {}


def kernel(x, meta_tokens, norm1_g, w_in, w_gate_up, b_gate, gla_norm_g, w_dw, b_dw, conv_ln_g, conv_ln_b,
           w_out, norm2_g, peer_wq, peer_keys1, peer_keys2, peer_u, peer_v, final_norm_g):
    x = np.asarray(x, dtype=np.float32)
    meta = np.asarray(meta_tokens, dtype=np.float32)
    wts = prep_weights(w_in, w_gate_up, b_gate, gla_norm_g, w_dw, b_dw, conv_ln_g, conv_ln_b, w_out, norm1_g,
                       norm2_g, peer_wq, peer_keys1, peer_keys2, peer_u, peer_v, final_norm_g)
    B, L, _ = x.shape
    per = N_MAIN_TILES * TT
    in_maps = []
    for c in range(8):
        b, j = c // 4, c % 4
        xm, xp = make_core_inputs(x[b], meta, j * per, N_MAIN_TILES, N_PRE_TILES)
        m = dict(wts)
        m["xm"] = xm
        m["xp"] = xp
        in_maps.append(m)
    if "nc" not in _NC_CACHE:
        _NC_CACHE["nc"] = build_nc()
    res = run_bass_kernel_spmd(_NC_CACHE["nc"], in_maps, core_ids=list(range(8)))
    out = np.empty((B, L, D), np.float32)
    for c in range(8):
        b, j = c // 4, c % 4
        out[b, j * per:(j + 1) * per] = res.results[c]["y"]
    return out
```

```python
import contextlib
import numpy as np
import concourse.bass as bass
import concourse.mybir as mybir
from concourse.bass_utils import run_bass_kernel_spmd

F32 = mybir.dt.float32
BF16 = mybir.dt.bfloat16
AF = mybir.ActivationFunctionType
OP = mybir.AluOpType

D = 4096
KC = 32
TT = 256
EPS = 1e-6
N_MAIN_TILES = 8
N_PRE_TILES = 26
NCOLS = 32 * 3 + 16 * 4 + 16 * 31


class Sched:
    ENGS = ("pe", "act", "dve", "pool", "sp")

    def __init__(self, nc, same_engine_sync=True):
        self.nc = nc
        self.ops = []
        self.state = {}
        self.same_engine_sync = same_engine_sync
        self.n_dma_sems = {"pool": 28, "sp": 24, "act": 2}
        self.unbarriered_dma = []
        self.nosync = ("pe",)
        self.persist = set()
        self.excl = set("P%d" % i for i in range(8))

    def _entries(self, key):
        base, idx = key if isinstance(key, tuple) else (key, None)
        st = self.state.setdefault(base, {"*": [None, []]})
        if idx is None:
            return list(st.values())
        if idx not in st:
            st[idx] = [st["*"][0], list(st["*"][1])]
        return [st[idx]]

    def _add(self, eng, fn, r, w, dma):
        xr = tuple(k for k in r if (k[0] if isinstance(k, tuple) else k) in self.excl)
        if xr:
            r = tuple(k for k in r if k not in xr)
            w = tuple(w) + xr
        i = len(self.ops)
        deps = set()
        for key in r:
            for ent in self._entries(key):
                if ent[0] is not None:
                    deps.add(ent[0])
        for key in w:
            for ent in self._entries(key):
                if ent[0] is not None:
                    deps.add(ent[0])
                deps.update(ent[1])
        for key in r:
            for ent in self._entries(key):
                if not dma:
                    ent[1][:] = [j for j in ent[1] if self.ops[j]["dma"] or self.ops[j]["eng"] != eng]
                ent[1].append(i)
        for key in w:
            for ent in self._entries(key):
                ent[0] = i
                ent[1] = []
        deps.discard(i)
        pers = dma and any((k[0] if isinstance(k, tuple) else k) in self.persist for k in w)
        self.ops.append(dict(eng=eng, fn=fn, deps=deps, dma=dma, needs_inc=False, pers=pers))
        if dma and not pers:
            self.unbarriered_dma.append(i)
        return i

    def op(self, eng, fn, r=(), w=(), big=False):
        i = self._add(eng, fn, tuple(r), tuple(w), False)
        self.ops[i]["big"] = big
        return i

    def dma(self, eng, fn, r=(), w=()):
        return self._add(eng, fn, tuple(r), tuple(w), True)

    def barrier(self):
        last = {}
        for i, o in enumerate(self.ops):
            if o.get("barrier") or o["dma"]:
                continue
            last[o["eng"]] = i
        deps = set(last.values()) | set(self.unbarriered_dma)
        self.unbarriered_dma = []
        self.state = {k: v for k, v in self.state.items() if k in self.persist}
        self.ops.append(dict(eng=None, fn=None, deps=deps, dma=False, needs_inc=False, barrier=True))

    def finish(self):
        self.barrier()
        self.ops.append(dict(eng="sp", fn=None, deps=set(), dma=False, needs_inc=False))

    def plan(self):
        ops = self.ops
        pending = {e: set() for e in self.ENGS}
        for o in ops:
            if o.get("barrier"):
                for e in self.ENGS:
                    pending[e] |= o["deps"]
            else:
                e = o["eng"]
                if pending[e]:
                    o["deps"] = set(o["deps"]) | pending[e]
                    pending[e] = set()
        real = [(i, o) for i, o in enumerate(ops) if not o.get("barrier")]
        dma_ctr = {e: 0 for e in self.ENGS}
        dma_prev = {}
        for i, o in real:
            if o["dma"]:
                e = o["eng"]
                if o.get("pers"):
                    e = "cv"
                    slot = dma_ctr.get("cv", 0)
                    dma_ctr["cv"] = slot + 1
                else:
                    slot = dma_ctr[e] % self.n_dma_sems[e]
                    dma_ctr[e] += 1
                o["dslot"] = (e, slot)
                prev = dma_prev.get((e, slot))
                o["dprev"] = prev
                o["dtarget"] = (ops[prev]["dtarget"] + 16) if prev is not None else 16
                dma_prev[(e, slot)] = i

        def skip_same(p, e):
            return p["eng"] == e and (e in self.nosync or (e == "dve" and p.get("big")) or not self.same_engine_sync)

        for i, o in real:
            for d in o["deps"]:
                p = ops[d]
                if p["dma"] or skip_same(p, o["eng"]):
                    continue
                p["needs_inc"] = True
        cnt = {e: 0 for e in self.ENGS}
        for i, o in real:
            if (not o["dma"]) and o["needs_inc"]:
                cnt[o["eng"]] += 1
                o["count"] = cnt[o["eng"]]
        seen = {e: {f: 0 for f in self.ENGS} for e in self.ENGS}
        seen_dma = {e: {} for e in self.ENGS}
        for i, o in real:
            e = o["eng"]
            best = {}
            cands = []
            if o["dma"] and o["dprev"] is not None:
                cands.append(o["dprev"])
            cands.extend(o["deps"])
            for d in cands:
                p = ops[d]
                if p["dma"]:
                    key = p["dslot"]
                    if seen_dma[e].get(key, 0) < p["dtarget"]:
                        seen_dma[e][key] = p["dtarget"]
                        best[("dma", key)] = max(best.get(("dma", key), 0), p["dtarget"])
                else:
                    if skip_same(p, e):
                        continue
                    if seen[e][p["eng"]] < p["count"]:
                        seen[e][p["eng"]] = p["count"]
                        best[("eng", p["eng"])] = max(best.get(("eng", p["eng"]), 0), p["count"])
            o["waits"] = [(k[0], k[1], v) for k, v in best.items()]
        self.cnt = cnt
        return real

    def run(self):
        nc = self.nc
        real = self.plan()
        with contextlib.ExitStack() as es:
            esem = {e: es.enter_context(nc.semaphore("s_" + e)) for e in self.ENGS}
            dsem = {}
            for key in sorted(set(o["dslot"] for i, o in real if o["dma"])):
                dsem[key] = es.enter_context(nc.semaphore("d_%s_%d" % key))
            block = es.enter_context(nc.Block())
            per_eng = {e: [] for e in self.ENGS}
            for i, o in real:
                per_eng[o["eng"]].append(o)

            def emit_engine(ename, eng):
                for o in per_eng[ename]:
                    for kind, key, val in o["waits"]:
                        eng.wait_ge(dsem[key] if kind == "dma" else esem[key], val)
                    if o["fn"] is None:
                        continue
                    ins = o["fn"](eng)
                    if o["dma"]:
                        ins.then_inc(dsem[o["dslot"]], 16)
                    elif o["needs_inc"]:
                        ins.then_inc(esem[ename], 1)

            @block.tensor
            def _(eng):
                emit_engine("pe", eng)

            @block.scalar
            def _(eng):
                emit_engine("act", eng)

            @block.vector
            def _(eng):
                emit_engine("dve", eng)

            @block.gpsimd
            def _(eng):
                emit_engine("pool", eng)

            @block.sync
            def _(eng):
                emit_engine("sp", eng)


def build_nc(n_main=N_MAIN_TILES, n_pre=N_PRE_TILES):
    nc = bass.Bass("TRN2", target_bir_lowering=False)
    S = Sched(nc)

    def din(name, shape):
        return nc.dram_tensor(name, shape, F32, kind="ExternalInput").ap()

    xm = din("xm", [n_main * TT, D])
    xp = din("xp", [n_pre * TT, D])
    win = din("win", [80, 128, D])
    wgd = din("wgd", [128, KC * 16])
    wout = din("wout", [32, 128, D])
    wqd = din("wq", [16, 128, D])
    utd = din("ut", [128, 128, D])
    vrd = din("vr", [128, 128, D])
    keyd = din("keyst", [128, 16 * 128])
    wgad = din("wga", [17, 1024])
    colsd = din("cols", [128, NCOLS])
    constd = din("consts", [128, 258])
    y = nc.dram_tensor("y", [n_main * TT, D], F32, kind="ExternalOutput").ap()
    winb = nc.dram_tensor("winb", [80, 128, D], BF16).ap()
    woutb = nc.dram_tensor("woutb", [32, 128, D], BF16).ap()
    wqb = nc.dram_tensor("wqb", [16, 128, D], BF16).ap()
    utb = nc.dram_tensor("utb", [128, 128, D], BF16).ap()
    vrb = nc.dram_tensor("vrb", [128, 128, D], BF16).ap()
    S.persist.update(["winb", "woutb", "wqb", "utb", "vrb"])

    off = [16512]

    def sb(name, shape, dt, at=None):
        nb = int(np.prod(shape[1:])) * (4 if dt == F32 else 2)
        nb = (nb + 63) // 64 * 64
        if at is None:
            t = nc.alloc_sbuf_tensor_at(name, shape, dt, offset=off[0])
            off[0] += nb
        else:
            t = nc.alloc_sbuf_tensor_at(name, shape, dt, offset=at[0])
            at[0] += nb
        return t

    identF = sb("identF", [128, 128], F32)
    triN = sb("triN", [128, 128], F32)
    chunkN = sb("chunkN", [128, 2], F32)
    identB = sb("identB", [128, 128], BF16)
    onesB = sb("onesB", [128, 128], BF16)
    cols = sb("cols", [128, NCOLS], F32)
    keysT = sb("keysT", [128, 16, 128], BF16)
    wgdT = sb("wgdT", [128, KC, 16], BF16)
    wgA = sb("wgA", [32, 1024], F32)
    gdA = sb("gdA", [32, TT], F32)
    Sst = sb("Sst", [128, 8, 256], F32)
    Sbf = sb("Sbf", [128, 8, 256], BF16)
    halo = sb("halo", [128, 16, 30], F32)
    xT = sb("xT", [128, KC, TT], F32)
    xnT = sb("xnT", [128, KC, TT], BF16)
    wt = [sb("wt%d" % i, [128, KC, 128], BF16) for i in range(3)]
    xin = sb("xin", [128, D], F32)
    rstd = sb("rstd", [128, TT], F32)
    sqb = [sb("sqb%d" % i, [128, 2 * TT], BF16) for i in range(2)]
    region = off[0]
    a1 = [region]
    xinB = nc.alloc_sbuf_tensor_at("xinB", [128, D], F32, offset=region)
    mixT = sb("mixT", [128, KC, TT], BF16, a1)
    nsp = sb("nsp", [128, 2, 1024], F32, a1)
    erevT = sb("erevT", [128, 8, TT], F32, a1)
    dec = sb("dec", [128, 32], F32, a1)
    qT = sb("qT", [128, 8, TT], BF16, a1)
    kdT = [sb("kdT%d" % i, [128, TT], BF16, a1) for i in range(2)]
    kd = sb("kd", [128, 2, 8, 128], BF16, a1)
    vtok = sb("vtok", [128, 2, 2048], BF16, a1)
    srT = sb("srT", [128, 16, TT], BF16, a1)
    uT = sb("uT", [128, 16, TT + 30], F32, a1)
    sig = [sb("sig%d" % i, [128, TT], F32, a1) for i in range(2)]
    tf = [sb("tf%d" % i, [128, TT], F32, a1) for i in range(3)]
    end1 = a1[0]
    a2 = [region]
    pqT = sb("pqT", [128, 16, TT], BF16, a2)
    sall = sb("sall", [128, 2, 16, 128], F32, a2)
    E2 = sb("E2", [128, 2, 8, 128], F32, a2)
    TH = sb("TH", [128, 2, 8, 128], F32, a2)
    E1 = sb("E1", [128, 2, 8, 128], F32, a2)
    vt = [sb("vt%d" % i, [128, 8, 512], BF16, a2) for i in range(2)]
    HT = [sb("HT%d" % i, [128, 8, TT], BF16, a2) for i in range(2)]
    gl = [sb("gl%d" % i, [128, TT], F32, a2) for i in range(2)]
    Wb = [sb("Wb%d" % i, [128, 128], BF16, a2) for i in range(16)]
    gtr = [sb("gtr%d" % i, [128, 128], F32, a2) for i in range(4)]
    gt = [sb("gt%d" % i, [128, 128], F32, a2) for i in range(2)]
    ga = [sb("ga%d" % i, [128, 128], F32, a2) for i in range(2)]
    v1s = [sb("v1_%d" % i, [128, 16], F32, a2) for i in range(2)]
    v2s = [sb("v2_%d" % i, [128, 16], F32, a2) for i in range(2)]
    c24s = [sb("c24_%d" % i, [128, 24], F32, a2) for i in range(2)]
    cands = [sb("cand%d" % i, [128, 256], F32, a2) for i in range(2)]
    wks = [sb("wk%d" % i, [128, 256], F32, a2) for i in range(2)]
    sms = [sb("sm%d" % i, [128, 16], F32, a2) for i in range(2)]
    d16s = [sb("d16_%d" % i, [128, 16], F32, a2) for i in range(2)]

    end2 = a2[0]
    assert max(end1, end2) <= 229344, (end1, end2)
    yT = xin

    P = [nc.alloc_psum_tensor("P%d" % i, [128, 512], F32) for i in range(3)]
    P3 = nc.alloc_psum_tensor("P3", [128, 1024], BF16)
    P += [None] + [nc.alloc_psum_tensor("P%d" % i, [128, 512], F32) for i in range(4, 8)]
    P2b = P[2][:].bitcast(BF16)
    TSLOT = [(P3, "P3"), (P2b, "P2")]
    GSLOT = [(P3[:].bitcast(F32), "P3"), (P[2], "P2")]

    g1c = cols[:, 0:32]
    g2c = cols[:, 32:64]
    gfc = cols[:, 64:96]
    ggc = cols[:, 96:112]
    lgc = cols[:, 112:128]
    lbc = cols[:, 128:144]
    bdc = cols[:, 144:160]
    wdwc = cols[:, 160:160 + 496]

    def MM(out, lhsT, rhs, start, stop, r, w):
        S.op("pe", lambda e: e.matmul(out, lhsT=lhsT, rhs=rhs, start=start, stop=stop), r=r, w=w)

    def TR(out, in_, ident, r, w):
        S.op("pe", lambda e: e.transpose(out=out, in_=in_, identity=ident), r=r, w=w)

    def ACT(out, in_, func, r, w, **kw):
        S.op("act", lambda e: e.activation(out=out, in_=in_, func=func, **kw), r=r, w=w)

    def isbig(ap):
        return int(np.prod(ap.shape[1:])) >= 128

    def TS(out, in0, s1, s2, op0, op1, r, w, eng="dve"):
        if s2 is None:
            S.op(eng, lambda e: e.tensor_scalar(out=out, in0=in0, scalar1=s1, scalar2=None, op0=op0), r=r, w=w, big=isbig(out))
        else:
            S.op(eng, lambda e: e.tensor_scalar(out=out, in0=in0, scalar1=s1, scalar2=s2, op0=op0, op1=op1), r=r, w=w, big=isbig(out))

    def TTo(out, in0, in1, op, r, w, eng="dve"):
        S.op(eng, lambda e: e.tensor_tensor(out=out, in0=in0, in1=in1, op=op), r=r, w=w, big=isbig(out))

    def STT(out, in0, scalar, in1, op0, op1, r, w):
        S.op("dve", lambda e: e.scalar_tensor_tensor(out=out, in0=in0, scalar=scalar, in1=in1, op0=op0, op1=op1), r=r, w=w,
             big=isbig(out))

    def CP(eng, out, in_, r, w):
        if eng == "act":
            S.op("act", lambda e: e.activation(out=out, in_=in_, func=AF.Copy), r=r, w=w)
        else:
            S.op(eng, lambda e: e.tensor_copy(out=out, in_=in_), r=r, w=w)

    def DMA(eng, out, in_, r, w):
        S.dma(eng, lambda e: e.dma_start(out=out, in_=in_), r=r, w=w)

    cp_rr = [0]

    def cp_eng():
        cp_rr[0] += 1
        return "act" if cp_rr[0] % 2 else "dve"

    wctr = [0]

    class WStream:
        def __init__(self, srcs, depth=2):
            self.srcs = srcs
            self.issued = 0
            self.base = wctr[0]
            self.depth = depth
            wctr[0] += len(srcs)

        def get(self, i):
            while self.issued < min(len(self.srcs), i + 1 + self.depth):
                b = (self.base + self.issued) % 3
                src, skey = self.srcs[self.issued]
                DMA("sp", wt[b][:].rearrange("p k c -> p (k c)"), src, r=[skey], w=[("wt", b)])
                self.issued += 1
            return (self.base + i) % 3

    pj = [0]

    def proj(b, rhsT, rkey, ncols=128):
        bank = pj[0] % 2
        pj[0] += 1
        for kc in range(KC):
            MM(P[bank][0:ncols, 0:TT], wt[b][:, kc, 0:ncols], rhsT[:, kc, :], kc == 0, kc == KC - 1,
               r=[("wt", b), rkey], w=["P%d" % bank])
        return P[bank], "P%d" % bank

    def fm_rstd(dim, nblk, src_of, src_key_of, pbank=4):
        for kc in range(nblk):
            sq = sqb[kc % 2]
            ACT(sq[:, 0:TT], src_of(kc), AF.Square, r=[src_key_of(kc)], w=[("sqb", kc % 2)])
            MM(P[pbank][:, 0:TT], onesB[:], sq[:, 0:TT], kc == 0, kc == nblk - 1,
               r=[("sqb", kc % 2), "onesB"], w=["P%d" % pbank])
        TS(rstd[:], P[pbank][:, 0:TT], 1.0 / dim, EPS, OP.mult, OP.add, r=["P%d" % pbank], w=["rstd"])
        ACT(rstd[:], rstd[:], AF.Sqrt, r=["rstd"], w=["rstd"])
        S.op("dve", lambda e: e.reciprocal(out=rstd[:], in_=rstd[:]), r=["rstd"], w=["rstd"])

    DMA("sp", identF[:], constd[:, 0:128], r=[], w=["identF"])
    DMA("sp", triN[:], constd[:, 128:256], r=[], w=["triN"])
    DMA("sp", chunkN[:], constd[:, 256:258], r=[], w=["chunkN"])
    DMA("sp", cols[:], colsd, r=[], w=["cols"])
    DMA("sp", wgA[0:17, :], wgad, r=[], w=["wgA"])
    DMA("pool", keysT[:].rearrange("p (a b) n -> p a (b n)", a=2), keyd.rearrange("p (a b) -> p a b", a=2), r=[], w=["keysT"])
    DMA("pool", wgdT[:].rearrange("p k c -> p (k c)"), wgd, r=[], w=["wgdT"])
    CP("act", identB[:], identF[:], r=["identF"], w=["identB"])
    S.op("dve", lambda e: e.memset(onesB[:], 1.0), w=["onesB"])
    S.op("dve", lambda e: e.memset(gdA[:], 1.0), w=["gdA"])
    S.op("dve", lambda e: e.memset(Sst[:].rearrange("p h v -> p (h v)"), 0.0), w=["Sst"])
    S.op("dve", lambda e: e.memset(halo[:].rearrange("p b j -> p (b j)"), 0.0), w=["halo"])

    def convert_ops(src3, dst3, nblk, key):
        ops = []
        for c0 in range(0, nblk, 16):
            n = min(16, nblk - c0)
            ops.append(lambda rr, c0=c0, n=n: DMA("pool", dst3[c0:c0 + n].rearrange("b p (a e) -> (b p) a e", a=2),
                                                  src3[c0:c0 + n].rearrange("b p (a e) -> (b p) a e", a=2), r=rr, w=[(key, c0 // 16)]))
        return ops

    for f in convert_ops(win, winb, 80, "winb"):
        f([])
    late_conv = (convert_ops(wout, woutb, 32, "woutb") + convert_ops(wqd, wqb, 16, "wqb") +
                 convert_ops(utd, utb, 128, "utb") + convert_ops(vrd, vrb, 128, "vrb"))

    def load_norm(src, gcol):
        for g in range(2):
            xb_, xk_ = (xin, "xin") if g == 0 else (xinB, "xinB")
            DMA("sp", xb_[:], src[g * 128:(g + 1) * 128, :], r=[], w=[xk_])
            for kq in range(8):
                for i in range(4):
                    kc = kq * 4 + i
                    TR(P[2][:, i * 128:(i + 1) * 128], xb_[:, kc * 128:(kc + 1) * 128], identF[:],
                       r=[xk_, "identF"], w=["P2"])
                CP(cp_eng(), xT[:, kq * 4:(kq + 1) * 4, g * 128:(g + 1) * 128],
                   P[2][:, 0:512].rearrange("p (a t) -> p a t", a=4), r=["P2"], w=[("xT", kq)])
        norm_xT(gcol)

    def norm_xT(gcol):
        fm_rstd(D, KC, lambda kc: xT[:, kc, :], lambda kc: ("xT", kc // 4))
        for kc in range(KC):
            STT(xnT[:, kc, :], xT[:, kc, :], gcol[:, kc:kc + 1], rstd[:], OP.mult, OP.mult,
                r=[("xT", kc // 4), "cols", "rstd"], w=[("xnT", kc)])

    def gla_gate():
        for kc in range(KC):
            MM(P[0][0:16, 0:TT], wgdT[:, kc, :], xnT[:, kc, :], kc == 0, kc == KC - 1,
               r=["wgdT", ("xnT", kc)], w=["P0"])
        CP("act", gdA[0:16, :], P[0][0:16, 0:TT], r=["P0"], w=["gdA"])
        for g in range(2):
            for hf in range(2):
                MM(P[4 + hf][:, 0:512], gdA[0:17, g * 128:(g + 1) * 128], wgA[0:17, hf * 512:(hf + 1) * 512],
                   True, True, r=["gdA", "wgA"], w=["P%d" % (4 + hf)])
                ACT(nsp[:, g, hf * 512:(hf + 1) * 512], P[4 + hf][:, 0:512], AF.Exp, r=["P%d" % (4 + hf)],
                    w=[("nsp", g)], scale=-1.0)
            TS(nsp[:, g, :], nsp[:, g, :], 1.0, None, OP.add, None, r=[("nsp", g)], w=[("nsp", g)])
            ACT(nsp[:, g, :], nsp[:, g, :], AF.Ln, r=[("nsp", g)], w=[("nsp", g)])
        for h in range(8):
            pb = 4 + (h % 2)
            for g in range(2):
                MM(P[pb][:, g * 128:(g + 1) * 128], nsp[:, g, h * 128:(h + 1) * 128], triN[:], True, True,
                   r=[("nsp", g), "triN"], w=["P%d" % pb])
            ACT(erevT[:, h, :], P[pb][:, 0:TT], AF.Exp, r=["P%d" % pb], w=[("erevT", h)])
            for g in range(2):
                MM(P[6][:, h * 4 + g * 2:h * 4 + g * 2 + 2], nsp[:, g, h * 128:(h + 1) * 128], chunkN[:], True, True,
                   r=[("nsp", g), "chunkN"], w=["P6"])
        ACT(dec[:], P[6][:, 0:32], AF.Exp, r=["P6"], w=["dec"])

    def token_mix(src, mode, ws):
        main = mode == "main"
        load_norm(src, g1c)
        wi = [0]

        def nextw():
            b = ws.get(wi[0])
            wi[0] += 1
            return b

        yv = yT[:].rearrange("p (b t) -> p b t", b=16)

        def conv_taps(blk):
            TS(yv[:, blk, :], uT[:, blk, 0:TT], wdwc[:, blk * 31:blk * 31 + 1], bdc[:, blk:blk + 1], OP.mult, OP.add,
               r=[("uT", blk), "uTh", "cols"], w=[("yT", blk)])
            for j in range(1, 31):
                STT(yv[:, blk, :], uT[:, blk, j:j + TT], wdwc[:, blk * 31 + j:blk * 31 + j + 1], yv[:, blk, :], OP.mult, OP.add,
                    r=[("uT", blk), "uTh", "cols", ("yT", blk)], w=[("yT", blk)])

        if mode != "pre":
            if main:
                CP("act", uT[:, :, 0:30], halo[:], r=["halo"], w=["uTh"])
            for blk in range(16):
                b = nextw()
                pcb, pkey = proj(b, xnT, "xnT")
                ACT(sig[blk % 2][:], pcb[:, 0:TT], AF.Sigmoid, r=[pkey], w=[("sig", blk % 2)])
                b = nextw()
                pca, pkey = proj(b, xnT, "xnT")
                TTo(uT[:, blk, 30:30 + TT], pca[:, 0:TT], sig[blk % 2][:], OP.mult, r=[pkey, ("sig", blk % 2)], w=[("uT", blk)])
            if not main:
                CP("act", halo[:], uT[:, :, TT:TT + 30], r=["uT"], w=["halo"])
        gla_gate()
        for h in range(8):
            b = nextw()
            pk, pkey = proj(b, xnT, "xnT")
            kt = kdT[h % 2]
            TTo(kt[:], pk[:, 0:TT], erevT[:, h, :], OP.mult, r=[pkey, ("erevT", h)], w=[("kdT", h % 2)])
            for g in range(2):
                TR(P3[:, g * 128:(g + 1) * 128], kt[:, g * 128:(g + 1) * 128], identB[:],
                   r=[("kdT", h % 2), "identB"], w=["P3"])
            CP("act", kd[:, :, h, :], P3[:, 0:256].rearrange("p (g n) -> p g n", g=2), r=["P3"], w=[("kd", h)])
            if main:
                conv_taps(2 * h)
            for a in range(2):
                b = nextw()
                pv, pkey = proj(b, xnT, "xnT")
                vtmp = sqb[a]
                CP("act", vtmp[:, 0:TT], pv[:, 0:TT], r=[pkey], w=[("sqb", a)])
                for g in range(2):
                    TR(P2b[:, g * 128:(g + 1) * 128], vtmp[:, g * 128:(g + 1) * 128], identB[:],
                       r=[("sqb", a), "identB"], w=["P2"])
                CP("act", vtok[:, :, (2 * h + a) * 128:(2 * h + a + 1) * 128],
                   P2b[:, 0:256].rearrange("p (g n) -> p g n", g=2), r=["P2"], w=[("vtok", h)])
            if main:
                conv_taps(2 * h + 1)
                b = nextw()
                pq_, pkey = proj(b, xnT, "xnT")
                ACT(qT[:, h, :], pq_[:, 0:TT], AF.Copy, r=[pkey], w=[("qT", h)], scale=128.0 ** -0.5)
                for a in range(2):
                    b = nextw()
                    pr, pkey = proj(b, xnT, "xnT")
                    ACT(srT[:, 2 * h + a, :], pr[:, 0:TT], AF.Silu, r=[pkey], w=[("srT", 2 * h + a)])
            po = P[6 + (h % 2)]
            pokey = "P%d" % (6 + (h % 2))
            for c in range(4):
                g, hf = c // 2, c % 2
                pkv = P[4 + (c % 2)]
                kvkey = "P%d" % (4 + (c % 2))
                MM(pkv[:, 0:256], kd[hf * 64:(hf + 1) * 64, g, h, :], vtok[hf * 64:(hf + 1) * 64, g, h * 256:(h + 1) * 256],
                   True, True, r=[("kd", h), ("vtok", h)], w=[kvkey])
                STT(Sst[:, h, :], Sst[:, h, :], dec[:, h * 4 + c:h * 4 + c + 1], pkv[:, 0:256], OP.mult, OP.add,
                    r=[("Sst", h), "dec", kvkey], w=[("Sst", h)])
                if main:
                    CP("act", Sbf[:, h, :], Sst[:, h, :], r=[("Sst", h)], w=[("Sbf", h)])
                    for a in range(2):
                        MM(po[:, a * 256 + c * 64:a * 256 + (c + 1) * 64], Sbf[:, h, a * 128:(a + 1) * 128],
                           qT[:, h, c * 64:(c + 1) * 64], True, True, r=[("Sbf", h), ("qT", h)], w=[pokey])
            if main:
                ACT(sqb[0][:], po[:, 0:512], AF.Square, r=[pokey], w=[("sqb", 0)])
                MM(P[4][:, 0:TT], onesB[:], sqb[0][:, 0:TT], True, False, r=[("sqb", 0), "onesB"], w=["P4"])
                MM(P[4][:, 0:TT], onesB[:], sqb[0][:, TT:2 * TT], False, True, r=[("sqb", 0), "onesB"], w=["P4"])
                TS(tf[0][:], P[4][:, 0:TT], 1.0 / 256, EPS, OP.mult, OP.add, r=["P4"], w=[("tf", 0)])
                ACT(tf[0][:], tf[0][:], AF.Sqrt, r=[("tf", 0)], w=[("tf", 0)])
                S.op("dve", lambda e: e.reciprocal(out=tf[0][:], in_=tf[0][:]), r=[("tf", 0)], w=[("tf", 0)])
                for a in range(2):
                    blk = 2 * h + a
                    TTo(tf[1 + a][:], po[:, a * 256:(a + 1) * 256], tf[0][:], OP.mult, r=[pokey, ("tf", 0)], w=[("tf", 1 + a)])
                    STT(mixT[:, blk, :], tf[1 + a][:], ggc[:, blk:blk + 1], srT[:, blk, :], OP.mult, OP.mult,
                        r=[("tf", 1 + a), "cols", ("srT", blk)], w=[("mixT", blk)])
        if not main:
            return
        CP("act", halo[:], uT[:, :, TT:TT + 30], r=["uT"], w=["halo"])
        for blk in range(16):
            ACT(sqb[0][:, 0:TT], yv[:, blk, :], AF.Copy, r=[("yT", blk)], w=[("sqb", 0)])
            MM(P[4][:, 0:TT], onesB[:], sqb[0][:, 0:TT], blk == 0, blk == 15, r=[("sqb", 0), "onesB"], w=["P4"])
            ACT(sqb[1][:, 0:TT], yv[:, blk, :], AF.Square, r=[("yT", blk)], w=[("sqb", 1)])
            MM(P[5][:, 0:TT], onesB[:], sqb[1][:, 0:TT], blk == 0, blk == 15, r=[("sqb", 1), "onesB"], w=["P5"])
        TS(tf[0][:], P[4][:, 0:TT], 1.0 / 2048, None, OP.mult, None, r=["P4"], w=[("tf", 0)])
        TTo(tf[1][:], tf[0][:], tf[0][:], OP.mult, r=[("tf", 0)], w=[("tf", 1)])
        STT(tf[1][:], P[5][:, 0:TT], 1.0 / 2048, tf[1][:], OP.mult, OP.subtract, r=["P5", ("tf", 1)], w=[("tf", 1)])
        TS(tf[1][:], tf[1][:], EPS, None, OP.add, None, r=[("tf", 1)], w=[("tf", 1)])
        ACT(tf[1][:], tf[1][:], AF.Sqrt, r=[("tf", 1)], w=[("tf", 1)])
        S.op("dve", lambda e: e.reciprocal(out=tf[1][:], in_=tf[1][:]), r=[("tf", 1)], w=[("tf", 1)])
        for blk in range(16):
            TTo(yv[:, blk, :], yv[:, blk, :], tf[0][:], OP.subtract, r=[("yT", blk), ("tf", 0)], w=[("yT", blk)])
            TTo(yv[:, blk, :], yv[:, blk, :], tf[1][:], OP.mult, r=[("yT", blk), ("tf", 1)], w=[("yT", blk)])
            TS(yv[:, blk, :], yv[:, blk, :], lgc[:, blk:blk + 1], lbc[:, blk:blk + 1], OP.mult, OP.add,
               r=[("yT", blk), "cols"], w=[("yT", blk)])
            ACT(mixT[:, 16 + blk, :], yv[:, blk, :], AF.Silu, r=[("yT", blk)], w=[("mixT", 16 + blk)])

    def out_proj(ws):
        for ob in range(32):
            b = ws.get(ob)
            po_, pkey = proj(b, mixT, "mixT")
            TTo(xT[:, ob, :], po_[:, 0:TT], xT[:, ob, :], OP.add, r=[pkey, ("xT", ob // 4)], w=[("xT", ob // 4)])

    def peer(wsq, wsu, vsrc):
        norm_xT(g2c)
        for blk in range(16):
            b = wsq.get(blk)
            pp, pkey = proj(b, xnT, "xnT")
            ACT(pqT[:, blk, :], pp[:, 0:TT], AF.Copy, r=[pkey], w=[("pqT", blk)])
        for g in range(2):
            for quad in range(4):
                for i in range(4):
                    blk = quad * 4 + i
                    MM(P[2][:, i * 128:(i + 1) * 128], pqT[:, blk, g * 128:(g + 1) * 128], keysT[:, blk, :], True, True,
                       r=[("pqT", blk), "keysT"], w=["P2"])
                CP(cp_eng(), sall[:, g, quad * 4:(quad + 1) * 4, :], P[2][:, 0:512].rearrange("p (a n) -> p a n", a=4),
                   r=["P2"], w=[("sall", g)])
        def topk_chain(g, h):
            v1, v2, c24, cand, wk, sm, d16 = v1s[g], v2s[g], c24s[g], cands[g], wks[g], sms[g], d16s[g]
            ta, tb = gt[g], ga[g]
            K = lambda n: (n, g)
            s1 = sall[:, g, 2 * h, :]
            s2 = sall[:, g, 2 * h + 1, :]
            for (s_, v_, vk) in ((s1, v1, K("v1")), (s2, v2, K("v2"))):
                S.op("dve", lambda e, s_=s_, v_=v_: e.max(out=v_[:, 0:8], in_=s_), r=[("sall", g)], w=[vk])
                yield
                S.op("dve", lambda e, s_=s_, v_=v_: e.match_replace(out=wk[:, 0:128], in_to_replace=v_[:, 0:8], in_values=s_,
                                                                    imm_value=-1e30), r=[("sall", g), vk], w=[K("wk")])
                yield
                S.op("dve", lambda e, v_=v_: e.max(out=v_[:, 8:16], in_=wk[:, 0:128]), r=[K("wk")], w=[vk])
                yield
            TTo(cand[:].rearrange("p (i j) -> p i j", i=16), v1[:].unsqueeze(2).to_broadcast([128, 16, 16]),
                v2[:].unsqueeze(1).to_broadcast([128, 16, 16]), OP.add, r=[K("v1"), K("v2")], w=[K("cand")])
            yield
            S.op("dve", lambda e: e.max(out=c24[:, 0:8], in_=cand[:]), r=[K("cand")], w=[K("c24")])
            yield
            S.op("dve", lambda e: e.match_replace(out=wk[:], in_to_replace=c24[:, 0:8], in_values=cand[:], imm_value=-1e30),
                 r=[K("cand"), K("c24")], w=[K("wk")])
            yield
            S.op("dve", lambda e: e.max(out=c24[:, 8:16], in_=wk[:]), r=[K("wk")], w=[K("c24")])
            yield
            S.op("dve", lambda e: e.match_replace(out=cand[:], in_to_replace=c24[:, 8:16], in_values=wk[:], imm_value=-1e30),
                 r=[K("wk"), K("c24")], w=[K("cand")])
            yield
            S.op("dve", lambda e: e.max(out=c24[:, 16:24], in_=cand[:]), r=[K("cand")], w=[K("c24")])
            yield
            TS(sm[:, 0:1], c24[:, 15:16], c24[:, 16:17], 0.5, OP.add, OP.mult, r=[K("c24")], w=[K("sm")])
            yield
            TS(d16[:], c24[:, 0:16], c24[:, 0:1], None, OP.subtract, None, r=[K("c24")], w=[K("d16")])
            yield
            ACT(d16[:], d16[:], AF.Exp, r=[K("d16")], w=[K("d16"), K("smz")], accum_out=sm[:, 1:2])
            yield
            S.op("dve", lambda e: e.reciprocal(out=sm[:, 2:3], in_=sm[:, 1:2]), r=[K("smz"), K("sm")], w=[K("sm")])
            yield
            TS(ta[:], s1, v1[:, 0:1], None, OP.subtract, None, r=[("sall", g), K("v1")], w=[("gt", g)])
            yield
            ACT(ta[:], ta[:], AF.Exp, r=[("gt", g)], w=[("gt", g)])
            yield
            TS(tb[:], s2, v2[:, 0:1], None, OP.subtract, None, r=[("sall", g), K("v2")], w=[("ga", g)])
            yield
            ACT(tb[:], tb[:], AF.Exp, r=[("ga", g)], w=[("ga", g)])
            yield
            STT(ta[:], s1, v1[:, 15:16], ta[:], OP.is_ge, OP.mult, r=[("sall", g), K("v1"), ("gt", g)], w=[("gt", g)])
            yield
            TS(E1[:, g, h, :], ta[:], sm[:, 2:3], None, OP.mult, None, r=[("gt", g), K("sm")], w=[("E1", g)])
            yield
            STT(E2[:, g, h, :], s2, v2[:, 15:16], tb[:], OP.is_ge, OP.mult, r=[("sall", g), K("v2"), ("ga", g)], w=[("E2", g)])
            yield
            TS(TH[:, g, h, :], s1, -1.0, sm[:, 0:1], OP.mult, OP.add, r=[("sall", g), K("sm")], w=[("TH", g)])
            yield

        for h in range(8):
            chains = [topk_chain(0, h), topk_chain(1, h)]
            while chains:
                for c in list(chains):
                    try:
                        next(c)
                    except StopIteration:
                        chains.remove(c)

        vctr = [0]
        wring = [0]

        def a_mms(n1):
            b = wsu.get(n1)
            bank = n1 % 2
            return [lambda kc=kc, b=b, bank=bank: MM(P[bank][:, 0:TT], wt[b][:, kc, :], xnT[:, kc, :], kc == 0, kc == KC - 1,
                                                     r=[("wt", b), "xnT"], w=["P%d" % bank]) for kc in range(KC)]

        def stage_G(n1):
            pe_ops = []
            sbuf_, skey = GSLOT[n1 % 2]
            for g in range(2):
                for h in range(8):
                    i = wring[0]
                    wring[0] += 1
                    gb, wb = gtr[i % 4], Wb[i % 16]
                    STT(gb[:], sall[:, g, 2 * h + 1, :], TH[:, g, h, n1:n1 + 1], E2[:, g, h, :], OP.is_ge, OP.mult,
                        r=[("sall", g), ("TH", g), ("E2", g)], w=[("gtr", i % 4)])
                    ACT(wb[:], gb[:], AF.Copy, r=[("gtr", i % 4), ("E1", g)], w=[("Wb", i % 16)], scale=E1[:, g, h, n1:n1 + 1])
                    pe_ops.append(lambda g=g, h=h, wb=wb, i=i, sbuf_=sbuf_, skey=skey: MM(
                        sbuf_[:, g * 128:(g + 1) * 128], wb[:], identB[:], h == 0, h == 7,
                        r=[("Wb", i % 16), "identB"], w=[skey]))
            return pe_ops

        def stage_T(n1):
            eg, eb = n1 // 8, n1 % 8
            hb = eg % 2
            sbuf_, skey = GSLOT[n1 % 2]
            TTo(HT[hb][:, eb, :], gl[n1 % 2][:], sbuf_[:, 0:256], OP.mult, r=[("gl", n1 % 2), skey], w=[("HT", hb)])

        def v_chunk(eg, ds):
            hb = eg % 2
            vb = vctr[0] % 2
            vctr[0] += 1
            DMA("sp", vt[vb][:].rearrange("p e d -> p (e d)"), vrb[eg * 8 + ds], r=[("vrb", (eg * 8 + ds) // 16)],
                w=[("vt", vb)])
            for dq in range(4):
                db = ds * 4 + dq
                for e8 in range(8):
                    MM(P[4 + dq][:, 0:TT], vt[vb][:, e8, dq * 128:(dq + 1) * 128], HT[hb][:, e8, :], e8 == 0, e8 == 7,
                       r=[("vt", vb), ("HT", hb)], w=["P%d" % (4 + dq)])
                TTo(xT[:, db, :], P[4 + dq][:, 0:TT], xT[:, db, :], OP.add, r=["P%d" % (4 + dq), ("xT", db // 4)], w=[("xT", db // 4)])

        pend = []
        for n1 in range(129):
            if n1 < 128:
                k = 0
                for j, f in enumerate(a_mms(n1)):
                    f()
                    if j % 2 == 1 and k < len(pend):
                        pend[k]()
                        k += 1
                while k < len(pend):
                    pend[k]()
                    k += 1
                ACT(gl[n1 % 2][:], P[n1 % 2][:, 0:TT], AF.Gelu, r=["P%d" % (n1 % 2)], w=[("gl", n1 % 2)])
                new_pend = stage_G(n1)
            else:
                for f in pend:
                    f()
                new_pend = []
            if n1 >= 1:
                m = n1 - 1
                stage_T(m)
                if m >= 8:
                    v_chunk(m // 8 - 1, m % 8)
            pend = new_pend
        for ds in range(8):
            v_chunk(15, ds)

    def final_out(dst):
        fm_rstd(D, KC, lambda kc: xT[:, kc, :], lambda kc: ("xT", kc // 4))
        for kc in range(KC):
            STT(xT[:, kc, :], xT[:, kc, :], gfc[:, kc:kc + 1], rstd[:], OP.mult, OP.mult,
                r=[("xT", kc // 4), "cols", "rstd"], w=[("xT", kc // 4)])
        for g in range(2):
            ob_, ok_ = (xin, "xin") if g == 0 else (xinB, "xinB")
            for kq in range(8):
                for i in range(4):
                    kc = kq * 4 + i
                    TR(P[2][:, i * 128:(i + 1) * 128], xT[:, kc, g * 128:(g + 1) * 128], identF[:],
                       r=[("xT", kq), "identF"], w=["P2"])
                CP(cp_eng(), ob_[:, kq * 512:(kq + 1) * 512], P[2][:, 0:512], r=["P2"], w=[ok_])
            DMA("sp", dst[g * 128:(g + 1) * 128, :], ob_[:], r=[ok_], w=["y"])

    def win_srcs(mode):
        idx = []
        if mode != "pre":
            idx += [48 + i for i in range(32)]
        for h in range(8):
            idx += [h * 6 + i for i in (range(6) if mode == "main" else range(3))]
        return [(winb[i], ("winb", i // 16)) for i in idx]

    for t in range(n_pre):
        mode = "pre_last" if t == n_pre - 1 else "pre"
        token_mix(xp[t * TT:(t + 1) * TT, :], mode, WStream(win_srcs(mode)))
        if late_conv and t < n_pre - 1:
            late_conv.pop(0)(["dec"])
        if t == n_pre - 1:
            while late_conv:
                late_conv.pop(0)(["dec"])
            S.barrier()
    for t in range(n_main):
        token_mix(xm[t * TT:(t + 1) * TT, :], "main", WStream(win_srcs("main")))
        out_proj(WStream([(woutb[i], ("woutb", i // 16)) for i in range(32)]))
        S.barrier()
        peer(WStream([(wqb[i], ("wqb", i // 16)) for i in range(16)]),
             WStream([(utb[i], ("utb", i // 16)) for i in range(128)]), vrd)
        final_out(y[t * TT:(t + 1) * TT, :])
        S.barrier()
    S.finish()
    S.run()
    return nc


def _blk(wm):
    k, n = wm.shape
    return np.ascontiguousarray(wm.reshape(k // 128, 128, n // 128, 128).transpose(2, 1, 0, 3)).reshape(n // 128, 128, k)


def _consts():
    c = np.zeros((128, 258), np.float32)
    c[:, 0:128] = np.eye(128, dtype=np.float32)
    t = np.arange(128)
    c[:, 128:256] = np.where((t[:, None] > t[None, :]) & ((t[:, None] // 64) == (t[None, :] // 64)), -1.0 / 16.0, 0.0)
    c[:, 256] = np.where(t < 64, -1.0 / 16.0, 0.0)
    c[:, 257] = np.where(t >= 64, -1.0 / 16.0, 0.0)
    return c


def prep_weights(w_in, w_gate_up, b_gate, gla_norm_g, w_dw, b_dw, conv_ln_g, conv_ln_b, w_out, norm1_g, norm2_g,
                 peer_wq, peer_keys1, peer_keys2, peer_u, peer_v, final_norm_g):
    f = lambda a: np.asarray(a, dtype=np.float32)
    wi = f(w_in)[0]
    q, k, v, r = wi[:, 0:1024], wi[:, 1024:2048], wi[:, 2048:4096], wi[:, 4096:6144]
    gd, ca, cb = wi[:, 6144:6160], wi[:, 6160:8208], wi[:, 8208:10256]
    qb, kb, vb, rb, cab, cbb = _blk(q), _blk(k), _blk(v), _blk(r), _blk(ca), _blk(cb)
    blocks = []
    for h in range(8):
        blocks += [kb[h], vb[2 * h], vb[2 * h + 1], qb[h], rb[2 * h], rb[2 * h + 1]]
    for b in range(16):
        blocks += [cbb[b], cab[b]]
    win = np.stack(blocks)
    wgd = np.ascontiguousarray(gd.reshape(32, 128, 16).transpose(1, 0, 2)).reshape(128, 512)
    wout = _blk(f(w_out)[0])
    wq = _blk(f(peer_wq)[0])
    ut = np.ascontiguousarray(f(peer_u)[0].reshape(128, 128, 32, 128).transpose(0, 3, 2, 1)).reshape(128, 128, 4096)
    vr = np.ascontiguousarray(f(peer_v)[0].reshape(16, 8, 128, 8, 512).transpose(0, 3, 2, 1, 4)).reshape(128, 128, 4096)
    k1, k2 = f(peer_keys1)[0], f(peer_keys2)[0]
    keyst = np.zeros((128, 16, 128), np.float32)
    for h in range(8):
        keyst[:, 2 * h, :] = k1[h].T
        keyst[:, 2 * h + 1, :] = k2[h].T
    keyst = keyst.reshape(128, 2048)
    wga = np.concatenate([f(w_gate_up)[0], f(b_gate)[0][None, :]], axis=0)
    colv = lambda a, n: np.ascontiguousarray(f(a).reshape(n, 128).T)
    cols = np.concatenate([
        colv(norm1_g[0], 32), colv(norm2_g[0], 32), colv(final_norm_g, 32),
        colv(gla_norm_g[0], 16), colv(conv_ln_g[0], 16), colv(conv_ln_b[0], 16), colv(b_dw[0], 16),
        np.ascontiguousarray(f(w_dw)[0].reshape(31, 16, 128).transpose(2, 1, 0)).reshape(128, 496),
    ], axis=1)
    assert cols.shape == (128, NCOLS)
    return dict(win=win, wgd=wgd, wout=wout, wq=wq, ut=ut, vr=vr, keyst=keyst, wga=wga,
                cols=np.ascontiguousarray(cols), consts=_consts())


def make_core_inputs(xb, meta, start, n_main, n_pre):
    xm = np.ascontiguousarray(xb[start:start + n_main * TT])
    xp = np.zeros((n_pre * TT, D), np.float32)
    pre = np.concatenate([meta, xb[:start]], axis=0)
    assert pre.shape[0] <= n_pre * TT
    xp[n_pre * TT - pre.shape[0]:] = pre
    return xm, xp


_NC_CACHE = {}


def kernel(x, meta_tokens, norm1_g, w_in, w_gate_up, b_gate, gla_norm_g, w_dw, b_dw, conv_ln_g, conv_ln_b,
           w_out, norm2_g, peer_wq, peer_keys1, peer_keys2, peer_u, peer_v, final_norm_g):
    x = np.asarray(x, dtype=np.float32)
    meta = np.asarray(meta_tokens, dtype=np.float32)
    wts = prep_weights(w_in, w_gate_up, b_gate, gla_norm_g, w_dw, b_dw, conv_ln_g, conv_ln_b, w_out, norm1_g,
                       norm2_g, peer_wq, peer_keys1, peer_keys2, peer_u, peer_v, final_norm_g)
    B, L, _ = x.shape
    per = N_MAIN_TILES * TT
    in_maps = []
    for c in range(8):
        b, j = c // 4, c % 4
        xm, xp = make_core_inputs(x[b], meta, j * per, N_MAIN_TILES, N_PRE_TILES)
        m = dict(wts)
        m["xm"] = xm
        m["xp"] = xp
        in_maps.append(m)
    if "nc" not in _NC_CACHE:
        _NC_CACHE["nc"] = build_nc()
    res = run_bass_kernel_spmd(_NC_CACHE["nc"], in_maps, core_ids=list(range(8)))
    out = np.empty((B, L, D), np.float32)
    for c in range(8):
        b, j = c // 4, c % 4
        out[b, j * per:(j + 1) * per] = res.results[c]["y"]
    return out
```

```python
import contextlib
import numpy as np
import concourse.bass as bass
import concourse.mybir as mybir
from concourse.bass_utils import run_bass_kernel_spmd

F32 = mybir.dt.float32
BF16 = mybir.dt.bfloat16
AF = mybir.ActivationFunctionType
OP = mybir.AluOpType

D = 4096
KC = 32
TT = 256
EPS = 1e-6
N_MAIN_TILES = 8
N_PRE_TILES = 25
NCOLS = 32 * 3 + 16 * 4 + 16 * 31


class Sched:
    ENGS = ("pe", "act", "dve", "pool", "sp")

    def __init__(self, nc, same_engine_sync=True):
        self.nc = nc
        self.ops = []
        self.state = {}
        self.same_engine_sync = same_engine_sync
        self.n_dma_sems = {"pool": 28, "sp": 24, "act": 2}
        self.unbarriered_dma = []
        self.nosync = ("pe",)
        self.persist = set()
        self.excl = set("P%d" % i for i in range(8))

    def _entries(self, key):
        base, idx = key if isinstance(key, tuple) else (key, None)
        st = self.state.setdefault(base, {"*": [None, []]})
        if idx is None:
            return list(st.values())
        if idx not in st:
            st[idx] = [st["*"][0], list(st["*"][1])]
        return [st[idx]]

    def _add(self, eng, fn, r, w, dma):
        xr = tuple(k for k in r if (k[0] if isinstance(k, tuple) else k) in self.excl)
        if xr:
            r = tuple(k for k in r if k not in xr)
            w = tuple(w) + xr
        i = len(self.ops)
        deps = set()
        for key in r:
            for ent in self._entries(key):
                if ent[0] is not None:
                    deps.add(ent[0])
        for key in w:
            for ent in self._entries(key):
                if ent[0] is not None:
                    deps.add(ent[0])
                deps.update(ent[1])
        for key in r:
            for ent in self._entries(key):
                if not dma:
                    ent[1][:] = [j for j in ent[1] if self.ops[j]["dma"] or self.ops[j]["eng"] != eng]
                ent[1].append(i)
        for key in w:
            for ent in self._entries(key):
                ent[0] = i
                ent[1] = []
        deps.discard(i)
        pers = dma and any((k[0] if isinstance(k, tuple) else k) in self.persist for k in w)
        self.ops.append(dict(eng=eng, fn=fn, deps=deps, dma=dma, needs_inc=False, pers=pers))
        if dma and not pers:
            self.unbarriered_dma.append(i)
        return i

    def op(self, eng, fn, r=(), w=(), big=False):
        i = self._add(eng, fn, tuple(r), tuple(w), False)
        self.ops[i]["big"] = big
        return i

    def dma(self, eng, fn, r=(), w=()):
        return self._add(eng, fn, tuple(r), tuple(w), True)

    def barrier(self):
        last = {}
        for i, o in enumerate(self.ops):
            if o.get("barrier") or o["dma"]:
                continue
            last[o["eng"]] = i
        deps = set(last.values()) | set(self.unbarriered_dma)
        self.unbarriered_dma = []
        self.state = {k: v for k, v in self.state.items() if k in self.persist}
        self.ops.append(dict(eng=None, fn=None, deps=deps, dma=False, needs_inc=False, barrier=True))

    def finish(self):
        self.barrier()
        self.ops.append(dict(eng="sp", fn=None, deps=set(), dma=False, needs_inc=False))

    def plan(self):
        ops = self.ops
        pending = {e: set() for e in self.ENGS}
        for o in ops:
            if o.get("barrier"):
                for e in self.ENGS:
                    pending[e] |= o["deps"]
            else:
                e = o["eng"]
                if pending[e]:
                    o["deps"] = set(o["deps"]) | pending[e]
                    pending[e] = set()
        real = [(i, o) for i, o in enumerate(ops) if not o.get("barrier")]
        dma_ctr = {e: 0 for e in self.ENGS}
        dma_prev = {}
        for i, o in real:
            if o["dma"]:
                e = o["eng"]
                if o.get("pers"):
                    e = "cv"
                    slot = dma_ctr.get("cv", 0)
                    dma_ctr["cv"] = slot + 1
                else:
                    slot = dma_ctr[e] % self.n_dma_sems[e]
                    dma_ctr[e] += 1
                o["dslot"] = (e, slot)
                prev = dma_prev.get((e, slot))
                o["dprev"] = prev
                o["dtarget"] = (ops[prev]["dtarget"] + 16) if prev is not None else 16
                dma_prev[(e, slot)] = i

        def skip_same(p, e):
            return p["eng"] == e and (e in self.nosync or (e == "dve" and p.get("big")) or not self.same_engine_sync)

        for i, o in real:
            for d in o["deps"]:
                p = ops[d]
                if p["dma"] or skip_same(p, o["eng"]):
                    continue
                p["needs_inc"] = True
        cnt = {e: 0 for e in self.ENGS}
        for i, o in real:
            if (not o["dma"]) and o["needs_inc"]:
                cnt[o["eng"]] += 1
                o["count"] = cnt[o["eng"]]
        seen = {e: {f: 0 for f in self.ENGS} for e in self.ENGS}
        seen_dma = {e: {} for e in self.ENGS}
        for i, o in real:
            e = o["eng"]
            best = {}
            cands = []
            if o["dma"] and o["dprev"] is not None:
                cands.append(o["dprev"])
            cands.extend(o["deps"])
            for d in cands:
                p = ops[d]
                if p["dma"]:
                    key = p["dslot"]
                    if seen_dma[e].get(key, 0) < p["dtarget"]:
                        seen_dma[e][key] = p["dtarget"]
                        best[("dma", key)] = max(best.get(("dma", key), 0), p["dtarget"])
                else:
                    if skip_same(p, e):
                        continue
                    if seen[e][p["eng"]] < p["count"]:
                        seen[e][p["eng"]] = p["count"]
                        best[("eng", p["eng"])] = max(best.get(("eng", p["eng"]), 0), p["count"])
            o["waits"] = [(k[0], k[1], v) for k, v in best.items()]
        self.cnt = cnt
        return real

    def run(self):
        nc = self.nc
        real = self.plan()
        with contextlib.ExitStack() as es:
            esem = {e: es.enter_context(nc.semaphore("s_" + e)) for e in self.ENGS}
            dsem = {}
            for key in sorted(set(o["dslot"] for i, o in real if o["dma"])):
                dsem[key] = es.enter_context(nc.semaphore("d_%s_%d" % key))
            block = es.enter_context(nc.Block())
            per_eng = {e: [] for e in self.ENGS}
            for i, o in real:
                per_eng[o["eng"]].append(o)

            def emit_engine(ename, eng):
                for o in per_eng[ename]:
                    for kind, key, val in o["waits"]:
                        eng.wait_ge(dsem[key] if kind == "dma" else esem[key], val)
                    if o["fn"] is None:
                        continue
                    ins = o["fn"](eng)
                    if o["dma"]:
                        ins.then_inc(dsem[o["dslot"]], 16)
                    elif o["needs_inc"]:
                        ins.then_inc(esem[ename], 1)

            @block.tensor
            def _(eng):
                emit_engine("pe", eng)

            @block.scalar
            def _(eng):
                emit_engine("act", eng)

            @block.vector
            def _(eng):
                emit_engine("dve", eng)

            @block.gpsimd
            def _(eng):
                emit_engine("pool", eng)

            @block.sync
            def _(eng):
                emit_engine("sp", eng)


def build_nc(n_main=N_MAIN_TILES, n_pre=N_PRE_TILES):
    nc = bass.Bass("TRN2", target_bir_lowering=False)
    S = Sched(nc)

    def din(name, shape):
        return nc.dram_tensor(name, shape, F32, kind="ExternalInput").ap()

    xm = din("xm", [n_main * TT, D])
    xp = din("xp", [n_pre * TT, D])
    win = din("win", [80, 128, D])
    wgd = din("wgd", [128, KC * 16])
    wout = din("wout", [32, 128, D])
    wqd = din("wq", [16, 128, D])
    utd = din("ut", [128, 128, D])
    vrd = din("vr", [128, 128, D])
    keyd = din("keyst", [128, 16 * 128])
    wgad = din("wga", [17, 1024])
    colsd = din("cols", [128, NCOLS])
    constd = din("consts", [128, 258])
    y = nc.dram_tensor("y", [n_main * TT, D], F32, kind="ExternalOutput").ap()
    winb = nc.dram_tensor("winb", [80, 128, D], BF16).ap()
    woutb = nc.dram_tensor("woutb", [32, 128, D], BF16).ap()
    wqb = nc.dram_tensor("wqb", [16, 128, D], BF16).ap()
    utb = nc.dram_tensor("utb", [128, 128, D], BF16).ap()
    vrb = nc.dram_tensor("vrb", [128, 128, D], BF16).ap()
    S.persist.update(["winb", "woutb", "wqb", "utb", "vrb"])

    off = [16512]

    def sb(name, shape, dt, at=None):
        nb = int(np.prod(shape[1:])) * (4 if dt == F32 else 2)
        nb = (nb + 63) // 64 * 64
        if at is None:
            t = nc.alloc_sbuf_tensor_at(name, shape, dt, offset=off[0])
            off[0] += nb
        else:
            t = nc.alloc_sbuf_tensor_at(name, shape, dt, offset=at[0])
            at[0] += nb
        return t

    identF = sb("identF", [128, 128], F32)
    triN = sb("triN", [128, 128], F32)
    chunkN = sb("chunkN", [128, 2], F32)
    identB = sb("identB", [128, 128], BF16)
    onesB = sb("onesB", [128, 128], BF16)
    cols = sb("cols", [128, NCOLS], F32)
    keysT = sb("keysT", [128, 16, 128], BF16)
    wgdT = sb("wgdT", [128, KC, 16], BF16)
    wgA = sb("wgA", [32, 1024], F32)
    gdA = sb("gdA", [32, TT], F32)
    Sst = sb("Sst", [128, 8, 256], F32)
    Sbf = sb("Sbf", [128, 8, 256], BF16)
    halo = sb("halo", [128, 16, 30], F32)
    xT = sb("xT", [128, KC, TT], F32)
    xnT = sb("xnT", [128, KC, TT], BF16)
    wt = [sb("wt%d" % i, [128, KC, 128], BF16) for i in range(3)]
    xin = sb("xin", [128, D], F32)
    rstd = sb("rstd", [128, TT], F32)
    sqb = [sb("sqb%d" % i, [128, 2 * TT], BF16) for i in range(2)]
    region = off[0]
    a1 = [region]
    xinB = nc.alloc_sbuf_tensor_at("xinB", [128, D], F32, offset=region)
    mixT = sb("mixT", [128, KC, TT], BF16, a1)
    nsp = sb("nsp", [128, 2, 1024], F32, a1)
    erevT = sb("erevT", [128, 8, TT], F32, a1)
    dec = sb("dec", [128, 32], F32, a1)
    qT = sb("qT", [128, 8, TT], BF16, a1)
    kdT = [sb("kdT%d" % i, [128, TT], BF16, a1) for i in range(2)]
    kd = sb("kd", [128, 2, 8, 128], BF16, a1)
    vtok = sb("vtok", [128, 2, 2048], BF16, a1)
    srT = sb("srT", [128, 16, TT], BF16, a1)
    uT = sb("uT", [128, 16, TT + 30], F32, a1)
    sig = [sb("sig%d" % i, [128, TT], F32, a1) for i in range(2)]
    tf = [sb("tf%d" % i, [128, TT], F32, a1) for i in range(3)]
    end1 = a1[0]
    a2 = [region]
    pqT = sb("pqT", [128, 16, TT], BF16, a2)
    sall = sb("sall", [128, 2, 16, 128], F32, a2)
    E2 = sb("E2", [128, 2, 8, 128], F32, a2)
    TH = sb("TH", [128, 2, 8, 128], F32, a2)
    E1 = sb("E1", [128, 2, 8, 128], F32, a2)
    vt = [sb("vt%d" % i, [128, 8, 512], BF16, a2) for i in range(2)]
    HT = [sb("HT%d" % i, [128, 8, TT], BF16, a2) for i in range(2)]
    gl = [sb("gl%d" % i, [128, TT], F32, a2) for i in range(2)]
    Wb = [sb("Wb%d" % i, [128, 128], BF16, a2) for i in range(16)]
    gtr = [sb("gtr%d" % i, [128, 128], F32, a2) for i in range(4)]
    gt = [sb("gt%d" % i, [128, 128], F32, a2) for i in range(2)]
    ga = [sb("ga%d" % i, [128, 128], F32, a2) for i in range(2)]
    v1s = [sb("v1_%d" % i, [128, 16], F32, a2) for i in range(2)]
    v2s = [sb("v2_%d" % i, [128, 16], F32, a2) for i in range(2)]
    c24s = [sb("c24_%d" % i, [128, 24], F32, a2) for i in range(2)]
    cands = [sb("cand%d" % i, [128, 256], F32, a2) for i in range(2)]
    wks = [sb("wk%d" % i, [128, 256], F32, a2) for i in range(2)]
    sms = [sb("sm%d" % i, [128, 16], F32, a2) for i in range(2)]
    d16s = [sb("d16_%d" % i, [128, 16], F32, a2) for i in range(2)]

    end2 = a2[0]
    assert max(end1, end2) <= 229344, (end1, end2)
    yT = xin

    P = [nc.alloc_psum_tensor("P%d" % i, [128, 512], F32) for i in range(3)]
    P3 = nc.alloc_psum_tensor("P3", [128, 1024], BF16)
    P += [None] + [nc.alloc_psum_tensor("P%d" % i, [128, 512], F32) for i in range(4, 8)]
    P2b = P[2][:].bitcast(BF16)
    TSLOT = [(P3, "P3"), (P2b, "P2")]
    GSLOT = [(P3[:].bitcast(F32), "P3"), (P[2], "P2")]

    g1c = cols[:, 0:32]
    g2c = cols[:, 32:64]
    gfc = cols[:, 64:96]
    ggc = cols[:, 96:112]
    lgc = cols[:, 112:128]
    lbc = cols[:, 128:144]
    bdc = cols[:, 144:160]
    wdwc = cols[:, 160:160 + 496]

    def MM(out, lhsT, rhs, start, stop, r, w):
        S.op("pe", lambda e: e.matmul(out, lhsT=lhsT, rhs=rhs, start=start, stop=stop), r=r, w=w)

    def TR(out, in_, ident, r, w):
        S.op("pe", lambda e: e.transpose(out=out, in_=in_, identity=ident), r=r, w=w)

    def ACT(out, in_, func, r, w, **kw):
        S.op("act", lambda e: e.activation(out=out, in_=in_, func=func, **kw), r=r, w=w)

    def isbig(ap):
        return int(np.prod(ap.shape[1:])) >= 128

    def TS(out, in0, s1, s2, op0, op1, r, w, eng="dve"):
        if s2 is None:
            S.op(eng, lambda e: e.tensor_scalar(out=out, in0=in0, scalar1=s1, scalar2=None, op0=op0), r=r, w=w, big=isbig(out))
        else:
            S.op(eng, lambda e: e.tensor_scalar(out=out, in0=in0, scalar1=s1, scalar2=s2, op0=op0, op1=op1), r=r, w=w, big=isbig(out))

    def TTo(out, in0, in1, op, r, w, eng="dve"):
        S.op(eng, lambda e: e.tensor_tensor(out=out, in0=in0, in1=in1, op=op), r=r, w=w, big=isbig(out))

    def STT(out, in0, scalar, in1, op0, op1, r, w):
        S.op("dve", lambda e: e.scalar_tensor_tensor(out=out, in0=in0, scalar=scalar, in1=in1, op0=op0, op1=op1), r=r, w=w,
             big=isbig(out))

    def CP(eng, out, in_, r, w):
        if eng == "act":
            S.op("act", lambda e: e.activation(out=out, in_=in_, func=AF.Copy), r=r, w=w)
        else:
            S.op(eng, lambda e: e.tensor_copy(out=out, in_=in_), r=r, w=w)

    def DMA(eng, out, in_, r, w):
        S.dma(eng, lambda e: e.dma_start(out=out, in_=in_), r=r, w=w)

    cp_rr = [0]

    def cp_eng():
        cp_rr[0] += 1
        return "act" if cp_rr[0] % 2 else "dve"

    wctr = [0]

    class WStream:
        def __init__(self, srcs, depth=2):
            self.srcs = srcs
            self.issued = 0
            self.base = wctr[0]
            self.depth = depth
            wctr[0] += len(srcs)

        def get(self, i):
            while self.issued < min(len(self.srcs), i + 1 + self.depth):
                b = (self.base + self.issued) % 3
                src, skey = self.srcs[self.issued]
                DMA("sp", wt[b][:].rearrange("p k c -> p (k c)"), src, r=[skey], w=[("wt", b)])
                self.issued += 1
            return (self.base + i) % 3

    pj = [0]

    def proj(b, rhsT, rkey, ncols=128):
        bank = pj[0] % 2
        pj[0] += 1
        for kc in range(KC):
            MM(P[bank][0:ncols, 0:TT], wt[b][:, kc, 0:ncols], rhsT[:, kc, :], kc == 0, kc == KC - 1,
               r=[("wt", b), rkey], w=["P%d" % bank])
        return P[bank], "P%d" % bank

    def fm_rstd(dim, nblk, src_of, src_key_of, pbank=4):
        for kc in range(nblk):
            sq = sqb[kc % 2]
            ACT(sq[:, 0:TT], src_of(kc), AF.Square, r=[src_key_of(kc)], w=[("sqb", kc % 2)])
            MM(P[pbank][:, 0:TT], onesB[:], sq[:, 0:TT], kc == 0, kc == nblk - 1,
               r=[("sqb", kc % 2), "onesB"], w=["P%d" % pbank])
        TS(rstd[:], P[pbank][:, 0:TT], 1.0 / dim, EPS, OP.mult, OP.add, r=["P%d" % pbank], w=["rstd"])
        ACT(rstd[:], rstd[:], AF.Sqrt, r=["rstd"], w=["rstd"])
        S.op("dve", lambda e: e.reciprocal(out=rstd[:], in_=rstd[:]), r=["rstd"], w=["rstd"])

    DMA("sp", identF[:], constd[:, 0:128], r=[], w=["identF"])
    DMA("sp", triN[:], constd[:, 128:256], r=[], w=["triN"])
    DMA("sp", chunkN[:], constd[:, 256:258], r=[], w=["chunkN"])
    DMA("sp", cols[:], colsd, r=[], w=["cols"])
    DMA("sp", wgA[0:17, :], wgad, r=[], w=["wgA"])
    DMA("pool", keysT[:].rearrange("p (a b) n -> p a (b n)", a=2), keyd.rearrange("p (a b) -> p a b", a=2), r=[], w=["keysT"])
    DMA("pool", wgdT[:].rearrange("p k c -> p (k c)"), wgd, r=[], w=["wgdT"])
    CP("act", identB[:], identF[:], r=["identF"], w=["identB"])
    S.op("dve", lambda e: e.memset(onesB[:], 1.0), w=["onesB"])
    S.op("dve", lambda e: e.memset(gdA[:], 1.0), w=["gdA"])
    S.op("dve", lambda e: e.memset(Sst[:].rearrange("p h v -> p (h v)"), 0.0), w=["Sst"])
    S.op("dve", lambda e: e.memset(halo[:].rearrange("p b j -> p (b j)"), 0.0), w=["halo"])

    def convert_ops(src3, dst3, nblk, key):
        ops = []
        for c0 in range(0, nblk, 16):
            n = min(16, nblk - c0)
            ops.append(lambda rr, c0=c0, n=n: DMA("pool", dst3[c0:c0 + n].rearrange("b p (a e) -> (b p) a e", a=2),
                                                  src3[c0:c0 + n].rearrange("b p (a e) -> (b p) a e", a=2), r=rr, w=[(key, c0 // 16)]))
        return ops

    for f in convert_ops(win, winb, 80, "winb"):
        f([])
    late_conv = (convert_ops(wout, woutb, 32, "woutb") + convert_ops(wqd, wqb, 16, "wqb") +
                 convert_ops(utd, utb, 128, "utb") + convert_ops(vrd, vrb, 128, "vrb"))

    def load_norm(src, gcol):
        for g in range(2):
            xb_, xk_ = (xin, "xin") if g == 0 else (xinB, "xinB")
            DMA("sp", xb_[:], src[g * 128:(g + 1) * 128, :], r=[], w=[xk_])
            for kq in range(8):
                for i in range(4):
                    kc = kq * 4 + i
                    TR(P[2][:, i * 128:(i + 1) * 128], xb_[:, kc * 128:(kc + 1) * 128], identF[:],
                       r=[xk_, "identF"], w=["P2"])
                CP(cp_eng(), xT[:, kq * 4:(kq + 1) * 4, g * 128:(g + 1) * 128],
                   P[2][:, 0:512].rearrange("p (a t) -> p a t", a=4), r=["P2"], w=[("xT", kq)])
        norm_xT(gcol)

    def norm_xT(gcol):
        fm_rstd(D, KC, lambda kc: xT[:, kc, :], lambda kc: ("xT", kc // 4))
        for kc in range(KC):
            STT(xnT[:, kc, :], xT[:, kc, :], gcol[:, kc:kc + 1], rstd[:], OP.mult, OP.mult,
                r=[("xT", kc // 4), "cols", "rstd"], w=[("xnT", kc)])

    def gla_gate():
        for kc in range(KC):
            MM(P[0][0:16, 0:TT], wgdT[:, kc, :], xnT[:, kc, :], kc == 0, kc == KC - 1,
               r=["wgdT", ("xnT", kc)], w=["P0"])
        CP("act", gdA[0:16, :], P[0][0:16, 0:TT], r=["P0"], w=["gdA"])
        for g in range(2):
            for hf in range(2):
                MM(P[4 + hf][:, 0:512], gdA[0:17, g * 128:(g + 1) * 128], wgA[0:17, hf * 512:(hf + 1) * 512],
                   True, True, r=["gdA", "wgA"], w=["P%d" % (4 + hf)])
                ACT(nsp[:, g, hf * 512:(hf + 1) * 512], P[4 + hf][:, 0:512], AF.Exp, r=["P%d" % (4 + hf)],
                    w=[("nsp", g)], scale=-1.0)
            TS(nsp[:, g, :], nsp[:, g, :], 1.0, None, OP.add, None, r=[("nsp", g)], w=[("nsp", g)])
            ACT(nsp[:, g, :], nsp[:, g, :], AF.Ln, r=[("nsp", g)], w=[("nsp", g)])
        for h in range(8):
            pb = 4 + (h % 2)
            for g in range(2):
                MM(P[pb][:, g * 128:(g + 1) * 128], nsp[:, g, h * 128:(h + 1) * 128], triN[:], True, True,
                   r=[("nsp", g), "triN"], w=["P%d" % pb])
            ACT(erevT[:, h, :], P[pb][:, 0:TT], AF.Exp, r=["P%d" % pb], w=[("erevT", h)])
            for g in range(2):
                MM(P[6][:, h * 4 + g * 2:h * 4 + g * 2 + 2], nsp[:, g, h * 128:(h + 1) * 128], chunkN[:], True, True,
                   r=[("nsp", g), "chunkN"], w=["P6"])
        ACT(dec[:], P[6][:, 0:32], AF.Exp, r=["P6"], w=["dec"])

    def token_mix(src, mode, ws):
        main = mode == "main"
        load_norm(src, g1c)
        wi = [0]

        def nextw():
            b = ws.get(wi[0])
            wi[0] += 1
            return b

        yv = yT[:].rearrange("p (b t) -> p b t", b=16)

        def conv_taps(blk):
            TS(yv[:, blk, :], uT[:, blk, 0:TT], wdwc[:, blk * 31:blk * 31 + 1], bdc[:, blk:blk + 1], OP.mult, OP.add,
               r=[("uT", blk), "uTh", "cols"], w=[("yT", blk)])
            for j in range(1, 31):
                STT(yv[:, blk, :], uT[:, blk, j:j + TT], wdwc[:, blk * 31 + j:blk * 31 + j + 1], yv[:, blk, :], OP.mult, OP.add,
                    r=[("uT", blk), "uTh", "cols", ("yT", blk)], w=[("yT", blk)])

        if mode != "pre":
            if main:
                CP("act", uT[:, :, 0:30], halo[:], r=["halo"], w=["uTh"])
            for blk in range(16):
                b = nextw()
                pcb, pkey = proj(b, xnT, "xnT")
                ACT(sig[blk % 2][:], pcb[:, 0:TT], AF.Sigmoid, r=[pkey], w=[("sig", blk % 2)])
                b = nextw()
                pca, pkey = proj(b, xnT, "xnT")
                TTo(uT[:, blk, 30:30 + TT], pca[:, 0:TT], sig[blk % 2][:], OP.mult, r=[pkey, ("sig", blk % 2)], w=[("uT", blk)])
            if not main:
                CP("act", halo[:], uT[:, :, TT:TT + 30], r=["uT"], w=["halo"])
        gla_gate()
        for h in range(8):
            b = nextw()
            pk, pkey = proj(b, xnT, "xnT")
            kt = kdT[h % 2]
            TTo(kt[:], pk[:, 0:TT], erevT[:, h, :], OP.mult, r=[pkey, ("erevT", h)], w=[("kdT", h % 2)])
            for g in range(2):
                TR(P3[:, g * 128:(g + 1) * 128], kt[:, g * 128:(g + 1) * 128], identB[:],
                   r=[("kdT", h % 2), "identB"], w=["P3"])
            CP("act", kd[:, :, h, :], P3[:, 0:256].rearrange("p (g n) -> p g n", g=2), r=["P3"], w=[("kd", h)])
            if main:
                conv_taps(2 * h)
            for a in range(2):
                b = nextw()
                pv, pkey = proj(b, xnT, "xnT")
                vtmp = sqb[a]
                CP("act", vtmp[:, 0:TT], pv[:, 0:TT], r=[pkey], w=[("sqb", a)])
                for g in range(2):
                    TR(P2b[:, g * 128:(g + 1) * 128], vtmp[:, g * 128:(g + 1) * 128], identB[:],
                       r=[("sqb", a), "identB"], w=["P2"])
                CP("act", vtok[:, :, (2 * h + a) * 128:(2 * h + a + 1) * 128],
                   P2b[:, 0:256].rearrange("p (g n) -> p g n", g=2), r=["P2"], w=[("vtok", h)])
            if main:
                conv_taps(2 * h + 1)
                b = nextw()
                pq_, pkey = proj(b, xnT, "xnT")
                ACT(qT[:, h, :], pq_[:, 0:TT], AF.Copy, r=[pkey], w=[("qT", h)], scale=128.0 ** -0.5)
                for a in range(2):
                    b = nextw()
                    pr, pkey = proj(b, xnT, "xnT")
                    ACT(srT[:, 2 * h + a, :], pr[:, 0:TT], AF.Silu, r=[pkey], w=[("srT", 2 * h + a)])
            po = P[6 + (h % 2)]
            pokey = "P%d" % (6 + (h % 2))
            for c in range(4):
                g, hf = c // 2, c % 2
                pkv = P[4 + (c % 2)]
                kvkey = "P%d" % (4 + (c % 2))
                MM(pkv[:, 0:256], kd[hf * 64:(hf + 1) * 64, g, h, :], vtok[hf * 64:(hf + 1) * 64, g, h * 256:(h + 1) * 256],
                   True, True, r=[("kd", h), ("vtok", h)], w=[kvkey])
                STT(Sst[:, h, :], Sst[:, h, :], dec[:, h * 4 + c:h * 4 + c + 1], pkv[:, 0:256], OP.mult, OP.add,
                    r=[("Sst", h), "dec", kvkey], w=[("Sst", h)])
                if main:
                    CP("act", Sbf[:, h, :], Sst[:, h, :], r=[("Sst", h)], w=[("Sbf", h)])
                    for a in range(2):
                        MM(po[:, a * 256 + c * 64:a * 256 + (c + 1) * 64], Sbf[:, h, a * 128:(a + 1) * 128],
                           qT[:, h, c * 64:(c + 1) * 64], True, True, r=[("Sbf", h), ("qT", h)], w=[pokey])
            if main:
                ACT(sqb[0][:], po[:, 0:512], AF.Square, r=[pokey], w=[("sqb", 0)])
                MM(P[4][:, 0:TT], onesB[:], sqb[0][:, 0:TT], True, False, r=[("sqb", 0), "onesB"], w=["P4"])
                MM(P[4][:, 0:TT], onesB[:], sqb[0][:, TT:2 * TT], False, True, r=[("sqb", 0), "onesB"], w=["P4"])
                TS(tf[0][:], P[4][:, 0:TT], 1.0 / 256, EPS, OP.mult, OP.add, r=["P4"], w=[("tf", 0)])
                ACT(tf[0][:], tf[0][:], AF.Sqrt, r=[("tf", 0)], w=[("tf", 0)])
                S.op("dve", lambda e: e.reciprocal(out=tf[0][:], in_=tf[0][:]), r=[("tf", 0)], w=[("tf", 0)])
                for a in range(2):
                    blk = 2 * h + a
                    TTo(tf[1 + a][:], po[:, a * 256:(a + 1) * 256], tf[0][:], OP.mult, r=[pokey, ("tf", 0)], w=[("tf", 1 + a)])
                    STT(mixT[:, blk, :], tf[1 + a][:], ggc[:, blk:blk + 1], srT[:, blk, :], OP.mult, OP.mult,
                        r=[("tf", 1 + a), "cols", ("srT", blk)], w=[("mixT", blk)])
        if not main:
            return
        CP("act", halo[:], uT[:, :, TT:TT + 30], r=["uT"], w=["halo"])
        for blk in range(16):
            ACT(sqb[0][:, 0:TT], yv[:, blk, :], AF.Copy, r=[("yT", blk)], w=[("sqb", 0)])
            MM(P[4][:, 0:TT], onesB[:], sqb[0][:, 0:TT], blk == 0, blk == 15, r=[("sqb", 0), "onesB"], w=["P4"])
            ACT(sqb[1][:, 0:TT], yv[:, blk, :], AF.Square, r=[("yT", blk)], w=[("sqb", 1)])
            MM(P[5][:, 0:TT], onesB[:], sqb[1][:, 0:TT], blk == 0, blk == 15, r=[("sqb", 1), "onesB"], w=["P5"])
        TS(tf[0][:], P[4][:, 0:TT], 1.0 / 2048, None, OP.mult, None, r=["P4"], w=[("tf", 0)])
        TTo(tf[1][:], tf[0][:], tf[0][:], OP.mult, r=[("tf", 0)], w=[("tf", 1)])
        STT(tf[1][:], P[5][:, 0:TT], 1.0 / 2048, tf[1][:], OP.mult, OP.subtract, r=["P5", ("tf", 1)], w=[("tf", 1)])
        TS(tf[1][:], tf[1][:], EPS, None, OP.add, None, r=[("tf", 1)], w=[("tf", 1)])
        ACT(tf[1][:], tf[1][:], AF.Sqrt, r=[("tf", 1)], w=[("tf", 1)])
        S.op("dve", lambda e: e.reciprocal(out=tf[1][:], in_=tf[1][:]), r=[("tf", 1)], w=[("tf", 1)])
        for blk in range(16):
            TTo(yv[:, blk, :], yv[:, blk, :], tf[0][:], OP.subtract, r=[("yT", blk), ("tf", 0)], w=[("yT", blk)])
            TTo(yv[:, blk, :], yv[:, blk, :], tf[1][:], OP.mult, r=[("yT", blk), ("tf", 1)], w=[("yT", blk)])
            TS(yv[:, blk, :], yv[:, blk, :], lgc[:, blk:blk + 1], lbc[:, blk:blk + 1], OP.mult, OP.add,
               r=[("yT", blk), "cols"], w=[("yT", blk)])
            ACT(mixT[:, 16 + blk, :], yv[:, blk, :], AF.Silu, r=[("yT", blk)], w=[("mixT", 16 + blk)])

    def out_proj(ws):
        for ob in range(32):
            b = ws.get(ob)
            po_, pkey = proj(b, mixT, "mixT")
            TTo(xT[:, ob, :], po_[:, 0:TT], xT[:, ob, :], OP.add, r=[pkey, ("xT", ob // 4)], w=[("xT", ob // 4)])

    def peer(wsq, wsu, vsrc):
        norm_xT(g2c)
        for blk in range(16):
            b = wsq.get(blk)
            pp, pkey = proj(b, xnT, "xnT")
            ACT(pqT[:, blk, :], pp[:, 0:TT], AF.Copy, r=[pkey], w=[("pqT", blk)])
        for g in range(2):
            for quad in range(4):
                for i in range(4):
                    blk = quad * 4 + i
                    MM(P[2][:, i * 128:(i + 1) * 128], pqT[:, blk, g * 128:(g + 1) * 128], keysT[:, blk, :], True, True,
                       r=[("pqT", blk), "keysT"], w=["P2"])
                CP(cp_eng(), sall[:, g, quad * 4:(quad + 1) * 4, :], P[2][:, 0:512].rearrange("p (a n) -> p a n", a=4),
                   r=["P2"], w=[("sall", g)])
        def topk_chain(g, h):
            v1, v2, c24, cand, wk, sm, d16 = v1s[g], v2s[g], c24s[g], cands[g], wks[g], sms[g], d16s[g]
            ta, tb = gt[g], ga[g]
            K = lambda n: (n, g)
            s1 = sall[:, g, 2 * h, :]
            s2 = sall[:, g, 2 * h + 1, :]
            for (s_, v_, vk) in ((s1, v1, K("v1")), (s2, v2, K("v2"))):
                S.op("dve", lambda e, s_=s_, v_=v_: e.max(out=v_[:, 0:8], in_=s_), r=[("sall", g)], w=[vk])
                yield
                S.op("dve", lambda e, s_=s_, v_=v_: e.match_replace(out=wk[:, 0:128], in_to_replace=v_[:, 0:8], in_values=s_,
                                                                    imm_value=-1e30), r=[("sall", g), vk], w=[K("wk")])
                yield
                S.op("dve", lambda e, v_=v_: e.max(out=v_[:, 8:16], in_=wk[:, 0:128]), r=[K("wk")], w=[vk])
                yield
            TTo(cand[:].rearrange("p (i j) -> p i j", i=16), v1[:].unsqueeze(2).to_broadcast([128, 16, 16]),
                v2[:].unsqueeze(1).to_broadcast([128, 16, 16]), OP.add, r=[K("v1"), K("v2")], w=[K("cand")])
            yield
            S.op("dve", lambda e: e.max(out=c24[:, 0:8], in_=cand[:]), r=[K("cand")], w=[K("c24")])
            yield
            S.op("dve", lambda e: e.match_replace(out=wk[:], in_to_replace=c24[:, 0:8], in_values=cand[:], imm_value=-1e30),
                 r=[K("cand"), K("c24")], w=[K("wk")])
            yield
            S.op("dve", lambda e: e.max(out=c24[:, 8:16], in_=wk[:]), r=[K("wk")], w=[K("c24")])
            yield
            S.op("dve", lambda e: e.match_replace(out=cand[:], in_to_replace=c24[:, 8:16], in_values=wk[:], imm_value=-1e30),
                 r=[K("wk"), K("c24")], w=[K("cand")])
            yield
            S.op("dve", lambda e: e.max(out=c24[:, 16:24], in_=cand[:]), r=[K("cand")], w=[K("c24")])
            yield
            TS(sm[:, 0:1], c24[:, 15:16], c24[:, 16:17], 0.5, OP.add, OP.mult, r=[K("c24")], w=[K("sm")])
            yield
            TS(d16[:], c24[:, 0:16], c24[:, 0:1], None, OP.subtract, None, r=[K("c24")], w=[K("d16")])
            yield
            ACT(d16[:], d16[:], AF.Exp, r=[K("d16")], w=[K("d16"), K("smz")], accum_out=sm[:, 1:2])
            yield
            S.op("dve", lambda e: e.reciprocal(out=sm[:, 2:3], in_=sm[:, 1:2]), r=[K("smz"), K("sm")], w=[K("sm")])
            yield
            TS(ta[:], s1, v1[:, 0:1], None, OP.subtract, None, r=[("sall", g), K("v1")], w=[("gt", g)])
            yield
            ACT(ta[:], ta[:], AF.Exp, r=[("gt", g)], w=[("gt", g)])
            yield
            TS(tb[:], s2, v2[:, 0:1], None, OP.subtract, None, r=[("sall", g), K("v2")], w=[("ga", g)])
            yield
            ACT(tb[:], tb[:], AF.Exp, r=[("ga", g)], w=[("ga", g)])
            yield
            STT(ta[:], s1, v1[:, 15:16], ta[:], OP.is_ge, OP.mult, r=[("sall", g), K("v1"), ("gt", g)], w=[("gt", g)])
            yield
            TS(E1[:, g, h, :], ta[:], sm[:, 2:3], None, OP.mult, None, r=[("gt", g), K("sm")], w=[("E1", g)])
            yield
            STT(E2[:, g, h, :], s2, v2[:, 15:16], tb[:], OP.is_ge, OP.mult, r=[("sall", g), K("v2"), ("ga", g)], w=[("E2", g)])
            yield
            TS(TH[:, g, h, :], s1, -1.0, sm[:, 0:1], OP.mult, OP.add, r=[("sall", g), K("sm")], w=[("TH", g)])
            yield

        for h in range(8):
            chains = [topk_chain(0, h), topk_chain(1, h)]
            while chains:
                for c in list(chains):
                    try:
                        next(c)
                    except StopIteration:
                        chains.remove(c)

        vctr = [0]
        wring = [0]

        def a_mms(n1):
            b = wsu.get(n1)
            bank = n1 % 2
            return [lambda kc=kc, b=b, bank=bank: MM(P[bank][:, 0:TT], wt[b][:, kc, :], xnT[:, kc, :], kc == 0, kc == KC - 1,
                                                     r=[("wt", b), "xnT"], w=["P%d" % bank]) for kc in range(KC)]

        def stage_G(n1):
            pe_ops = []
            sbuf_, skey = GSLOT[n1 % 2]
            for g in range(2):
                for h in range(8):
                    i = wring[0]
                    wring[0] += 1
                    gb, wb = gtr[i % 4], Wb[i % 16]
                    STT(gb[:], sall[:, g, 2 * h + 1, :], TH[:, g, h, n1:n1 + 1], E2[:, g, h, :], OP.is_ge, OP.mult,
                        r=[("sall", g), ("TH", g), ("E2", g)], w=[("gtr", i % 4)])
                    ACT(wb[:], gb[:], AF.Copy, r=[("gtr", i % 4), ("E1", g)], w=[("Wb", i % 16)], scale=E1[:, g, h, n1:n1 + 1])
                    pe_ops.append(lambda g=g, h=h, wb=wb, i=i, sbuf_=sbuf_, skey=skey: MM(
                        sbuf_[:, g * 128:(g + 1) * 128], wb[:], identB[:], h == 0, h == 7,
                        r=[("Wb", i % 16), "identB"], w=[skey]))
            return pe_ops

        def stage_T(n1):
            eg, eb = n1 // 8, n1 % 8
            hb = eg % 2
            sbuf_, skey = GSLOT[n1 % 2]
            TTo(HT[hb][:, eb, :], gl[n1 % 2][:], sbuf_[:, 0:256], OP.mult, r=[("gl", n1 % 2), skey], w=[("HT", hb)])

        def v_chunk(eg, ds):
            hb = eg % 2
            vb = vctr[0] % 2
            vctr[0] += 1
            DMA("sp", vt[vb][:].rearrange("p e d -> p (e d)"), vrb[eg * 8 + ds], r=[("vrb", (eg * 8 + ds) // 16)],
                w=[("vt", vb)])
            for dq in range(4):
                db = ds * 4 + dq
                for e8 in range(8):
                    MM(P[4 + dq][:, 0:TT], vt[vb][:, e8, dq * 128:(dq + 1) * 128], HT[hb][:, e8, :], e8 == 0, e8 == 7,
                       r=[("vt", vb), ("HT", hb)], w=["P%d" % (4 + dq)])
                TTo(xT[:, db, :], P[4 + dq][:, 0:TT], xT[:, db, :], OP.add, r=["P%d" % (4 + dq), ("xT", db // 4)], w=[("xT", db // 4)])

        pend = []
        for n1 in range(129):
            if n1 < 128:
                k = 0
                for j, f in enumerate(a_mms(n1)):
                    f()
                    if j % 2 == 1 and k < len(pend):
                        pend[k]()
                        k += 1
                while k < len(pend):
                    pend[k]()
                    k += 1
                ACT(gl[n1 % 2][:], P[n1 % 2][:, 0:TT], AF.Gelu, r=["P%d" % (n1 % 2)], w=[("gl", n1 % 2)])
                new_pend = stage_G(n1)
            else:
                for f in pend:
                    f()
                new_pend = []
            if n1 >= 1:
                m = n1 - 1
                stage_T(m)
                if m >= 8:
                    v_chunk(m // 8 - 1, m % 8)
            pend = new_pend
        for ds in range(8):
            v_chunk(15, ds)

    def final_out(dst):
        fm_rstd(D, KC, lambda kc: xT[:, kc, :], lambda kc: ("xT", kc // 4))
        for kc in range(KC):
            STT(xT[:, kc, :], xT[:, kc, :], gfc[:, kc:kc + 1], rstd[:], OP.mult, OP.mult,
                r=[("xT", kc // 4), "cols", "rstd"], w=[("xT", kc // 4)])
        for g in range(2):
            ob_, ok_ = (xin, "xin") if g == 0 else (xinB, "xinB")
            for kq in range(8):
                for i in range(4):
                    kc = kq * 4 + i
                    TR(P[2][:, i * 128:(i + 1) * 128], xT[:, kc, g * 128:(g + 1) * 128], identF[:],
                       r=[("xT", kq), "identF"], w=["P2"])
                CP(cp_eng(), ob_[:, kq * 512:(kq + 1) * 512], P[2][:, 0:512], r=["P2"], w=[ok_])
            DMA("sp", dst[g * 128:(g + 1) * 128, :], ob_[:], r=[ok_], w=["y"])

    def win_srcs(mode):
        idx = []
        if mode != "pre":
            idx += [48 + i for i in range(32)]
        for h in range(8):
            idx += [h * 6 + i for i in (range(6) if mode == "main" else range(3))]
        return [(winb[i], ("winb", i // 16)) for i in idx]

    for t in range(n_pre):
        mode = "pre_last" if t == n_pre - 1 else "pre"
        token_mix(xp[t * TT:(t + 1) * TT, :], mode, WStream(win_srcs(mode)))
        if late_conv and t < n_pre - 1:
            late_conv.pop(0)(["dec"])
        if t == n_pre - 1:
            while late_conv:
                late_conv.pop(0)(["dec"])
            S.barrier()
    for t in range(n_main):
        token_mix(xm[t * TT:(t + 1) * TT, :], "main", WStream(win_srcs("main")))
        out_proj(WStream([(woutb[i], ("woutb", i // 16)) for i in range(32)]))
        S.barrier()
        peer(WStream([(wqb[i], ("wqb", i // 16)) for i in range(16)]),
             WStream([(utb[i], ("utb", i // 16)) for i in range(128)]), vrd)
        final_out(y[t * TT:(t + 1) * TT, :])
        S.barrier()
    S.finish()
    S.run()
    return nc


def _blk(wm):
    k, n = wm.shape
    return np.ascontiguousarray(wm.reshape(k // 128, 128, n // 128, 128).transpose(2, 1, 0, 3)).reshape(n // 128, 128, k)


def _consts():
    c = np.zeros((128, 258), np.float32)
    c[:, 0:128] = np.eye(128, dtype=np.float32)
    t = np.arange(128)
    c[:, 128:256] = np.where((t[:, None] > t[None, :]) & ((t[:, None] // 64) == (t[None, :] // 64)), -1.0 / 16.0, 0.0)
    c[:, 256] = np.where(t < 64, -1.0 / 16.0, 0.0)
    c[:, 257] = np.where(t >= 64, -1.0 / 16.0, 0.0)
    return c


def prep_weights(w_in, w_gate_up, b_gate, gla_norm_g, w_dw, b_dw, conv_ln_g, conv_ln_b, w_out, norm1_g, norm2_g,
                 peer_wq, peer_keys1, peer_keys2, peer_u, peer_v, final_norm_g):
    f = lambda a: np.asarray(a, dtype=np.float32)
    wi = f(w_in)[0]
    q, k, v, r = wi[:, 0:1024], wi[:, 1024:2048], wi[:, 2048:4096], wi[:, 4096:6144]
    gd, ca, cb = wi[:, 6144:6160], wi[:, 6160:8208], wi[:, 8208:10256]
    qb, kb, vb, rb, cab, cbb = _blk(q), _blk(k), _blk(v), _blk(r), _blk(ca), _blk(cb)
    blocks = []
    for h in range(8):
        blocks += [kb[h], vb[2 * h], vb[2 * h + 1], qb[h], rb[2 * h], rb[2 * h + 1]]
    for b in range(16):
        blocks += [cbb[b], cab[b]]
    win = np.stack(blocks)
    wgd = np.ascontiguousarray(gd.reshape(32, 128, 16).transpose(1, 0, 2)).reshape(128, 512)
    wout = _blk(f(w_out)[0])
    wq = _blk(f(peer_wq)[0])
    ut = np.ascontiguousarray(f(peer_u)[0].reshape(128, 128, 32, 128).transpose(0, 3, 2, 1)).reshape(128, 128, 4096)
    vr = np.ascontiguousarray(f(peer_v)[0].reshape(16, 8, 128, 8, 512).transpose(0, 3, 2, 1, 4)).reshape(128, 128, 4096)
    k1, k2 = f(peer_keys1)[0], f(peer_keys2)[0]
    keyst = np.zeros((128, 16, 128), np.float32)
    for h in range(8):
        keyst[:, 2 * h, :] = k1[h].T
        keyst[:, 2 * h + 1, :] = k2[h].T
    keyst = keyst.reshape(128, 2048)
    wga = np.concatenate([f(w_gate_up)[0], f(b_gate)[0][None, :]], axis=0)
    colv = lambda a, n: np.ascontiguousarray(f(a).reshape(n, 128).T)
    cols = np.concatenate([
        colv(norm1_g[0], 32), colv(norm2_g[0], 32), colv(final_norm_g, 32),
        colv(gla_norm_g[0], 16), colv(conv_ln_g[0], 16), colv(conv_ln_b[0], 16), colv(b_dw[0], 16),
        np.ascontiguousarray(f(w_dw)[0].reshape(31, 16, 128).transpose(2, 1, 0)).reshape(128, 496),
    ], axis=1)
    assert cols.shape == (128, NCOLS)
    return dict(win=win, wgd=wgd, wout=wout, wq=wq, ut=ut, vr=vr, keyst=keyst, wga=wga,
                cols=np.ascontiguousarray(cols), consts=_consts())


def make_core_inputs(xb, meta, start, n_main, n_pre):
    xm = np.ascontiguousarray(xb[start:start + n_main * TT])
    xp = np.zeros((n_pre * TT, D), np.float32)
    pre = np.concatenate([meta, xb[:start]], axis=0)
    assert pre.shape[0] <= n_pre * TT
    xp[n_pre * TT - pre.shape[0]:] = pre
    return xm, xp


_NC_CACHE = {}


def kernel(x, meta_tokens, norm1_g, w_in, w_gate_up, b_gate, gla_norm_g, w_dw, b_dw, conv_ln_g, conv_ln_b,
           w_out, norm2_g, peer_wq, peer_keys1, peer_keys2, peer_u, peer_v, final_norm_g):
    x = np.asarray(x, dtype=np.float32)
    meta = np.asarray(meta_tokens, dtype=np.float32)
    wts = prep_weights(w_in, w_gate_up, b_gate, gla_norm_g, w_dw, b_dw, conv_ln_g, conv_ln_b, w_out, norm1_g,
                       norm2_g, peer_wq, peer_keys1, peer_keys2, peer_u, peer_v, final_norm_g)
    B, L, _ = x.shape
    per = N_MAIN_TILES * TT
    in_maps = []
    for c in range(8):
        b, j = c // 4, c % 4
        xm, xp = make_core_inputs(x[b], meta, j * per, N_MAIN_TILES, N_PRE_TILES)
        m = dict(wts)
        m["xm"] = xm
        m["xp"] = xp
        in_maps.append(m)
    if "nc" not in _NC_CACHE:
        _NC_CACHE["nc"] = build_nc()
    res = run_bass_kernel_spmd(_NC_CACHE["nc"], in_maps, core_ids=list(range(8)))
    out = np.empty((B, L, D), np.float32)
    for c in range(8):
        b, j = c // 4, c % 4
        out[b, j * per:(j + 1) * per] = res.results[c]["y"]
    return out
```
